# Optimizing a Trainium2 kernel written in Bass

```python
import jax, jax.numpy as jnp
from jax import lax
import numpy as np

D_MODEL = 4096
BATCH = 1
SEQ = 16384
DEPTH = 1

HG_WIDTH = D_MODEL // 2
CONV_WIDTH = D_MODEL - HG_WIDTH
HG_EXPAND = 128
HG_HEADS = HG_WIDTH // HG_EXPAND
HG_VDIM = HG_WIDTH // HG_HEADS
HG_CHUNK = 64
CONV_KERNEL = 31
IN_COLS = 4 * HG_WIDTH + 2 * CONV_WIDTH
PEER_HEADS = 8
PEER_QDIM = 256
PEER_HALF = PEER_QDIM // 2
N_KEYS = 128
N_EXPERTS = N_KEYS * N_KEYS
PEER_TOPK = 16
PEER_BLOCK = 128
ADA_SLOTS = 6
NORM_EPS = 1e-6
LN_EPS = 1e-5

kernel_name = 'hymba_style_hgrn2_conformer_peer_adaln'


def rms_norm(x, gain):
    xf = x.astype(jnp.float32)
    y = xf * lax.rsqrt(jnp.mean(xf * xf, axis=-1, keepdims=True) + NORM_EPS)
    return (y * gain.astype(jnp.float32)).astype(x.dtype)


def modulate(h, shift, scale):
    return h * (1 + scale[:, None, :]) + shift[:, None, :]


def hgrn2_group(q, fz, i, g, lb, norm_g):
    B, S, _ = q.shape
    f32 = jnp.float32
    qf = jax.nn.silu(q.astype(f32))
    lbf = lb.astype(f32)
    f = lbf + (1 - lbf) * jax.nn.sigmoid(fz.astype(f32))
    k = 1 - f
    logf = jnp.log(f)
    v = i.astype(f32)
    nc = S // HG_CHUNK

    def chunks(t, d):
        return t.reshape(B, nc, HG_CHUNK, HG_HEADS, d).transpose(1, 0, 3, 2, 4)

    qc = chunks(qf, HG_EXPAND)
    kc = chunks(k, HG_EXPAND)
    vc = chunks(v, HG_VDIM)
    bc = jnp.cumsum(chunks(logf, HG_EXPAND), axis=3)
    causal = jnp.tril(jnp.ones((HG_CHUNK, HG_CHUNK), dtype=bool))

    def step(state, inp):
        qt, kt, vt, bt = inp
        diff = bt[:, :, :, None, :] - bt[:, :, None, :, :]
        decay = jnp.exp(jnp.where(causal[:, :, None], diff, -jnp.inf))
        scores = jnp.einsum('bhtk,bhsk,bhtsk->bhts', qt, kt, decay)
        o = jnp.einsum('bhts,bhsv->bhtv', scores, vt) + jnp.einsum('bhtk,bhkv->bhtv', qt * jnp.exp(bt), state)
        blast = bt[:, :, -1:, :]
        state = jnp.exp(blast[:, :, 0, :])[..., None] * state + jnp.einsum('bhsk,bhsv->bhkv', kt * jnp.exp(blast - bt), vt)
        return state, o

    state0 = jnp.zeros((B, HG_HEADS, HG_EXPAND, HG_VDIM), f32)
    _, o = lax.scan(step, state0, (qc, kc, vc, bc))
    o = o.transpose(1, 0, 3, 2, 4).reshape(B, S, HG_HEADS, HG_VDIM)
    o = o * lax.rsqrt(jnp.mean(o * o, axis=-1, keepdims=True) + NORM_EPS)
    o = o * norm_g.astype(f32).reshape(HG_HEADS, HG_VDIM)
    o = o.reshape(B, S, HG_WIDTH) * jax.nn.silu(g.astype(f32))
    return o.astype(q.dtype)


def conformer_conv_group(a, b, w_dw, b_dw, ln_g, ln_b):
    u = a * jax.nn.sigmoid(b)
    y = lax.conv_general_dilated(
        u, w_dw[:, None, :].astype(u.dtype), window_strides=(1,),
        padding=[(CONV_KERNEL - 1, 0)], dimension_numbers=('NWC', 'WIO', 'NWC'),
        feature_group_count=CONV_WIDTH) + b_dw
    yf = y.astype(jnp.float32)
    mu = jnp.mean(yf, axis=-1, keepdims=True)
    var = jnp.mean(jnp.square(yf - mu), axis=-1, keepdims=True)
    yn = (yf - mu) * lax.rsqrt(var + LN_EPS) * ln_g.astype(jnp.float32) + ln_b.astype(jnp.float32)
    return jax.nn.silu(yn).astype(a.dtype)


def peer_layer(h, w_pq, keys1, keys2, u, v):
    B, S, D = h.shape
    T = B * S
    hf = h.reshape(T, D)
    q = (hf @ w_pq).reshape(T, PEER_HEADS, 2, PEER_HALF).astype(jnp.float32)
    s1 = jnp.einsum('thd,hnd->thn', q[:, :, 0], keys1.astype(jnp.float32))
    s2 = jnp.einsum('thd,hnd->thn', q[:, :, 1], keys2.astype(jnp.float32))
    v1, i1 = lax.top_k(s1, PEER_TOPK)
    v2, i2 = lax.top_k(s2, PEER_TOPK)
    cand_s = (v1[..., :, None] + v2[..., None, :]).reshape(T, PEER_HEADS, PEER_TOPK * PEER_TOPK)
    cand_i = (i1[..., :, None] * N_KEYS + i2[..., None, :]).reshape(T, PEER_HEADS, PEER_TOPK * PEER_TOPK)
    top_s, pos = lax.top_k(cand_s, PEER_TOPK)
    idx = jnp.take_along_axis(cand_i, pos, axis=-1)
    gate = jax.nn.softmax(top_s, axis=-1).astype(h.dtype)
    n_sel = PEER_HEADS * PEER_TOPK
    nb = T // PEER_BLOCK

    def block(args):
        xb, ib, gb = args
        a = jnp.einsum('td,tkd->tk', xb, u[ib])
        return jnp.einsum('tk,tkd->td', jax.nn.gelu(a, approximate=False) * gb, v[ib])

    y = lax.map(block, (hf.reshape(nb, PEER_BLOCK, D), idx.reshape(nb, PEER_BLOCK, n_sel),
                        gate.reshape(nb, PEER_BLOCK, n_sel)))
    return y.reshape(B, S, D)


def setup_inputs(seed: int = 0) -> dict:
    key = jax.random.key(seed)
    ks = jax.random.split(key, 24)
    f32 = jnp.float32

    def nrm(k, shape, s):
        return jax.random.normal(k, shape, f32) * s

    D = D_MODEL
    return {
        'x': nrm(ks[0], (BATCH, SEQ, D), 1.0),
        'c': nrm(ks[1], (BATCH, D), 1.0),
        'w_ada': nrm(ks[2], (DEPTH, D, ADA_SLOTS * D), 0.5 * D ** -0.5),
        'b_ada': nrm(ks[3], (DEPTH, ADA_SLOTS * D), 0.01),
        'w_ada_out': nrm(ks[4], (D, 2 * D), 0.5 * D ** -0.5),
        'b_ada_out': nrm(ks[5], (2 * D,), 0.01),
        'g_mix': 1.0 + nrm(ks[6], (DEPTH, D), 0.01),
        'g_ffn': 1.0 + nrm(ks[7], (DEPTH, D), 0.01),
        'g_out': 1.0 + nrm(ks[8], (D,), 0.01),
        'w_in': nrm(ks[9], (DEPTH, D, IN_COLS), D ** -0.5),
        'b_glu': nrm(ks[10], (DEPTH, 2 * CONV_WIDTH), 0.01),
        'lb_logits': nrm(ks[11], (DEPTH + 1, HG_WIDTH), 0.1),
        'w_dw': nrm(ks[12], (DEPTH, CONV_KERNEL, CONV_WIDTH), CONV_KERNEL ** -0.5),
        'b_dw': nrm(ks[13], (DEPTH, CONV_WIDTH), 0.01),
        'ln_g': 1.0 + nrm(ks[14], (DEPTH, CONV_WIDTH), 0.01),
        'ln_b': nrm(ks[15], (DEPTH, CONV_WIDTH), 0.01),
        'hg_norm_g': 1.0 + nrm(ks[16], (DEPTH, HG_WIDTH), 0.01),
        'w_out': nrm(ks[17], (DEPTH, HG_WIDTH + CONV_WIDTH, D), (HG_WIDTH + CONV_WIDTH) ** -0.5),
        'w_pq': nrm(ks[18], (DEPTH, D, PEER_HEADS * PEER_QDIM), D ** -0.5),
        'sub_keys1': nrm(ks[19], (DEPTH, PEER_HEADS, N_KEYS, PEER_HALF), PEER_HALF ** -0.5),
        'sub_keys2': nrm(ks[20], (DEPTH, PEER_HEADS, N_KEYS, PEER_HALF), PEER_HALF ** -0.5),
        'expert_u': nrm(ks[21], (DEPTH, N_EXPERTS, D), D ** -0.5),
        'expert_v': nrm(ks[22], (DEPTH, N_EXPERTS, D), 1.0),
    }


def reference(x, c, w_ada, b_ada, w_ada_out, b_ada_out, g_mix, g_ffn, g_out, w_in, b_glu,
              lb_logits, w_dw, b_dw, ln_g, ln_b, hg_norm_g, w_out, w_pq, sub_keys1, sub_keys2,
              expert_u, expert_v):
    c_act = jax.nn.silu(c)
    lb_all = jnp.cumsum(jax.nn.softmax(lb_logits.astype(jnp.float32), axis=0), axis=0)
    split_pts = [HG_WIDTH, 2 * HG_WIDTH, 3 * HG_WIDTH, 4 * HG_WIDTH]
    for l in range(DEPTH):
        mod = c_act @ w_ada[l] + b_ada[l]
        sh_m, sc_m, gt_m, sh_f, sc_f, gt_f = jnp.split(mod, ADA_SLOTS, axis=-1)
        h = modulate(rms_norm(x, g_mix[l]), sh_m, sc_m)
        proj = jnp.einsum('bsd,de->bse', h, w_in[l])
        qh, fh, ih, gh, conv_in = jnp.split(proj, split_pts, axis=-1)
        conv_a, conv_b = jnp.split(conv_in + b_glu[l], 2, axis=-1)
        y_hg = hgrn2_group(qh, fh, ih, gh, lb_all[l], hg_norm_g[l])
        y_cv = conformer_conv_group(conv_a, conv_b, w_dw[l], b_dw[l], ln_g[l], ln_b[l])
        y_mix = jnp.einsum('bse,ed->bsd', jnp.concatenate([y_hg, y_cv], axis=-1), w_out[l])
        x = x + gt_m[:, None, :] * y_mix
        h = modulate(rms_norm(x, g_ffn[l]), sh_f, sc_f)
        y_ffn = peer_layer(h, w_pq[l], sub_keys1[l], sub_keys2[l], expert_u[l], expert_v[l])
        x = x + gt_f[:, None, :] * y_ffn
    mod_o = c_act @ w_ada_out + b_ada_out
    sh_o, sc_o = jnp.split(mod_o, 2, axis=-1)
    return modulate(rms_norm(x, g_out), sh_o, sc_o)
```

```python
import numpy as np
from contextlib import ExitStack
import concourse.bass as bass
import concourse.mybir as mybir
from concourse.bass_utils import run_bass_kernel_spmd

F32 = mybir.dt.float32
BF16 = mybir.dt.bfloat16
AF = mybir.ActivationFunctionType
ALU = mybir.AluOpType
NEG = -1.0e30


class Cfg:
    def __init__(self, D=4096, SEQ=16384, PH=8, G=256):
        self.D = D; self.SEQ = SEQ; self.PH = PH; self.G = G
        self.KD = D // 128
        self.NH = (D // 2) // 128
        self.NCT = (D // 2) // 128
        self.CW = D // 2
        self.NCOL = 3 * D // 128
        self.NQT = 2 * PH
        self.NEC = 128
        self.SBC = 32
        self.NSB = self.NEC // self.SBC
        self.NT = G // 128
        self.NCH = G // 64
        self.NG = SEQ // G
        self.DQW = min(4, self.KD)
        self.NDQ = self.KD // self.DQW
        self.VU = D // (self.DQW * 128)
        self.T_WIN = 0
        self.T_WOUT = self.T_WIN + self.NCOL
        self.T_WPQ = self.T_WOUT + self.KD
        self.T_UT = self.T_WPQ + self.NQT
        self.T_V = self.T_UT + self.NEC
        self.NTILES = self.T_V + self.NEC
        o = 0
        def take(n):
            nonlocal o
            r = o; o += n; return r
        KD, NH, NCT = self.KD, self.NH, self.NCT
        self.V_C = take(KD); self.V_BADA = take(8 * KD)
        self.V_GMIX = take(KD); self.V_GFFN = take(KD); self.V_GOUT = take(KD)
        self.V_BGA = take(NCT); self.V_BGB = take(NCT)
        self.V_LB0 = take(NH); self.V_LB1 = take(NH)
        self.V_WDW = take(NCT * 31); self.V_BDW = take(NCT)
        self.V_LNG = take(NCT); self.V_LNB = take(NCT); self.V_HGN = take(NH)
        self.NV = o


class Tracker:
    EPOCH = 60000

    def __init__(self):
        self.streams = {n: [] for n in ("pe", "act", "dve", "pool", "sp")}
        self.cnt = {n: 0 for n in self.streams}
        self.epoch = {n: 0 for n in self.streams}
        self.waited = {n: {} for n in self.streams}
        self.keys = {}
        self.dcnt = {}
        self.semids = set()
        self.last_ev = {}
        self.pending = {n: False for n in self.streams}

    def _need(self, eng, ev, needs):
        if ev is None:
            return
        sid, val = ev
        if self.waited[eng].get(sid, 0) >= val:
            return
        if needs.get(sid, 0) < val:
            needs[sid] = val

    dead = False

    def emit(self, eng, fn, reads=(), writes=(), signal=True, dsem=None):
        if self.dead:
            return None
        needs = {}
        if dsem is None and self.cnt[eng] >= 55000 and not self.pending[eng]:
            self.epoch[eng] += 1; self.cnt[eng] = 0
        my_sid = (eng, self.epoch[eng])
        excl = my_sid if eng == "pe" else None
        for k in reads:
            st = self.keys.get(k)
            if st is not None:
                self._need(eng, st[0], needs)
        for k in writes:
            st = self.keys.get(k)
            if st is not None:
                lw = st[0]
                if lw is not None and lw[0] != excl:
                    self._need(eng, lw, needs)
                for sid, val in st[1].items():
                    if sid != excl:
                        self._need(eng, (sid, val), needs)
        waits = []
        for sid, val in needs.items():
            if sid[0] == eng and sid[1] == self.epoch[eng]:
                assert val <= self.cnt[eng], ("wait on future own milestone", eng, val, self.cnt[eng])
            elif sid[0] in self.cnt and sid[1] == self.epoch[sid[0]]:
                assert val <= self.cnt[sid[0]], ("wait on future milestone", eng, sid, val)
            waits.append((sid, val))
            self.waited[eng][sid] = val
        if dsem is not None:
            ep, c = self.dcnt.get(dsem, (0, 0))
            if c + 16 > self.EPOCH:
                ep += 1; c = 0
            c += 16
            self.dcnt[dsem] = (ep, c)
            sid = ("dma", dsem, ep)
            ev = (sid, c)
            inc = (sid, 16)
        elif signal:
            assert self.cnt[eng] + 1 <= self.EPOCH
            self.pending[eng] = False
            self.cnt[eng] += 1
            ev = (my_sid, self.cnt[eng])
            inc = (my_sid, 1)
        else:
            ev = (my_sid, self.cnt[eng] + 1)
            self.pending[eng] = True
            assert self.cnt[eng] + 1 <= self.EPOCH
            inc = None
        self.semids.add(ev[0])
        self.last_ev[ev[0]] = max(self.last_ev.get(ev[0], 0), ev[1])
        for k in reads:
            if isinstance(k, tuple) and k[0] == "psbw":
                continue
            st = self.keys.setdefault(k, [None, {}])
            st[1][ev[0]] = max(st[1].get(ev[0], 0), ev[1])
        for k in writes:
            self.keys[k] = [ev, {}]
        self.streams[eng].append((waits, fn, inc))
        return ev

    def barrier(self):
        evs = dict(self.last_ev)
        for eng in self.streams:
            waits = []
            for sid, val in evs.items():
                if sid[0] == eng:
                    continue
                if self.waited[eng].get(sid, 0) < val:
                    waits.append((sid, val)); self.waited[eng][sid] = val
            if waits:
                self.streams[eng].append((waits, None, None))
        self.keys = {}

    def final_wait(self, eng):
        waits = []
        for sid, val in self.last_ev.items():
            if sid[0] == eng:
                continue
            if self.waited[eng].get(sid, 0) < val:
                waits.append((sid, val)); self.waited[eng][sid] = val
        self.streams[eng].append((waits, None, None))

    def replay(self, nc, es):
        sems = {}
        for i, sid in enumerate(sorted(self.semids, key=str)):
            sems[sid] = es.enter_context(nc.semaphore("s%d" % i))
        blk = es.enter_context(nc.Block())
        T = self

        def run(name, e):
            for waits, fn, inc in T.streams[name]:
                for sid, val in waits:
                    e.wait_ge(sems[sid], val)
                if fn is not None:
                    ins = fn(e)
                    if inc is not None:
                        ins.then_inc(sems[inc[0]], inc[1])

        @blk.sync
        def _(e): run("sp", e)

        @blk.gpsimd
        def _(e): run("pool", e)

        @blk.scalar
        def _(e): run("act", e)

        @blk.vector
        def _(e): run("dve", e)

        @blk.tensor
        def _(e): run("pe", e)


def build(cfg):
    c = cfg
    D, KD, NH, NCT, G, NT, NCH, PH = c.D, c.KD, c.NH, c.NCT, c.G, c.NT, c.NCH, c.PH
    nc = bass.Bass("TRN2", target_bir_lowering=False)
    xT_d = nc.dram_tensor("xT", [c.NG, 128, KD, G], F32, kind="ExternalInput").ap()
    wall_d = nc.dram_tensor("wall", [c.NTILES, 128, D], F32, kind="ExternalInput").ap()
    wada_d = nc.dram_tensor("wada", [KD, 128, 8 * D], F32, kind="ExternalInput").ap()
    vec_d = nc.dram_tensor("vec", [128, c.NV], F32, kind="ExternalInput").ap()
    keys_d = nc.dram_tensor("keysT", [128, 2 * PH, 128], F32, kind="ExternalInput").ap()
    out_d = nc.dram_tensor("outT", [c.NG, 128, KD, G], F32, kind="ExternalOutput").ap()
    segs = [(c.T_WIN, c.T_WOUT), (c.T_WOUT, c.T_UT), (c.T_UT, c.T_V), (c.T_V, c.NTILES)]
    wbs = [nc.dram_tensor("wb%d" % j, [b_ - a_, 128, D], BF16, kind="Internal").ap() for j, (a_, b_) in enumerate(segs)]

    def wbt(i):
        for j, (a_, b_) in enumerate(segs):
            if a_ <= i < b_:
                return wbs[j][i - a_]
        raise IndexError(i)

    T = Tracker()
    es = ExitStack()

    def sb(name, shape, dt):
        return es.enter_context(nc.sbuf_tensor(name, shape, dt))

    xT = sb("xT_sb", [128, KD, G], F32)
    hT = sb("hT_sb", [128, KD, G], BF16)
    ycat = sb("ycat", [128, KD, G], BF16)
    PT = sb("PT", [128, c.SBC, G], BF16)
    NB = 3
    wring = sb("wring", [128, NB, D], BF16)
    GH = sb("GH", [128, NT, PH, 4, 128], BF16)
    NTMP = 18
    tmp = sb("tmp", [128, NTMP, G], F32)
    tb = sb("tb16", [128, 12, G], BF16)
    s12 = sb("s12", [128, NT, PH, 2, 128], F32)
    S32 = sb("S32", [128, NH, 128], F32)
    Sbf = sb("Sbf", [128, NH, 128], BF16)
    ubuf = sb("ubuf", [128, 2, 30 + G], F32)
    tails = sb("tails", [128, NCT, 30], F32)
    vec = sb("vec_sb", [128, c.NV], F32)
    modT = sb("modT", [128, 8 * KD], F32)
    der = sb("der", [128, 8 * KD + 4 * NH], F32)
    keysb = sb("keysb", [128, 2 * PH, 128], BF16)
    ident = sb("ident", [128, 128], BF16)
    onesf = sb("onesf", [128, 128], F32)
    cmask = sb("cmask", [128, 128], F32)
    smask = sb("smask", [128, G], F32)
    zer = sb("zer", [128, 512], BF16)
    cact = sb("cact", [128, KD], F32)
    obuf = sb("obuf", [128, 2, 2, G], F32)
    tk = sb("tk", [128, 8, 16], F32)
    tkw = sb("tkw", [128, 2, 256], F32)
    tau = sb("tau", [128, NT, PH], F32)
    nbias = sb("nbias", [128, NT, PH], F32)
    dch = sb("dch", [128, 2, NCH], F32)
    psum = [es.enter_context(nc.psum_tensor("ps%d" % i, [128, 512], F32)) for i in range(8)]

    def PSH(b, h):
        return psum[b][:, h * 256:h * 256 + G], [("ps", b)]

    def PSB(b):
        return psum[b][:, :], [("ps", b)]

    DV = {}
    o = 0
    for nm, n in (("gs_m", KD), ("sh_m", KD), ("gt_m", KD), ("gs_f", KD), ("sh_f", KD), ("gt_f", KD),
                  ("gs_o", KD), ("sh_o", KD), ("lb", NH), ("oml", NH), ("noml", NH), ("sp", NH)):
        DV[nm] = o; o += n

    def dv(nm, i):
        return der[:, DV[nm] + i:DV[nm] + i + 1]

    def vc(off, i):
        return vec[:, off + i:off + i + 1]

    def act(out, in_, func, r, w, bias=None, scale=None, accum=None):
        kw = {}
        if bias is not None: kw["bias"] = bias
        if scale is not None: kw["scale"] = scale
        if accum is not None: kw["accum_out"] = accum
        return T.emit("act", lambda e: e.activation(out=out, in_=in_, func=func, **kw), r, w)

    def tt(out, in0, in1, op, r, w, eng="dve"):
        return T.emit(eng, lambda e: e.tensor_tensor(out=out, in0=in0, in1=in1, op=op), r, w)

    def ts(out, in0, s1, s2, op0, op1, r, w, eng="dve"):
        if s2 is None:
            return T.emit(eng, lambda e: e.tensor_scalar(out=out, in0=in0, scalar1=s1, scalar2=None, op0=op0), r, w)
        return T.emit(eng, lambda e: e.tensor_scalar(out=out, in0=in0, scalar1=s1, scalar2=s2, op0=op0, op1=op1), r, w)

    def stt(out, in0, s, in1, op0, op1, r, w):
        return T.emit("dve", lambda e: e.scalar_tensor_tensor(out=out, in0=in0, scalar=s, in1=in1, op0=op0, op1=op1), r, w)

    def cp(eng, out, in_, r, w):
        if eng == "act":
            return T.emit("act", lambda e: e.copy(out=out, in_=in_), r, w)
        return T.emit(eng, lambda e: e.tensor_copy(out=out, in_=in_), r, w)

    def mm(out, lhsT, rhs, start, stop, r, w, signal=True, sgc=False):
        return T.emit("pe", lambda e: e.matmul(out, lhsT=lhsT, rhs=rhs, start=start, stop=stop, skip_group_check=sgc), r, w, signal=signal)

    def dma(q, out, in_, r, w, dsem):
        return T.emit(q, lambda e: e.dma_start(out=out, in_=in_), r, w, dsem=dsem)

    def TM(i):
        return tmp[:, i, :], [("tmp", i)]

    def TB(i):
        return tb[:, i, :], [("tb", i)]

    import os
    KSTOP = float(os.environ.get("KSTOP", "99"))

    def chk(n):
        if KSTOP <= n:
            T.dead = True

    T.emit("pool", lambda e: e.memset(onesf[:], 1.0), [], ["onesf"])
    T.emit("pool", lambda e: e.affine_select(out=ident[:], in_=onesf[:], pattern=[[-1, 128]], compare_op=ALU.is_equal,
                                              fill=0.0, base=0, channel_multiplier=1), ["onesf"], ["ident"])
    T.emit("pool", lambda e: e.affine_select(out=cmask[:], in_=onesf[:], pattern=[[1, 128]], compare_op=ALU.is_ge,
                                              fill=0.0, base=0, channel_multiplier=-1), ["onesf"], ["cmask"])
    T.emit("pool", lambda e: e.memset(cmask[0:64, 64:128], 0.0), [], ["cmask"])
    T.emit("pool", lambda e: e.memset(smask[:], 1.0), [], ["smask"])
    for ch in range(NCH):
        T.emit("pool", lambda e, ch=ch: e.memset(smask[:, ch * 64:ch * 64 + 1], 0.0), [], ["smask"])
    T.emit("pool", lambda e: e.memset(zer[:], 0.0), [], ["zer"])
    T.emit("pool", lambda e: e.memset(S32[:], 0.0), [], ["S32"])
    T.emit("pool", lambda e: e.memset(Sbf[:], 0.0), [], ["Sbf"])
    T.emit("pool", lambda e: e.memset(tails[:], 0.0), [], ["tails"])
    dma("sp", vec[:], vec_d, [], ["vec"], "d_vec")
    act(cact[:], vec[:, c.V_C:c.V_C + KD], AF.Silu, ["vec"], ["cact"])

    chk(0)
    stg = tmp[:].rearrange("p a b -> p (a b)")[:, 0:D]
    stg_keys = [("tmp", i) for i in range(NTMP)]
    kst = tmp[:].rearrange("p a b -> p (a b)")[:, 0:2 * PH * 128]
    dma("sp", kst, keys_d.rearrange("p a n -> p (a n)"), [], stg_keys, "d_stg")
    cp("dve", keysb[:].rearrange("p a n -> p (a n)"), kst, stg_keys, ["keysb"])
    NCB = 8 * D // D
    mp, mpk = PSB(7)
    mm(mp, zer[:, 0:128], zer[:, 0:512], True, False, ["zer"], mpk, sgc=True)
    for k in range(KD):
        for cbk in range(NCB):
            dma("sp", stg, wada_d[k, :, cbk * D:(cbk + 1) * D], [], stg_keys, "d_stg")
            nj = D // 128
            for j in range(nj):
                col = cbk * nj + j
                mm(psum[7][:, col:col + 1], stg[:, j * 128:(j + 1) * 128], cact[:, k:k + 1], False, True,
                   stg_keys + ["cact"], mpk, signal=(j == nj - 1), sgc=True)
    tt(modT[:], psum[7][:, 0:8 * KD], vec[:, c.V_BADA:c.V_BADA + 8 * KD], ALU.add, mpk + ["vec"], ["modT"])
    def msl(s):
        return modT[:, s * KD:(s + 1) * KD]
    def dsl(nm, n=KD):
        return der[:, DV[nm]:DV[nm] + n]
    stt(dsl("gs_m"), msl(1), 1.0, vec[:, c.V_GMIX:c.V_GMIX + KD], ALU.add, ALU.mult, ["modT", "vec"], ["der"])
    stt(dsl("gs_f"), msl(4), 1.0, vec[:, c.V_GFFN:c.V_GFFN + KD], ALU.add, ALU.mult, ["modT", "vec"], ["der"])
    stt(dsl("gs_o"), msl(7), 1.0, vec[:, c.V_GOUT:c.V_GOUT + KD], ALU.add, ALU.mult, ["modT", "vec"], ["der"])
    for nm, s in (("sh_m", 0), ("gt_m", 2), ("sh_f", 3), ("gt_f", 5), ("sh_o", 6)):
        cp("dve", dsl(nm), msl(s), ["modT"], ["der"])
    tt(dsl("sp", NH), vec[:, c.V_LB0:c.V_LB0 + NH], vec[:, c.V_LB1:c.V_LB1 + NH], ALU.subtract, ["vec", "der"], ["der"])
    act(dsl("lb", NH), dsl("sp", NH), AF.Sigmoid, ["der"], ["der"])
    ts(dsl("oml", NH), dsl("lb", NH), -1.0, 1.0, ALU.mult, ALU.add, ["der"], ["der"])
    ts(dsl("noml", NH), dsl("oml", NH), -1.0, None, ALU.mult, None, ["der"], ["der"])

    chk(1)
    ghflat = GH[:].rearrange("p a b c d -> p (a b c d)")
    assert NT * PH * 4 * 128 >= 2 * D or True
    ncb = max(1, min(2, (NT * PH * 4 * 128) // D))
    for i in range(c.NTILES):
        dma("sp", stg, wall_d[i], [], stg_keys, "d_stg")
        s = i % ncb
        cb = ghflat[:, s * D:(s + 1) * D]
        eng = ("dve", "act", "pool")[i % 3]
        cp(eng, cb, stg, stg_keys, [("cb", s)])
        dma("pool", wbt(i), cb, [("cb", s)], [("wb", i)], "d_cbst%d" % s)
    chk(2)
    T.barrier()

    def group_order():
        od = []
        for hh in range(NH):
            od += [("t", c.T_WIN + hh), ("t", c.T_WIN + NH + hh), ("t", c.T_WIN + 2 * NH + hh), ("t", c.T_WIN + 3 * NH + hh)]
        for ct in range(NCT):
            od += [("t", c.T_WIN + 4 * NH + ct), ("t", c.T_WIN + 4 * NH + NCT + ct)]
        od += [("t", c.T_WOUT + i) for i in range(KD)]
        od += [("t", c.T_WPQ + i) for i in range(c.NQT)]
        for sbi in range(c.NSB):
            od += [("t", c.T_UT + sbi * c.SBC + i) for i in range(c.SBC)]
            for dq in range(c.NDQ):
                od += [("v", sbi, dq, cg) for cg in range(c.SBC // c.VU)]
        return od

    order = group_order() * c.NG
    ws = {"issued": 0, "pos": 0}

    def ws_issue(j):
        it = order[j]
        slot = j % NB
        if it[0] == "t":
            dma("sp", wring[:, slot, :], wbt(it[1]), [], [("w", slot)], "d_w%d" % slot)
        else:
            _, sbi, dq, cg = it
            c0 = sbi * c.SBC + cg * c.VU
            W = c.DQW * 128
            src = wbs[3][c0:c0 + c.VU, :, dq * W:(dq + 1) * W].rearrange("c p w -> p c w")
            dst = wring[:, slot, 0:c.VU * W].rearrange("p (c w) -> p c w", c=c.VU)
            dma("sp", dst, src, [], [("w", slot)], "d_w%d" % slot)

    def ws_next(expect):
        j = ws["pos"]
        assert order[j] == expect, (order[j], expect)
        while ws["issued"] < min(len(order), j + NB):
            ws_issue(ws["issued"]); ws["issued"] += 1
        ws["pos"] += 1
        slot = j % NB
        return wring[:, slot, :], [("w", slot)]

    inv_sqrt_eps = None

    def rms_rstd(eps):
        pn, pnk = PSH(7, 0)
        for k in range(KD):
            sq, sqk = TM(k % 2)
            act(sq, xT[:, k, :], AF.Square, [("xT", k)], sqk)
            mm(pn, onesf[:], sq, k == 0, k == KD - 1, ["onesf"] + sqk, pnk, signal=(k % 2 == 1 or k == KD - 1))
        sd, sdk = TM(2)
        act(sd, pn, AF.Sqrt, pnk, sdk, bias=eps_ap, scale=1.0 / D)
        rs, rsk = TM(3)
        T.emit("dve", lambda e: e.reciprocal(out=rs, in_=sd), sdk, rsk)
        return rs, rsk

    def make_hT(gs, sh, rs, rsk):
        for k in range(KD):
            t, tkk = TM(4 + k % 2)
            tt(t, xT[:, k, :], rs, ALU.mult, [("xT", k)] + rsk, tkk)
            act(hT[:, k, :], t, AF.Identity, tkk + ["der"], [("hT", k)], bias=dv(sh, k), scale=dv(gs, k))

    def proj(wt, wtk, rhs_tile, rhs_key, out, outk):
        for k in range(KD):
            mm(out, wt[:, k * 128:(k + 1) * 128], rhs_tile[:, k, :], k == 0, k == KD - 1,
               wtk + [(rhs_key, k)], outk, signal=(k == KD - 1))

    eps_t = sb("eps_t", [128, 2], F32)
    T.emit("pool", lambda e: e.memset(eps_t[:, 0:1], 1e-6), [], ["eps"])
    T.emit("pool", lambda e: e.memset(eps_t[:, 1:2], 1e-5), [], ["eps"])
    eps_ap = eps_t[:, 0:1]
    lneps_ap = eps_t[:, 1:2]

    proj_slots = [(0, 0), (1, 0), (5, 0), (6, 0)]
    pslot = {"i": 0}

    def next_pslot():
        b, h = proj_slots[pslot["i"] % len(proj_slots)]
        pslot["i"] += 1
        return PSH(b, h)

    for g in range(c.NG):
        xk = [("xT", k) for k in range(KD)]
        dma("pool", xT[:], xT_d[g], [], xk, "d_x")
        rs, rsk = rms_rstd(1e-6)
        make_hT("gs_m", "sh_m", rs, rsk)
        chk(3)
        for hh in range(NH):
            pq, pqk = next_pslot(); pf, pfk = next_pslot(); pi, pik = next_pslot(); pg, pgk = next_pslot()
            for (po, pok, tix) in ((pq, pqk, hh), (pf, pfk, NH + hh), (pi, pik, 2 * NH + hh), (pg, pgk, 3 * NH + hh)):
                wt, wtk = ws_next(("t", c.T_WIN + tix))
                proj(wt, wtk, hT, "hT", po, pok)
            q, qk = TM(6); sg, sgk = TM(7); sgg, sggk = TM(8)
            act(q, pq, AF.Silu, pqk, qk)
            act(sgg, pg, AF.Silu, pgk, sggk)
            act(sg, pf, AF.Sigmoid, pfk, sgk)
            vT, vTk = TB(0)
            cp("act", vT, pi, pik, vTk)
            chk(3.1)
            f, fk = TM(9); kk, kkk = TM(10)
            ts(f, sg, dv("oml", hh), dv("lb", hh), ALU.mult, ALU.add, sgk + ["der"], fk)
            ts(kk, sg, dv("noml", hh), dv("oml", hh), ALU.mult, ALU.add, sgk + ["der"], kkk)
            lf, lfk = TM(11)
            act(lf, f, AF.Ln, fk, lfk)
            b, bk = TM(12)
            T.emit("dve", lambda e, b=b, lf=lf: e.tensor_tensor_scan(out=b, data0=smask[:], data1=lf, initial=0.0,
                                                                      op0=ALU.mult, op1=ALU.add), lfk + ["smask"], bk)
            chk(3.2)
            eb, ebk = TM(13); enb, enbk = TM(14)
            act(eb, b, AF.Exp, bk, ebk)
            act(enb, b, AF.Exp, bk, enbk, scale=-1.0)
            dc = dch[:, hh % 2, :]; dck = [("dch", hh % 2)]
            blast = b.rearrange("p (c s) -> p c s", s=64)[:, :, 63]
            act(dc, blast, AF.Exp, bk, dck)
            Qt, Qtk = TB(1); Ktb, Ktbk = TB(2); Kh, Khk = TB(3)
            tt(Qt, q, eb, ALU.mult, qk + ebk, Qtk)
            K32, K32k = TM(15)
            tt(K32, kk, enb, ALU.mult, kkk + enbk, K32k)
            cp("act", Ktb, K32, K32k, Ktbk)
            tt(Kh.rearrange("p (c s) -> p c s", s=64), K32.rearrange("p (c s) -> p c s", s=64),
               dc.unsqueeze(2).broadcast_to([128, NCH, 64]), ALU.mult, K32k + dck, Khk)
            chk(3.3)
            psc, psck = PSH(2, 0)
            for tb_ in range(NT):
                mm(psc[:, tb_ * 128:(tb_ + 1) * 128], Ktb[:, tb_ * 128:(tb_ + 1) * 128], Qt[:, tb_ * 128:(tb_ + 1) * 128],
                   True, True, Ktbk + Qtk, psck, signal=(tb_ == NT - 1))
            scm, scmk = TB(4)
            tt(scm.rearrange("p (a t) -> p a t", a=NT), psc.rearrange("p (a t) -> p a t", a=NT),
               cmask[:].unsqueeze(1).broadcast_to([128, NT, 128]), ALU.mult, psck + ["cmask"], scmk)
            chk(3.4)
            pv, pvk = PSH(3, 0); pk, pkk = PSH(4, 0)
            for tb_ in range(NT):
                mm(pv[:, tb_ * 128:(tb_ + 1) * 128], vT[:, tb_ * 128:(tb_ + 1) * 128], ident[:], True, True,
                   vTk + ["ident"], pvk, signal=(tb_ == NT - 1))
            chk(3.41)
            for tb_ in range(NT):
                mm(pk[:, tb_ * 128:(tb_ + 1) * 128], Kh[:, tb_ * 128:(tb_ + 1) * 128], ident[:], True, True,
                   Khk + ["ident"], pkk, signal=(tb_ == NT - 1))
            chk(3.42)
            Vt, Vtk = TB(5); Kt, Ktk = TB(6)
            cp("act", Vt, pv, pvk, Vtk)
            chk(3.43)
            ts(Kt, pk, 1.0, None, ALU.mult, None, pkk, Ktk)
            chk(3.5)
            po_, pok_ = PSH(7, 0)
            for tb_ in range(NT):
                mm(po_[:, tb_ * 128:(tb_ + 1) * 128], Vt[:, tb_ * 128:(tb_ + 1) * 128], scm[:, tb_ * 128:(tb_ + 1) * 128],
                   tb_ == 0, False, Vtk + scmk, pok_, signal=False, sgc=True)
            pss, pssk = PSH(2, 0)
            skey = [("S", hh)]
            for ch in range(NCH):
                tb_ = ch // 2; p0 = (ch % 2) * 64
                mm(po_[:, ch * 64:(ch + 1) * 64], Sbf[:, hh, :], Qt[:, ch * 64:(ch + 1) * 64], False, True,
                   [("Sb", hh)] + Qtk, pok_, signal=True, sgc=True)
                mm(pss[:, 0:128], Kt[p0:p0 + 64, tb_ * 128:(tb_ + 1) * 128], Vt[p0:p0 + 64, tb_ * 128:(tb_ + 1) * 128],
                   True, True, Ktk + Vtk, pssk)
                stt(S32[:, hh, :], S32[:, hh, :], dc[:, ch:ch + 1], pss[:, 0:128], ALU.mult, ALU.add,
                    skey + dck + pssk, skey)
                cp("act", Sbf[:, hh, :], S32[:, hh, :], skey, [("Sb", hh)])
            chk(3.6)
            osq, osqk = TM(16)
            act(osq, po_, AF.Square, pok_, osqk)
            pn, pnk = PSH(3, 0)
            mm(pn, onesf[:], osq, True, True, ["onesf"] + osqk, pnk)
            sd, sdk = TM(17)
            act(sd, pn, AF.Sqrt, pnk, sdk, bias=eps_ap, scale=1.0 / 128)
            ri, rik = TM(16)
            T.emit("dve", lambda e, ri=ri, sd=sd: e.reciprocal(out=ri, in_=sd), sdk, rik)
            t1, t1k = TM(17)
            tt(t1, po_, ri, ALU.mult, pok_ + rik, t1k)
            stt(ycat[:, hh, :], t1, vc(c.V_HGN, hh), sgg, ALU.mult, ALU.mult, t1k + sggk + ["vec"], [("yc", hh)])
        chk(4)
        pl1, pl1k = PSH(2, 0); pl2, pl2k = PSH(3, 0)
        for ct in range(NCT):
            pa, pak = next_pslot(); pb, pbk = next_pslot()
            wt, wtk = ws_next(("t", c.T_WIN + 4 * NH + ct)); proj(wt, wtk, hT, "hT", pa, pak)
            wt, wtk = ws_next(("t", c.T_WIN + 4 * NH + NCT + ct)); proj(wt, wtk, hT, "hT", pb, pbk)
            sgb, sgbk = TM(6 + ct % 2)
            act(sgb, pb, AF.Sigmoid, pbk + ["vec"], sgbk, bias=vc(c.V_BGB, ct))
            ub = ubuf[:, ct % 2, :]; ubk = [("ub", ct % 2)]
            cp("pool", ub[:, 0:30], tails[:, ct, :], [("tl", ct)], ubk)
            stt(ub[:, 30:30 + G], pa, vc(c.V_BGA, ct), sgb, ALU.add, ALU.mult, pak + sgbk + ["vec"], ubk)
            cp("pool", tails[:, ct, :], ub[:, G:G + 30], ubk, [("tl", ct)])
            acc, acck = TM(8 + ct % 2)
            ts(acc, ub[:, 0:G], vc(c.V_WDW, ct * 31), vc(c.V_BDW, ct), ALU.mult, ALU.add, ubk + ["vec"], acck)
            for j in range(1, 31):
                stt(acc, ub[:, j:j + G], vc(c.V_WDW, ct * 31 + j), acc, ALU.mult, ALU.add, ubk + acck + ["vec"], acck)
            sq, sqk = TM(10 + ct % 2)
            act(sq, acc, AF.Square, acck, sqk)
            mm(pl1, onesf[:], acc, ct == 0, ct == NCT - 1, ["onesf"] + acck, pl1k)
            mm(pl2, onesf[:], sq, ct == 0, ct == NCT - 1, ["onesf"] + sqk, pl2k)
            cp("act", ycat[:, NH + ct, :], acc, acck, [("yc", NH + ct)])
        mean, meank = TM(12); msq, msqk = TM(13); var, vark = TM(14); lsd, lsdk = TM(15); lrs, lrsk = TM(16)
        act(mean, pl1, AF.Identity, pl1k, meank, scale=1.0 / c.CW)
        tt(msq, mean, mean, ALU.mult, meank, msqk)
        stt(var, pl2, 1.0 / c.CW, msq, ALU.mult, ALU.subtract, pl2k + msqk, vark)
        act(lsd, var, AF.Sqrt, vark, lsdk, bias=lneps_ap, scale=1.0)
        T.emit("dve", lambda e, lrs=lrs, lsd=lsd: e.reciprocal(out=lrs, in_=lsd), lsdk, lrsk)
        for ct in range(NCT):
            t, tk_ = TM(6 + ct % 2); t2, t2k = TM(8 + ct % 2)
            tt(t, ycat[:, NH + ct, :], mean, ALU.subtract, [("yc", NH + ct)] + meank, tk_)
            tt(t2, t, lrs, ALU.mult, tk_ + lrsk, t2k)
            act(ycat[:, NH + ct, :], t2, AF.Silu, t2k + ["vec"], [("yc", NH + ct)], bias=vc(c.V_LNB, ct), scale=vc(c.V_LNG, ct))
        chk(5)
        for dt in range(KD):
            wt, wtk = ws_next(("t", c.T_WOUT + dt))
            po2, po2k = next_pslot()
            proj(wt, wtk, ycat, "yc", po2, po2k)
            stt(xT[:, dt, :], po2, dv("gt_m", dt), xT[:, dt, :], ALU.mult, ALU.add, po2k + ["der", ("xT", dt)], [("xT", dt)])
        chk(6)
        rs, rsk = rms_rstd(1e-6)
        make_hT("gs_f", "sh_f", rs, rsk)
        qTt = PT
        for jt in range(c.NQT):
            wt, wtk = ws_next(("t", c.T_WPQ + jt))
            pq2, pq2k = next_pslot()
            proj(wt, wtk, hT, "hT", pq2, pq2k)
            cp("act", qTt[:, jt, :], pq2, pq2k, [("PT", jt)])
        chk(7)
        for tt_ in range(NT):
            for h in range(PH):
                b_, hf = [(0, 0), (1, 0), (2, 0), (3, 0)][h % 4]
                pS, pSk = PSH(b_, hf)
                for half in range(2):
                    mm(pS[:, half * 128:(half + 1) * 128], qTt[:, 2 * h + half, tt_ * 128:(tt_ + 1) * 128], keysb[:, 2 * h + half, :],
                       True, True, [("PT", 2 * h + half), "keysb"], pSk, signal=(half == 1))
                skey2 = [("s12", tt_, h)]
                cp("act", s12[:, tt_, h, :, :].rearrange("p a n -> p (a n)"), pS, pSk, skey2)
                for half in range(2):
                    sv = s12[:, tt_, h, half, :]
                    v16 = tk[:, half, :]
                    T.emit("dve", lambda e, sv=sv, v16=v16: e.max(out=v16[:, 0:8], in_=sv), skey2, [("tk", half)])
                    wv = tkw[:, 0, 0:128]
                    T.emit("dve", lambda e, sv=sv, v16=v16, wv=wv: e.match_replace(out=wv, in_to_replace=v16[:, 0:8], in_values=sv, imm_value=NEG),
                           skey2 + [("tk", half)], [("tkw", 0)])
                    T.emit("dve", lambda e, v16=v16, wv=wv: e.max(out=v16[:, 8:16], in_=wv), [("tkw", 0)], [("tk", half)])
                cand = tkw[:, 1, :]
                tt(cand.rearrange("p (a b) -> p a b", a=16), tk[:, 0, :].unsqueeze(2).broadcast_to([128, 16, 16]),
                   tk[:, 1, :].unsqueeze(1).broadcast_to([128, 16, 16]), ALU.add, [("tk", 0), ("tk", 1)], [("tkw", 1)])
                tops = tk[:, 2, :]
                T.emit("dve", lambda e, tops=tops, cand=cand: e.max(out=tops[:, 0:8], in_=cand), [("tkw", 1)], [("tk", 2)])
                cw = tkw[:, 0, :]
                T.emit("dve", lambda e, tops=tops, cand=cand, cw=cw: e.match_replace(out=cw, in_to_replace=tops[:, 0:8], in_values=cand, imm_value=NEG),
                       [("tkw", 1), ("tk", 2)], [("tkw", 0)])
                T.emit("dve", lambda e, tops=tops, cw=cw: e.max(out=tops[:, 8:16], in_=cw), [("tkw", 0)], [("tk", 2)])
                cp("dve", tau[:, tt_, h:h + 1], tops[:, 15:16], [("tk", 2)], [("tau", tt_, h)])
                negm = tk[:, 3, 0:1]; Z = tk[:, 3, 1:2]; lnZ = tk[:, 3, 2:3]; exs = tk[:, 4, :]
                ts(negm, tops[:, 0:1], -1.0, None, ALU.mult, None, [("tk", 2)], [("tk", 3)])
                act(exs, tops, AF.Exp, [("tk", 2), ("tk", 3)], [("tk", 4), ("tk", 3)], bias=negm, accum=Z)
                act(lnZ, Z, AF.Ln, [("tk", 3)], [("tk", 3)])
                tt(nbias[:, tt_, h:h + 1], negm, lnZ, ALU.subtract, [("tk", 3)], [("nb", tt_, h)])
        chk(8)
        acc_slots = [(0, 0), (1, 0), (2, 0), (3, 0)]
        for sbi in range(c.NSB):
            for blk in range(c.SBC // 4):
                c0 = sbi * c.SBC + blk * 4
                for tt_ in range(NT):
                    for h in range(PH):
                        i_ = (tt_ * PH + h) % 2
                        val = tmp[:, 2 * i_:2 * i_ + 2, :].rearrange("p a b -> p (a b)")
                        valk = [("tmp", 2 * i_), ("tmp", 2 * i_ + 1)]
                        Wv = tmp[:, 4 + 2 * i_:6 + 2 * i_, :].rearrange("p a b -> p (a b)")
                        Wk = [("tmp", 4 + 2 * i_), ("tmp", 5 + 2 * i_)]
                        tt(val.rearrange("p (a n) -> p a n", a=4),
                           s12[:, tt_, h, 0, c0:c0 + 4].unsqueeze(2).broadcast_to([128, 4, 128]),
                           s12[:, tt_, h, 1, :].unsqueeze(1).broadcast_to([128, 4, 128]), ALU.add,
                           [("s12", tt_, h)], valk)
                        act(Wv, val, AF.Exp, valk + [("nb", tt_, h)], Wk, bias=nbias[:, tt_, h:h + 1])
                        stt(GH[:, tt_, h, :, :].rearrange("p a n -> p (a n)"), val, tau[:, tt_, h:h + 1], Wv, ALU.is_ge, ALU.mult,
                            valk + Wk + [("tau", tt_, h)], [("GH", tt_, h)])
                for ci in range(4):
                    ch_ = c0 + ci
                    cc = blk * 4 + ci
                    ut, utk = ws_next(("t", c.T_UT + ch_))
                    pA, pAk = PSH(4 + 2 * (cc % 2), 0)
                    proj(ut, utk, hT, "hT", pA, pAk)
                    pG, pGk = PSH(5 + 2 * (cc % 2), 0)
                    for tt_ in range(NT):
                        for h in range(PH):
                            mm(pG[:, tt_ * 128:(tt_ + 1) * 128], GH[:, tt_, h, ci, :], ident[:], h == 0, h == PH - 1,
                               [("GH", tt_, h), "ident"], pGk, signal=(h == PH - 1 and tt_ == NT - 1))
                    ge, gek = TM(8 + cc % 2)
                    act(ge, pA, AF.Gelu, pAk, gek)
                    tt(PT[:, cc, :], ge, pG, ALU.mult, gek + pGk, [("PT", cc)])
            for dq in range(c.NDQ):
                nb_ = c.DQW
                for bnk in range(nb_):
                    pz, pzk = PSB(bnk)
                    mm(pz, zer[:, 0:128], zer[:, 0:512], True, False, ["zer"], pzk, sgc=True)
                for cg in range(c.SBC // c.VU):
                    vb, vbk = ws_next(("v", sbi, dq, cg))
                    W = c.DQW * 128
                    for ci in range(c.VU):
                        cc = cg * c.VU + ci
                        for dtl in range(c.DQW):
                            pa2, pa2k = PSH(*acc_slots[dtl])
                            mm(pa2, vb[:, ci * W + dtl * 128:ci * W + (dtl + 1) * 128], PT[:, cc, :], False, True,
                               vbk + [("PT", cc)], pa2k, signal=(cc == c.SBC - 1 or (ci == c.VU - 1 and dtl == c.DQW - 1)), sgc=True)
                for dtl in range(c.DQW):
                    dt = dq * c.DQW + dtl
                    pa2, pa2k = PSH(*acc_slots[dtl])
                    stt(xT[:, dt, :], pa2, dv("gt_f", dt), xT[:, dt, :], ALU.mult, ALU.add,
                        pa2k + ["der", ("xT", dt)], [("xT", dt)])
        chk(9)
        rs, rsk = rms_rstd(1e-6)
        for k4 in range(0, KD, 2):
            ob = obuf[:, (k4 // 2) % 2, :, :]; obk = [("ob", (k4 // 2) % 2)]
            for k in range(k4, min(KD, k4 + 2)):
                t, tkk = TM(4 + k % 2)
                tt(t, xT[:, k, :], rs, ALU.mult, [("xT", k)] + rsk, tkk)
                act(ob[:, k - k4, :], t, AF.Identity, tkk + ["der"], obk, bias=dv("sh_o", k), scale=dv("gs_o", k))
            n = min(KD, k4 + 2) - k4
            dma("pool", out_d[g, :, k4:k4 + n, :], ob[:, 0:n, :], obk, [("out", g, k4)], "d_o%d" % ((k4 // 2) % 2))
    T.dead = False
    T.final_wait("sp")
    T.replay(nc, es)
    es.close()
    return nc


def host_prepare(cfg, x, c, w_ada, b_ada, w_ada_out, b_ada_out, g_mix, g_ffn, g_out, w_in, b_glu,
                 lb_logits, w_dw, b_dw, ln_g, ln_b, hg_norm_g, w_out, w_pq, sub_keys1, sub_keys2,
                 expert_u, expert_v):
    cf = cfg
    D, KD, G = cf.D, cf.KD, cf.G
    f = lambda a: np.asarray(a, dtype=np.float32)
    x = f(x)[0]
    xT = np.ascontiguousarray(x.reshape(cf.NG, G, KD, 128).transpose(0, 3, 2, 1))

    def coltiles(w):
        K, N = w.shape
        return w.reshape(K // 128, 128, N // 128, 128).transpose(2, 1, 0, 3).reshape(N // 128, 128, K)

    wall = np.empty((cf.NTILES, 128, D), np.float32)
    wall[cf.T_WIN:cf.T_WIN + cf.NCOL] = coltiles(f(w_in)[0])
    wall[cf.T_WOUT:cf.T_WOUT + KD] = coltiles(f(w_out)[0])
    wall[cf.T_WPQ:cf.T_WPQ + cf.NQT] = coltiles(f(w_pq)[0])
    u = f(expert_u)[0]
    wall[cf.T_UT:cf.T_UT + cf.NEC] = u.reshape(cf.NEC, 128, KD, 128).transpose(0, 3, 2, 1).reshape(cf.NEC, 128, D)
    wall[cf.T_V:cf.T_V + cf.NEC] = f(expert_v)[0].reshape(cf.NEC, 128, D)
    wcat = np.concatenate([f(w_ada)[0], f(w_ada_out)], axis=1)
    wada = np.ascontiguousarray(wcat.reshape(KD, 128, 8 * D))
    vec = np.zeros((128, cf.NV), np.float32)

    def put(off, v, n):
        vec[:, off:off + n] = v.reshape(n, 128).T

    put(cf.V_C, f(c)[0], KD)
    put(cf.V_BADA, np.concatenate([f(b_ada)[0], f(b_ada_out)]), 8 * KD)
    put(cf.V_GMIX, f(g_mix)[0], KD); put(cf.V_GFFN, f(g_ffn)[0], KD); put(cf.V_GOUT, f(g_out), KD)
    bg = f(b_glu)[0]
    put(cf.V_BGA, bg[:cf.CW], cf.NCT); put(cf.V_BGB, bg[cf.CW:], cf.NCT)
    lbl = f(lb_logits)
    put(cf.V_LB0, lbl[0], cf.NH); put(cf.V_LB1, lbl[1], cf.NH)
    wd = f(w_dw)[0]
    vec[:, cf.V_WDW:cf.V_WDW + cf.NCT * 31] = wd.reshape(31, cf.NCT, 128).transpose(2, 1, 0).reshape(128, cf.NCT * 31)
    put(cf.V_BDW, f(b_dw)[0], cf.NCT); put(cf.V_LNG, f(ln_g)[0], cf.NCT); put(cf.V_LNB, f(ln_b)[0], cf.NCT)
    put(cf.V_HGN, f(hg_norm_g)[0], cf.NH)
    k1 = f(sub_keys1)[0]; k2 = f(sub_keys2)[0]
    ks = np.stack([k1, k2], axis=1).reshape(2 * cf.PH, 128, 128)
    keysT = np.ascontiguousarray(ks.transpose(2, 0, 1))
    return {"xT": xT, "wall": wall, "wada": wada, "vec": vec, "keysT": keysT}


def run(cfg, inputs):
    nc = build(cfg)
    im = host_prepare(cfg, **inputs)
    res = run_bass_kernel_spmd(nc, [im], core_ids=[0])
    oT = res.results[0]["outT"]
    out = oT.transpose(0, 3, 2, 1).reshape(1, cfg.SEQ, cfg.D)
    return np.ascontiguousarray(out.astype(np.float32))


def kernel(**inputs):
    return run(Cfg(), inputs)
```

```python
import numpy as np
from contextlib import ExitStack
import concourse.bass as bass
import concourse.mybir as mybir
from concourse.bass_utils import run_bass_kernel_spmd

F32 = mybir.dt.float32
BF16 = mybir.dt.bfloat16
AF = mybir.ActivationFunctionType
ALU = mybir.AluOpType
NEG = -1.0e30


class Cfg:
    def __init__(self, D=4096, SEQ=16384, PH=8, G=256, NCORES=4):
        self.D = D; self.SEQ = SEQ; self.PH = PH; self.G = G; self.NCORES = NCORES
        self.KD = D // 128
        self.NH = (D // 2) // 128
        self.NCT = (D // 2) // 128
        self.CW = D // 2
        self.NCOL = 3 * D // 128
        self.NQT = 2 * PH
        self.NEC = 128
        self.SBC = 32
        self.NSB = self.NEC // self.SBC
        self.NT = G // 128
        self.NCH = G // 64
        self.NG = SEQ // G
        self.NGL = self.NG // NCORES
        self.HALO = 1 if NCORES > 1 else 0
        self.NGP = self.NGL + self.HALO
        self.DQW = min(4, self.KD)
        self.NDQ = self.KD // self.DQW
        self.VU = D // (self.DQW * 128)
        self.T_WIN = 0
        self.T_WOUT = self.T_WIN + self.NCOL
        self.T_WPQ = self.T_WOUT + self.KD
        self.T_UT = self.T_WPQ + self.NQT
        self.T_V = self.T_UT + self.NEC
        self.NTILES = self.T_V + self.NEC
        o = 0
        def take(n):
            nonlocal o
            r = o; o += n; return r
        KD, NH, NCT = self.KD, self.NH, self.NCT
        self.V_C = take(KD); self.V_BADA = take(8 * KD)
        self.V_GMIX = take(KD); self.V_GFFN = take(KD); self.V_GOUT = take(KD)
        self.V_BGA = take(NCT); self.V_BGB = take(NCT)
        self.V_LB0 = take(NH); self.V_LB1 = take(NH)
        self.V_WDW = take(NCT * 31); self.V_BDW = take(NCT)
        self.V_LNG = take(NCT); self.V_LNB = take(NCT); self.V_HGN = take(NH); self.V_HM = take(1)
        self.NV = o


class Tracker:
    EPOCH = 60000

    def __init__(self):
        self.streams = {n: [] for n in ("pe", "act", "dve", "pool", "sp")}
        self.cnt = {n: 0 for n in self.streams}
        self.epoch = {n: 0 for n in self.streams}
        self.waited = {n: {} for n in self.streams}
        self.keys = {}
        self.dcnt = {}
        self.semids = set()
        self.last_ev = {}
        self.pending = {n: False for n in self.streams}

    def _need(self, eng, ev, needs):
        if ev is None:
            return
        sid, val = ev
        if self.waited[eng].get(sid, 0) >= val:
            return
        if needs.get(sid, 0) < val:
            needs[sid] = val

    dead = False

    def emit(self, eng, fn, reads=(), writes=(), signal=True, dsem=None):
        if self.dead:
            return None
        needs = {}
        if dsem is None and self.cnt[eng] >= 55000 and not self.pending[eng]:
            self.epoch[eng] += 1; self.cnt[eng] = 0
        my_sid = (eng, self.epoch[eng])
        excl = my_sid if eng == "pe" else None
        for k in reads:
            st = self.keys.get(k)
            if st is not None:
                self._need(eng, st[0], needs)
        for k in writes:
            st = self.keys.get(k)
            if st is not None:
                lw = st[0]
                if lw is not None and lw[0] != excl:
                    self._need(eng, lw, needs)
                for sid, val in st[1].items():
                    if sid != excl:
                        self._need(eng, (sid, val), needs)
        waits = []
        for sid, val in needs.items():
            if sid[0] == eng and sid[1] == self.epoch[eng]:
                assert val <= self.cnt[eng], ("wait on future own milestone", eng, val, self.cnt[eng])
            elif sid[0] in self.cnt and sid[1] == self.epoch[sid[0]]:
                assert val <= self.cnt[sid[0]], ("wait on future milestone", eng, sid, val)
            waits.append((sid, val))
            self.waited[eng][sid] = val
        if dsem is not None:
            ep, c = self.dcnt.get(dsem, (0, 0))
            if c + 16 > self.EPOCH:
                ep += 1; c = 0
            c += 16
            self.dcnt[dsem] = (ep, c)
            sid = ("dma", dsem, ep)
            ev = (sid, c)
            inc = (sid, 16)
        elif signal:
            assert self.cnt[eng] + 1 <= self.EPOCH
            self.pending[eng] = False
            self.cnt[eng] += 1
            ev = (my_sid, self.cnt[eng])
            inc = (my_sid, 1)
        else:
            ev = (my_sid, self.cnt[eng] + 1)
            self.pending[eng] = True
            assert self.cnt[eng] + 1 <= self.EPOCH
            inc = None
        self.semids.add(ev[0])
        self.last_ev[ev[0]] = max(self.last_ev.get(ev[0], 0), ev[1])
        for k in reads:
            if isinstance(k, tuple) and k[0] == "psbw":
                continue
            st = self.keys.setdefault(k, [None, {}])
            st[1][ev[0]] = max(st[1].get(ev[0], 0), ev[1])
        for k in writes:
            self.keys[k] = [ev, {}]
        self.streams[eng].append((waits, fn, inc))
        return ev

    def barrier(self):
        evs = dict(self.last_ev)
        for eng in self.streams:
            waits = []
            for sid, val in evs.items():
                if sid[0] == eng:
                    continue
                if self.waited[eng].get(sid, 0) < val:
                    waits.append((sid, val)); self.waited[eng][sid] = val
            if waits:
                self.streams[eng].append((waits, None, None))
        self.keys = {}

    def final_wait(self, eng):
        waits = []
        for sid, val in self.last_ev.items():
            if sid[0] == eng:
                continue
            if self.waited[eng].get(sid, 0) < val:
                waits.append((sid, val)); self.waited[eng][sid] = val
        self.streams[eng].append((waits, None, None))

    def replay(self, nc, es):
        sems = {}
        for i, sid in enumerate(sorted(self.semids, key=str)):
            sems[sid] = es.enter_context(nc.semaphore("s%d" % i))
        blk = es.enter_context(nc.Block())
        T = self

        def run(name, e):
            for waits, fn, inc in T.streams[name]:
                for sid, val in waits:
                    e.wait_ge(sems[sid], val)
                if fn is not None:
                    ins = fn(e)
                    if inc is not None:
                        ins.then_inc(sems[inc[0]], inc[1])

        @blk.sync
        def _(e): run("sp", e)

        @blk.gpsimd
        def _(e): run("pool", e)

        @blk.scalar
        def _(e): run("act", e)

        @blk.vector
        def _(e): run("dve", e)

        @blk.tensor
        def _(e): run("pe", e)


def build(cfg):
    c = cfg
    D, KD, NH, NCT, G, NT, NCH, PH = c.D, c.KD, c.NH, c.NCT, c.G, c.NT, c.NCH, c.PH
    nc = bass.Bass("TRN2", target_bir_lowering=False)
    xT_d = nc.dram_tensor("xT", [c.NGP, 128, KD, G], F32, kind="ExternalInput").ap()
    wall_d = nc.dram_tensor("wall", [c.NTILES, 128, D], F32, kind="ExternalInput").ap()
    wada_d = nc.dram_tensor("wada", [KD, 128, 8 * D], F32, kind="ExternalInput").ap()
    vec_d = nc.dram_tensor("vec", [128, c.NV], F32, kind="ExternalInput").ap()
    keys_d = nc.dram_tensor("keysT", [128, 2 * PH, 128], F32, kind="ExternalInput").ap()
    out_d = nc.dram_tensor("outT", [c.NGL, 128, KD, G], F32, kind="ExternalOutput").ap()
    segs = [(c.T_WIN, c.T_WOUT), (c.T_WOUT, c.T_UT), (c.T_UT, c.T_V), (c.T_V, c.NTILES)]
    wbs = [nc.dram_tensor("wb%d" % j, [b_ - a_, 128, D], BF16, kind="Internal").ap() for j, (a_, b_) in enumerate(segs)]

    def wbt(i):
        for j, (a_, b_) in enumerate(segs):
            if a_ <= i < b_:
                return wbs[j][i - a_]
        raise IndexError(i)

    T = Tracker()
    es = ExitStack()

    def sb(name, shape, dt):
        return es.enter_context(nc.sbuf_tensor(name, shape, dt))

    xT = sb("xT_sb", [128, KD, G], F32)
    hT = sb("hT_sb", [128, KD, G], BF16)
    ycat = sb("ycat", [128, KD, G], BF16)
    PT = sb("PT", [128, c.SBC, G], BF16)
    NB = 3
    wring = sb("wring", [128, NB, D], BF16)
    GH = sb("GH", [128, NT, PH, 4, 128], BF16)
    NTMP = 18
    tmp = sb("tmp", [128, NTMP, G], F32)
    tb = sb("tb16", [128, 12, G], BF16)
    s12 = sb("s12", [128, NT, PH, 2, 128], F32)
    S32 = sb("S32", [128, NH, 128], F32)
    Sbf = sb("Sbf", [128, NH, 128], BF16)
    ubuf = sb("ubuf", [128, 2, 30 + G], F32)
    tails = sb("tails", [128, NCT, 30], F32)
    vec = sb("vec_sb", [128, c.NV], F32)
    modT = sb("modT", [128, 8 * KD], F32)
    der = sb("der", [128, 8 * KD + 4 * NH], F32)
    keysb = sb("keysb", [128, 2 * PH, 128], BF16)
    ident = sb("ident", [128, 128], BF16)
    onesf = sb("onesf", [128, 128], F32)
    cmask = sb("cmask", [128, 128], F32)
    smask = sb("smask", [128, G], F32)
    zer = sb("zer", [128, 512], BF16)
    cact = sb("cact", [128, KD], F32)
    obuf = sb("obuf", [128, 2, 2, G], F32)
    tk = sb("tk", [128, 8, 16], F32)
    tkw = sb("tkw", [128, 2, 256], F32)
    tau = sb("tau", [128, NT, PH], F32)
    nbias = sb("nbias", [128, NT, PH], F32)
    dch = sb("dch", [128, 2, NCH], F32)
    psum = [es.enter_context(nc.psum_tensor("ps%d" % i, [128, 512], F32)) for i in range(8)]

    def PSH(b, h):
        return psum[b][:, h * 256:h * 256 + G], [("ps", b)]

    def PSB(b):
        return psum[b][:, :], [("ps", b)]

    DV = {}
    o = 0
    for nm, n in (("gs_m", KD), ("sh_m", KD), ("gt_m", KD), ("gs_f", KD), ("sh_f", KD), ("gt_f", KD),
                  ("gs_o", KD), ("sh_o", KD), ("lb", NH), ("oml", NH), ("noml", NH), ("sp", NH)):
        DV[nm] = o; o += n

    def dv(nm, i):
        return der[:, DV[nm] + i:DV[nm] + i + 1]

    def vc(off, i):
        return vec[:, off + i:off + i + 1]

    def act(out, in_, func, r, w, bias=None, scale=None, accum=None):
        kw = {}
        if bias is not None: kw["bias"] = bias
        if scale is not None: kw["scale"] = scale
        if accum is not None: kw["accum_out"] = accum
        return T.emit("act", lambda e: e.activation(out=out, in_=in_, func=func, **kw), r, w)

    def tt(out, in0, in1, op, r, w, eng="dve"):
        return T.emit(eng, lambda e: e.tensor_tensor(out=out, in0=in0, in1=in1, op=op), r, w)

    def ts(out, in0, s1, s2, op0, op1, r, w, eng="dve"):
        if s2 is None:
            return T.emit(eng, lambda e: e.tensor_scalar(out=out, in0=in0, scalar1=s1, scalar2=None, op0=op0), r, w)
        return T.emit(eng, lambda e: e.tensor_scalar(out=out, in0=in0, scalar1=s1, scalar2=s2, op0=op0, op1=op1), r, w)

    def stt(out, in0, s, in1, op0, op1, r, w):
        return T.emit("dve", lambda e: e.scalar_tensor_tensor(out=out, in0=in0, scalar=s, in1=in1, op0=op0, op1=op1), r, w)

    def cp(eng, out, in_, r, w):
        if eng == "act":
            return T.emit("act", lambda e: e.copy(out=out, in_=in_), r, w)
        return T.emit(eng, lambda e: e.tensor_copy(out=out, in_=in_), r, w)

    def mm(out, lhsT, rhs, start, stop, r, w, signal=True, sgc=False):
        return T.emit("pe", lambda e: e.matmul(out, lhsT=lhsT, rhs=rhs, start=start, stop=stop, skip_group_check=sgc), r, w, signal=signal)

    def dma(q, out, in_, r, w, dsem):
        return T.emit(q, lambda e: e.dma_start(out=out, in_=in_), r, w, dsem=dsem)

    def TM(i):
        return tmp[:, i, :], [("tmp", i)]

    def TB(i):
        return tb[:, i, :], [("tb", i)]

    import os
    KSTOP = float(os.environ.get("KSTOP", "99"))

    def chk(n):
        if KSTOP <= n:
            T.dead = True

    T.emit("pool", lambda e: e.memset(onesf[:], 1.0), [], ["onesf"])
    T.emit("pool", lambda e: e.affine_select(out=ident[:], in_=onesf[:], pattern=[[-1, 128]], compare_op=ALU.is_equal,
                                              fill=0.0, base=0, channel_multiplier=1), ["onesf"], ["ident"])
    T.emit("pool", lambda e: e.affine_select(out=cmask[:], in_=onesf[:], pattern=[[1, 128]], compare_op=ALU.is_ge,
                                              fill=0.0, base=0, channel_multiplier=-1), ["onesf"], ["cmask"])
    T.emit("pool", lambda e: e.memset(cmask[0:64, 64:128], 0.0), [], ["cmask"])
    T.emit("pool", lambda e: e.memset(smask[:], 1.0), [], ["smask"])
    for ch in range(NCH):
        T.emit("pool", lambda e, ch=ch: e.memset(smask[:, ch * 64:ch * 64 + 1], 0.0), [], ["smask"])
    T.emit("pool", lambda e: e.memset(zer[:], 0.0), [], ["zer"])
    T.emit("pool", lambda e: e.memset(S32[:], 0.0), [], ["S32"])
    T.emit("pool", lambda e: e.memset(Sbf[:], 0.0), [], ["Sbf"])
    T.emit("pool", lambda e: e.memset(tails[:], 0.0), [], ["tails"])
    dma("sp", vec[:], vec_d, [], ["vec"], "d_vec")
    act(cact[:], vec[:, c.V_C:c.V_C + KD], AF.Silu, ["vec"], ["cact"])

    chk(0)
    stg = tmp[:].rearrange("p a b -> p (a b)")[:, 0:D]
    stg_keys = [("tmp", i) for i in range(NTMP)]
    kst = tmp[:].rearrange("p a b -> p (a b)")[:, 0:2 * PH * 128]
    dma("sp", kst, keys_d.rearrange("p a n -> p (a n)"), [], stg_keys, "d_stg")
    cp("dve", keysb[:].rearrange("p a n -> p (a n)"), kst, stg_keys, ["keysb"])
    NCB = 8 * D // D
    mp, mpk = PSB(7)
    mm(mp, zer[:, 0:128], zer[:, 0:512], True, False, ["zer"], mpk, sgc=True)
    for k in range(KD):
        for cbk in range(NCB):
            dma("sp", stg, wada_d[k, :, cbk * D:(cbk + 1) * D], [], stg_keys, "d_stg")
            nj = D // 128
            for j in range(nj):
                col = cbk * nj + j
                mm(psum[7][:, col:col + 1], stg[:, j * 128:(j + 1) * 128], cact[:, k:k + 1], False, True,
                   stg_keys + ["cact"], mpk, signal=(j == nj - 1), sgc=True)
    tt(modT[:], psum[7][:, 0:8 * KD], vec[:, c.V_BADA:c.V_BADA + 8 * KD], ALU.add, mpk + ["vec"], ["modT"])
    def msl(s):
        return modT[:, s * KD:(s + 1) * KD]
    def dsl(nm, n=KD):
        return der[:, DV[nm]:DV[nm] + n]
    stt(dsl("gs_m"), msl(1), 1.0, vec[:, c.V_GMIX:c.V_GMIX + KD], ALU.add, ALU.mult, ["modT", "vec"], ["der"])
    stt(dsl("gs_f"), msl(4), 1.0, vec[:, c.V_GFFN:c.V_GFFN + KD], ALU.add, ALU.mult, ["modT", "vec"], ["der"])
    stt(dsl("gs_o"), msl(7), 1.0, vec[:, c.V_GOUT:c.V_GOUT + KD], ALU.add, ALU.mult, ["modT", "vec"], ["der"])
    for nm, s in (("sh_m", 0), ("gt_m", 2), ("sh_f", 3), ("gt_f", 5), ("sh_o", 6)):
        cp("dve", dsl(nm), msl(s), ["modT"], ["der"])
    tt(dsl("sp", NH), vec[:, c.V_LB0:c.V_LB0 + NH], vec[:, c.V_LB1:c.V_LB1 + NH], ALU.subtract, ["vec", "der"], ["der"])
    act(dsl("lb", NH), dsl("sp", NH), AF.Sigmoid, ["der"], ["der"])
    ts(dsl("oml", NH), dsl("lb", NH), -1.0, 1.0, ALU.mult, ALU.add, ["der"], ["der"])
    ts(dsl("noml", NH), dsl("oml", NH), -1.0, None, ALU.mult, None, ["der"], ["der"])

    chk(1)
    ghflat = GH[:].rearrange("p a b c d -> p (a b c d)")
    assert NT * PH * 4 * 128 >= 2 * D or True
    ncb = max(1, min(2, (NT * PH * 4 * 128) // D))
    for i in range(c.NTILES):
        dma("sp", stg, wall_d[i], [], stg_keys, "d_stg")
        s = i % ncb
        cb = ghflat[:, s * D:(s + 1) * D]
        eng = ("dve", "act", "pool")[i % 3]
        cp(eng, cb, stg, stg_keys, [("cb", s)])
        dma("pool", wbt(i), cb, [("cb", s)], [("wb", i)], "d_cbst%d" % s)
    chk(2)
    T.barrier()

    def group_order(halo=False):
        od = []
        for hh in range(NH):
            od += [("t", c.T_WIN + hh), ("t", c.T_WIN + NH + hh), ("t", c.T_WIN + 2 * NH + hh), ("t", c.T_WIN + 3 * NH + hh)]
        for ct in range(NCT):
            od += [("t", c.T_WIN + 4 * NH + ct), ("t", c.T_WIN + 4 * NH + NCT + ct)]
        if halo:
            return od
        od += [("t", c.T_WOUT + i) for i in range(KD)]
        od += [("t", c.T_WPQ + i) for i in range(c.NQT)]
        for sbi in range(c.NSB):
            od += [("t", c.T_UT + sbi * c.SBC + i) for i in range(c.SBC)]
            for dq in range(c.NDQ):
                od += [("v", sbi, dq, cg) for cg in range(c.SBC // c.VU)]
        return od

    order = (group_order(True) if c.HALO else []) + group_order() * c.NGL
    ws = {"issued": 0, "pos": 0}

    def ws_issue(j):
        it = order[j]
        slot = j % NB
        if it[0] == "t":
            dma("sp", wring[:, slot, :], wbt(it[1]), [], [("w", slot)], "d_w%d" % slot)
        else:
            _, sbi, dq, cg = it
            c0 = sbi * c.SBC + cg * c.VU
            W = c.DQW * 128
            src = wbs[3][c0:c0 + c.VU, :, dq * W:(dq + 1) * W].rearrange("c p w -> p c w")
            dst = wring[:, slot, 0:c.VU * W].rearrange("p (c w) -> p c w", c=c.VU)
            dma("sp", dst, src, [], [("w", slot)], "d_w%d" % slot)

    def ws_next(expect):
        j = ws["pos"]
        assert order[j] == expect, (order[j], expect)
        while ws["issued"] < min(len(order), j + NB):
            ws_issue(ws["issued"]); ws["issued"] += 1
        ws["pos"] += 1
        slot = j % NB
        return wring[:, slot, :], [("w", slot)]

    inv_sqrt_eps = None

    def rms_rstd(eps):
        pn, pnk = PSH(7, 0)
        for k in range(KD):
            sq, sqk = TM(k % 2)
            act(sq, xT[:, k, :], AF.Square, [("xT", k)], sqk)
            mm(pn, onesf[:], sq, k == 0, k == KD - 1, ["onesf"] + sqk, pnk, signal=(k % 2 == 1 or k == KD - 1))
        sd, sdk = TM(2)
        act(sd, pn, AF.Sqrt, pnk, sdk, bias=eps_ap, scale=1.0 / D)
        rs, rsk = TM(3)
        T.emit("dve", lambda e: e.reciprocal(out=rs, in_=sd), sdk, rsk)
        return rs, rsk

    def make_hT(gs, sh, rs, rsk):
        for k in range(KD):
            t, tkk = TM(4 + k % 2)
            tt(t, xT[:, k, :], rs, ALU.mult, [("xT", k)] + rsk, tkk)
            act(hT[:, k, :], t, AF.Identity, tkk + ["der"], [("hT", k)], bias=dv(sh, k), scale=dv(gs, k))

    def proj(wt, wtk, rhs_tile, rhs_key, out, outk):
        for k in range(KD):
            mm(out, wt[:, k * 128:(k + 1) * 128], rhs_tile[:, k, :], k == 0, k == KD - 1,
               wtk + [(rhs_key, k)], outk, signal=(k == KD - 1))

    eps_t = sb("eps_t", [128, 2], F32)
    T.emit("pool", lambda e: e.memset(eps_t[:, 0:1], 1e-6), [], ["eps"])
    T.emit("pool", lambda e: e.memset(eps_t[:, 1:2], 1e-5), [], ["eps"])
    eps_ap = eps_t[:, 0:1]
    lneps_ap = eps_t[:, 1:2]

    proj_slots = [(0, 0), (1, 0), (5, 0), (6, 0)]
    pslot = {"i": 0}

    def next_pslot():
        b, h = proj_slots[pslot["i"] % len(proj_slots)]
        pslot["i"] += 1
        return PSH(b, h)

    for g in range(c.NGP):
        halo = (c.HALO == 1 and g == 0)
        xk = [("xT", k) for k in range(KD)]
        dma("pool", xT[:], xT_d[g], [], xk, "d_x")
        rs, rsk = rms_rstd(1e-6)
        make_hT("gs_m", "sh_m", rs, rsk)
        chk(3)
        for hh in range(NH):
            pq, pqk = next_pslot(); pf, pfk = next_pslot(); pi, pik = next_pslot(); pg, pgk = next_pslot()
            for (po, pok, tix) in ((pq, pqk, hh), (pf, pfk, NH + hh), (pi, pik, 2 * NH + hh), (pg, pgk, 3 * NH + hh)):
                wt, wtk = ws_next(("t", c.T_WIN + tix))
                proj(wt, wtk, hT, "hT", po, pok)
            q, qk = TM(6); sg, sgk = TM(7); sgg, sggk = TM(8)
            act(q, pq, AF.Silu, pqk, qk)
            act(sgg, pg, AF.Silu, pgk, sggk)
            act(sg, pf, AF.Sigmoid, pfk, sgk)
            vT, vTk = TB(0)
            cp("act", vT, pi, pik, vTk)
            chk(3.1)
            f, fk = TM(9); kk, kkk = TM(10)
            ts(f, sg, dv("oml", hh), dv("lb", hh), ALU.mult, ALU.add, sgk + ["der"], fk)
            ts(kk, sg, dv("noml", hh), dv("oml", hh), ALU.mult, ALU.add, sgk + ["der"], kkk)
            lf, lfk = TM(11)
            act(lf, f, AF.Ln, fk, lfk)
            b, bk = TM(12)
            T.emit("dve", lambda e, b=b, lf=lf: e.tensor_tensor_scan(out=b, data0=smask[:], data1=lf, initial=0.0,
                                                                      op0=ALU.mult, op1=ALU.add), lfk + ["smask"], bk)
            chk(3.2)
            eb, ebk = TM(13); enb, enbk = TM(14)
            act(eb, b, AF.Exp, bk, ebk)
            act(enb, b, AF.Exp, bk, enbk, scale=-1.0)
            dc = dch[:, hh % 2, :]; dck = [("dch", hh % 2)]
            blast = b.rearrange("p (c s) -> p c s", s=64)[:, :, 63]
            act(dc, blast, AF.Exp, bk, dck)
            Qt, Qtk = TB(1); Ktb, Ktbk = TB(2); Kh, Khk = TB(3)
            tt(Qt, q, eb, ALU.mult, qk + ebk, Qtk)
            K32, K32k = TM(15)
            tt(K32, kk, enb, ALU.mult, kkk + enbk, K32k)
            cp("act", Ktb, K32, K32k, Ktbk)
            tt(Kh.rearrange("p (c s) -> p c s", s=64), K32.rearrange("p (c s) -> p c s", s=64),
               dc.unsqueeze(2).broadcast_to([128, NCH, 64]), ALU.mult, K32k + dck, Khk)
            chk(3.3)
            psc, psck = PSH(2, 0)
            for tb_ in range(NT):
                mm(psc[:, tb_ * 128:(tb_ + 1) * 128], Ktb[:, tb_ * 128:(tb_ + 1) * 128], Qt[:, tb_ * 128:(tb_ + 1) * 128],
                   True, True, Ktbk + Qtk, psck, signal=(tb_ == NT - 1))
            scm, scmk = TB(4)
            tt(scm.rearrange("p (a t) -> p a t", a=NT), psc.rearrange("p (a t) -> p a t", a=NT),
               cmask[:].unsqueeze(1).broadcast_to([128, NT, 128]), ALU.mult, psck + ["cmask"], scmk)
            chk(3.4)
            pv, pvk = PSH(3, 0); pk, pkk = PSH(4, 0)
            for tb_ in range(NT):
                mm(pv[:, tb_ * 128:(tb_ + 1) * 128], vT[:, tb_ * 128:(tb_ + 1) * 128], ident[:], True, True,
                   vTk + ["ident"], pvk, signal=(tb_ == NT - 1))
            chk(3.41)
            for tb_ in range(NT):
                mm(pk[:, tb_ * 128:(tb_ + 1) * 128], Kh[:, tb_ * 128:(tb_ + 1) * 128], ident[:], True, True,
                   Khk + ["ident"], pkk, signal=(tb_ == NT - 1))
            chk(3.42)
            Vt, Vtk = TB(5); Kt, Ktk = TB(6)
            cp("act", Vt, pv, pvk, Vtk)
            chk(3.43)
            ts(Kt, pk, 1.0, None, ALU.mult, None, pkk, Ktk)
            chk(3.5)
            po_, pok_ = PSH(7, 0)
            for tb_ in range(NT):
                mm(po_[:, tb_ * 128:(tb_ + 1) * 128], Vt[:, tb_ * 128:(tb_ + 1) * 128], scm[:, tb_ * 128:(tb_ + 1) * 128],
                   tb_ == 0, False, Vtk + scmk, pok_, signal=False, sgc=True)
            pss, pssk = PSH(2, 0)
            skey = [("S", hh)]
            for ch in range(NCH):
                tb_ = ch // 2; p0 = (ch % 2) * 64
                mm(po_[:, ch * 64:(ch + 1) * 64], Sbf[:, hh, :], Qt[:, ch * 64:(ch + 1) * 64], False, True,
                   [("Sb", hh)] + Qtk, pok_, signal=True, sgc=True)
                mm(pss[:, 0:128], Kt[p0:p0 + 64, tb_ * 128:(tb_ + 1) * 128], Vt[p0:p0 + 64, tb_ * 128:(tb_ + 1) * 128],
                   True, True, Ktk + Vtk, pssk)
                stt(S32[:, hh, :], S32[:, hh, :], dc[:, ch:ch + 1], pss[:, 0:128], ALU.mult, ALU.add,
                    skey + dck + pssk, skey)
                cp("act", Sbf[:, hh, :], S32[:, hh, :], skey, [("Sb", hh)])
            chk(3.6)
            osq, osqk = TM(16)
            act(osq, po_, AF.Square, pok_, osqk)
            pn, pnk = PSH(3, 0)
            mm(pn, onesf[:], osq, True, True, ["onesf"] + osqk, pnk)
            sd, sdk = TM(17)
            act(sd, pn, AF.Sqrt, pnk, sdk, bias=eps_ap, scale=1.0 / 128)
            ri, rik = TM(16)
            T.emit("dve", lambda e, ri=ri, sd=sd: e.reciprocal(out=ri, in_=sd), sdk, rik)
            t1, t1k = TM(17)
            tt(t1, po_, ri, ALU.mult, pok_ + rik, t1k)
            stt(ycat[:, hh, :], t1, vc(c.V_HGN, hh), sgg, ALU.mult, ALU.mult, t1k + sggk + ["vec"], [("yc", hh)])
        chk(4)
        pl1, pl1k = PSH(2, 0); pl2, pl2k = PSH(3, 0)
        for ct in range(NCT):
            pa, pak = next_pslot(); pb, pbk = next_pslot()
            wt, wtk = ws_next(("t", c.T_WIN + 4 * NH + ct)); proj(wt, wtk, hT, "hT", pa, pak)
            wt, wtk = ws_next(("t", c.T_WIN + 4 * NH + NCT + ct)); proj(wt, wtk, hT, "hT", pb, pbk)
            sgb, sgbk = TM(6 + ct % 2)
            act(sgb, pb, AF.Sigmoid, pbk + ["vec"], sgbk, bias=vc(c.V_BGB, ct))
            ub = ubuf[:, ct % 2, :]; ubk = [("ub", ct % 2)]
            cp("pool", ub[:, 0:30], tails[:, ct, :], [("tl", ct)], ubk)
            stt(ub[:, 30:30 + G], pa, vc(c.V_BGA, ct), sgb, ALU.add, ALU.mult, pak + sgbk + ["vec"], ubk)
            cp("pool", tails[:, ct, :], ub[:, G:G + 30], ubk, [("tl", ct)])
            acc, acck = TM(8 + ct % 2)
            ts(acc, ub[:, 0:G], vc(c.V_WDW, ct * 31), vc(c.V_BDW, ct), ALU.mult, ALU.add, ubk + ["vec"], acck)
            for j in range(1, 31):
                stt(acc, ub[:, j:j + G], vc(c.V_WDW, ct * 31 + j), acc, ALU.mult, ALU.add, ubk + acck + ["vec"], acck)
            sq, sqk = TM(10 + ct % 2)
            act(sq, acc, AF.Square, acck, sqk)
            mm(pl1, onesf[:], acc, ct == 0, ct == NCT - 1, ["onesf"] + acck, pl1k)
            mm(pl2, onesf[:], sq, ct == 0, ct == NCT - 1, ["onesf"] + sqk, pl2k)
            cp("act", ycat[:, NH + ct, :], acc, acck, [("yc", NH + ct)])
        if halo:
            hm = vec[:, c.V_HM:c.V_HM + 1]
            allS = [("S", h_) for h_ in range(NH)]; allSb = [("Sb", h_) for h_ in range(NH)]; allT = [("tl", t_) for t_ in range(NCT)]
            ts(S32[:].rearrange("p a b -> p (a b)"), S32[:].rearrange("p a b -> p (a b)"), hm, None, ALU.mult, None, allS + ["vec"], allS)
            ts(Sbf[:].rearrange("p a b -> p (a b)"), Sbf[:].rearrange("p a b -> p (a b)"), hm, None, ALU.mult, None, allSb + ["vec"], allSb)
            ts(tails[:].rearrange("p a b -> p (a b)"), tails[:].rearrange("p a b -> p (a b)"), hm, None, ALU.mult, None, allT + ["vec"], allT)
            continue
        mean, meank = TM(12); msq, msqk = TM(13); var, vark = TM(14); lsd, lsdk = TM(15); lrs, lrsk = TM(16)
        act(mean, pl1, AF.Identity, pl1k, meank, scale=1.0 / c.CW)
        tt(msq, mean, mean, ALU.mult, meank, msqk)
        stt(var, pl2, 1.0 / c.CW, msq, ALU.mult, ALU.subtract, pl2k + msqk, vark)
        act(lsd, var, AF.Sqrt, vark, lsdk, bias=lneps_ap, scale=1.0)
        T.emit("dve", lambda e, lrs=lrs, lsd=lsd: e.reciprocal(out=lrs, in_=lsd), lsdk, lrsk)
        for ct in range(NCT):
            t, tk_ = TM(6 + ct % 2); t2, t2k = TM(8 + ct % 2)
            tt(t, ycat[:, NH + ct, :], mean, ALU.subtract, [("yc", NH + ct)] + meank, tk_)
            tt(t2, t, lrs, ALU.mult, tk_ + lrsk, t2k)
            act(ycat[:, NH + ct, :], t2, AF.Silu, t2k + ["vec"], [("yc", NH + ct)], bias=vc(c.V_LNB, ct), scale=vc(c.V_LNG, ct))
        chk(5)
        for dt in range(KD):
            wt, wtk = ws_next(("t", c.T_WOUT + dt))
            po2, po2k = next_pslot()
            proj(wt, wtk, ycat, "yc", po2, po2k)
            stt(xT[:, dt, :], po2, dv("gt_m", dt), xT[:, dt, :], ALU.mult, ALU.add, po2k + ["der", ("xT", dt)], [("xT", dt)])
        chk(6)
        rs, rsk = rms_rstd(1e-6)
        make_hT("gs_f", "sh_f", rs, rsk)
        qTt = PT
        for jt in range(c.NQT):
            wt, wtk = ws_next(("t", c.T_WPQ + jt))
            pq2, pq2k = next_pslot()
            proj(wt, wtk, hT, "hT", pq2, pq2k)
            cp("act", qTt[:, jt, :], pq2, pq2k, [("PT", jt)])
        chk(7)
        for tt_ in range(NT):
            for h in range(PH):
                b_, hf = [(0, 0), (1, 0), (2, 0), (3, 0)][h % 4]
                pS, pSk = PSH(b_, hf)
                for half in range(2):
                    mm(pS[:, half * 128:(half + 1) * 128], qTt[:, 2 * h + half, tt_ * 128:(tt_ + 1) * 128], keysb[:, 2 * h + half, :],
                       True, True, [("PT", 2 * h + half), "keysb"], pSk, signal=(half == 1))
                skey2 = [("s12", tt_, h)]
                cp("act", s12[:, tt_, h, :, :].rearrange("p a n -> p (a n)"), pS, pSk, skey2)
                for half in range(2):
                    sv = s12[:, tt_, h, half, :]
                    v16 = tk[:, half, :]
                    T.emit("dve", lambda e, sv=sv, v16=v16: e.max(out=v16[:, 0:8], in_=sv), skey2, [("tk", half)])
                    wv = tkw[:, 0, 0:128]
                    T.emit("dve", lambda e, sv=sv, v16=v16, wv=wv: e.match_replace(out=wv, in_to_replace=v16[:, 0:8], in_values=sv, imm_value=NEG),
                           skey2 + [("tk", half)], [("tkw", 0)])
                    T.emit("dve", lambda e, v16=v16, wv=wv: e.max(out=v16[:, 8:16], in_=wv), [("tkw", 0)], [("tk", half)])
                cand = tkw[:, 1, :]
                tt(cand.rearrange("p (a b) -> p a b", a=16), tk[:, 0, :].unsqueeze(2).broadcast_to([128, 16, 16]),
                   tk[:, 1, :].unsqueeze(1).broadcast_to([128, 16, 16]), ALU.add, [("tk", 0), ("tk", 1)], [("tkw", 1)])
                tops = tk[:, 2, :]
                T.emit("dve", lambda e, tops=tops, cand=cand: e.max(out=tops[:, 0:8], in_=cand), [("tkw", 1)], [("tk", 2)])
                cw = tkw[:, 0, :]
                T.emit("dve", lambda e, tops=tops, cand=cand, cw=cw: e.match_replace(out=cw, in_to_replace=tops[:, 0:8], in_values=cand, imm_value=NEG),
                       [("tkw", 1), ("tk", 2)], [("tkw", 0)])
                T.emit("dve", lambda e, tops=tops, cw=cw: e.max(out=tops[:, 8:16], in_=cw), [("tkw", 0)], [("tk", 2)])
                cp("dve", tau[:, tt_, h:h + 1], tops[:, 15:16], [("tk", 2)], [("tau", tt_, h)])
                negm = tk[:, 3, 0:1]; Z = tk[:, 3, 1:2]; lnZ = tk[:, 3, 2:3]; exs = tk[:, 4, :]
                ts(negm, tops[:, 0:1], -1.0, None, ALU.mult, None, [("tk", 2)], [("tk", 3)])
                act(exs, tops, AF.Exp, [("tk", 2), ("tk", 3)], [("tk", 4), ("tk", 3)], bias=negm, accum=Z)
                act(lnZ, Z, AF.Ln, [("tk", 3)], [("tk", 3)])
                tt(nbias[:, tt_, h:h + 1], negm, lnZ, ALU.subtract, [("tk", 3)], [("nb", tt_, h)])
        chk(8)
        acc_slots = [(0, 0), (1, 0), (2, 0), (3, 0)]
        for sbi in range(c.NSB):
            for blk in range(c.SBC // 4):
                c0 = sbi * c.SBC + blk * 4
                for tt_ in range(NT):
                    for h in range(PH):
                        i_ = (tt_ * PH + h) % 2
                        val = tmp[:, 2 * i_:2 * i_ + 2, :].rearrange("p a b -> p (a b)")
                        valk = [("tmp", 2 * i_), ("tmp", 2 * i_ + 1)]
                        Wv = tmp[:, 4 + 2 * i_:6 + 2 * i_, :].rearrange("p a b -> p (a b)")
                        Wk = [("tmp", 4 + 2 * i_), ("tmp", 5 + 2 * i_)]
                        tt(val.rearrange("p (a n) -> p a n", a=4),
                           s12[:, tt_, h, 0, c0:c0 + 4].unsqueeze(2).broadcast_to([128, 4, 128]),
                           s12[:, tt_, h, 1, :].unsqueeze(1).broadcast_to([128, 4, 128]), ALU.add,
                           [("s12", tt_, h)], valk)
                        act(Wv, val, AF.Exp, valk + [("nb", tt_, h)], Wk, bias=nbias[:, tt_, h:h + 1])
                        stt(GH[:, tt_, h, :, :].rearrange("p a n -> p (a n)"), val, tau[:, tt_, h:h + 1], Wv, ALU.is_ge, ALU.mult,
                            valk + Wk + [("tau", tt_, h)], [("GH", tt_, h)])
                for ci in range(4):
                    ch_ = c0 + ci
                    cc = blk * 4 + ci
                    ut, utk = ws_next(("t", c.T_UT + ch_))
                    pA, pAk = PSH(4 + 2 * (cc % 2), 0)
                    proj(ut, utk, hT, "hT", pA, pAk)
                    pG, pGk = PSH(5 + 2 * (cc % 2), 0)
                    for tt_ in range(NT):
                        for h in range(PH):
                            mm(pG[:, tt_ * 128:(tt_ + 1) * 128], GH[:, tt_, h, ci, :], ident[:], h == 0, h == PH - 1,
                               [("GH", tt_, h), "ident"], pGk, signal=(h == PH - 1 and tt_ == NT - 1))
                    ge, gek = TM(8 + cc % 2)
                    act(ge, pA, AF.Gelu, pAk, gek)
                    tt(PT[:, cc, :], ge, pG, ALU.mult, gek + pGk, [("PT", cc)])
            for dq in range(c.NDQ):
                nb_ = c.DQW
                for bnk in range(nb_):
                    pz, pzk = PSB(bnk)
                    mm(pz, zer[:, 0:128], zer[:, 0:512], True, False, ["zer"], pzk, sgc=True)
                for cg in range(c.SBC // c.VU):
                    vb, vbk = ws_next(("v", sbi, dq, cg))
                    W = c.DQW * 128
                    for ci in range(c.VU):
                        cc = cg * c.VU + ci
                        for dtl in range(c.DQW):
                            pa2, pa2k = PSH(*acc_slots[dtl])
                            mm(pa2, vb[:, ci * W + dtl * 128:ci * W + (dtl + 1) * 128], PT[:, cc, :], False, True,
                               vbk + [("PT", cc)], pa2k, signal=(cc == c.SBC - 1 or (ci == c.VU - 1 and dtl == c.DQW - 1)), sgc=True)
                for dtl in range(c.DQW):
                    dt = dq * c.DQW + dtl
                    pa2, pa2k = PSH(*acc_slots[dtl])
                    stt(xT[:, dt, :], pa2, dv("gt_f", dt), xT[:, dt, :], ALU.mult, ALU.add,
                        pa2k + ["der", ("xT", dt)], [("xT", dt)])
        chk(9)
        rs, rsk = rms_rstd(1e-6)
        for k4 in range(0, KD, 2):
            ob = obuf[:, (k4 // 2) % 2, :, :]; obk = [("ob", (k4 // 2) % 2)]
            for k in range(k4, min(KD, k4 + 2)):
                t, tkk = TM(4 + k % 2)
                tt(t, xT[:, k, :], rs, ALU.mult, [("xT", k)] + rsk, tkk)
                act(ob[:, k - k4, :], t, AF.Identity, tkk + ["der"], obk, bias=dv("sh_o", k), scale=dv("gs_o", k))
            n = min(KD, k4 + 2) - k4
            dma("pool", out_d[g - c.HALO, :, k4:k4 + n, :], ob[:, 0:n, :], obk, [("out", g, k4)], "d_o%d" % ((k4 // 2) % 2))
    T.dead = False
    T.final_wait("sp")
    T.replay(nc, es)
    es.close()
    return nc


def host_prepare(cfg, x, c, w_ada, b_ada, w_ada_out, b_ada_out, g_mix, g_ffn, g_out, w_in, b_glu,
                 lb_logits, w_dw, b_dw, ln_g, ln_b, hg_norm_g, w_out, w_pq, sub_keys1, sub_keys2,
                 expert_u, expert_v):
    cf = cfg
    D, KD, G = cf.D, cf.KD, cf.G
    f = lambda a: np.asarray(a, dtype=np.float32)
    x = f(x)[0]
    xTall = x.reshape(cf.NG, G, KD, 128).transpose(0, 3, 2, 1)

    def coltiles(w):
        K, N = w.shape
        return w.reshape(K // 128, 128, N // 128, 128).transpose(2, 1, 0, 3).reshape(N // 128, 128, K)

    wall = np.empty((cf.NTILES, 128, D), np.float32)
    wall[cf.T_WIN:cf.T_WIN + cf.NCOL] = coltiles(f(w_in)[0])
    wall[cf.T_WOUT:cf.T_WOUT + KD] = coltiles(f(w_out)[0])
    wall[cf.T_WPQ:cf.T_WPQ + cf.NQT] = coltiles(f(w_pq)[0])
    u = f(expert_u)[0]
    wall[cf.T_UT:cf.T_UT + cf.NEC] = u.reshape(cf.NEC, 128, KD, 128).transpose(0, 3, 2, 1).reshape(cf.NEC, 128, D)
    wall[cf.T_V:cf.T_V + cf.NEC] = f(expert_v)[0].reshape(cf.NEC, 128, D)
    wcat = np.concatenate([f(w_ada)[0], f(w_ada_out)], axis=1)
    wada = np.ascontiguousarray(wcat.reshape(KD, 128, 8 * D))
    vec = np.zeros((128, cf.NV), np.float32)

    def put(off, v, n):
        vec[:, off:off + n] = v.reshape(n, 128).T

    put(cf.V_C, f(c)[0], KD)
    put(cf.V_BADA, np.concatenate([f(b_ada)[0], f(b_ada_out)]), 8 * KD)
    put(cf.V_GMIX, f(g_mix)[0], KD); put(cf.V_GFFN, f(g_ffn)[0], KD); put(cf.V_GOUT, f(g_out), KD)
    bg = f(b_glu)[0]
    put(cf.V_BGA, bg[:cf.CW], cf.NCT); put(cf.V_BGB, bg[cf.CW:], cf.NCT)
    lbl = f(lb_logits)
    put(cf.V_LB0, lbl[0], cf.NH); put(cf.V_LB1, lbl[1], cf.NH)
    wd = f(w_dw)[0]
    vec[:, cf.V_WDW:cf.V_WDW + cf.NCT * 31] = wd.reshape(31, cf.NCT, 128).transpose(2, 1, 0).reshape(128, cf.NCT * 31)
    put(cf.V_BDW, f(b_dw)[0], cf.NCT); put(cf.V_LNG, f(ln_g)[0], cf.NCT); put(cf.V_LNB, f(ln_b)[0], cf.NCT)
    put(cf.V_HGN, f(hg_norm_g)[0], cf.NH)
    k1 = f(sub_keys1)[0]; k2 = f(sub_keys2)[0]
    ks = np.stack([k1, k2], axis=1).reshape(2 * cf.PH, 128, 128)
    keysT = np.ascontiguousarray(ks.transpose(2, 0, 1))
    maps = []
    for r in range(cf.NCORES):
        g0 = r * cf.NGL
        xr = np.zeros((cf.NGP, 128, KD, G), np.float32)
        if cf.HALO and r > 0:
            xr[0] = xTall[g0 - 1]
        xr[cf.HALO:] = xTall[g0:g0 + cf.NGL]
        vr = vec.copy()
        vr[:, cf.V_HM] = 0.0 if r == 0 else 1.0
        maps.append({"xT": xr, "wall": wall, "wada": wada, "vec": vr, "keysT": keysT})
    return maps


def run(cfg, inputs):
    nc = build(cfg)
    maps = host_prepare(cfg, **inputs)
    res = run_bass_kernel_spmd(nc, maps, core_ids=list(range(cfg.NCORES)))
    oT = np.concatenate([res.results[r]["outT"] for r in range(cfg.NCORES)], axis=0)
    out = oT.transpose(0, 3, 2, 1).reshape(1, cfg.SEQ, cfg.D)
    return np.ascontiguousarray(out.astype(np.float32))


def kernel(**inputs):
    return run(Cfg(), inputs)
```

```python
import numpy as np
from contextlib import ExitStack
import concourse.bass as bass
import concourse.mybir as mybir
from concourse.bass_utils import run_bass_kernel_spmd

F32 = mybir.dt.float32
BF16 = mybir.dt.bfloat16
AF = mybir.ActivationFunctionType
ALU = mybir.AluOpType
NEG = -1.0e30


class Cfg:
    def __init__(self, D=4096, SEQ=16384, PH=8, G=256, NCORES=8):
        self.D = D; self.SEQ = SEQ; self.PH = PH; self.G = G; self.NCORES = NCORES
        self.KD = D // 128
        self.NH = (D // 2) // 128
        self.NCT = (D // 2) // 128
        self.CW = D // 2
        self.NCOL = 3 * D // 128
        self.NQT = 2 * PH
        self.NEC = 128
        self.SBC = 32
        self.NSB = self.NEC // self.SBC
        self.NT = G // 128
        self.NCH = G // 64
        self.NG = SEQ // G
        self.NGL = self.NG // NCORES
        self.HALO = 1 if NCORES > 1 else 0
        self.NGP = self.NGL + self.HALO
        self.DQW = min(4, self.KD)
        self.NDQ = self.KD // self.DQW
        self.VU = D // (self.DQW * 128)
        self.T_WIN = 0
        self.T_WOUT = self.T_WIN + self.NCOL
        self.T_WPQ = self.T_WOUT + self.KD
        self.T_UT = self.T_WPQ + self.NQT
        self.T_V = self.T_UT + self.NEC
        self.NTILES = self.T_V + self.NEC
        o = 0
        def take(n):
            nonlocal o
            r = o; o += n; return r
        KD, NH, NCT = self.KD, self.NH, self.NCT
        self.V_C = take(KD); self.V_BADA = take(8 * KD)
        self.V_GMIX = take(KD); self.V_GFFN = take(KD); self.V_GOUT = take(KD)
        self.V_BGA = take(NCT); self.V_BGB = take(NCT)
        self.V_LB0 = take(NH); self.V_LB1 = take(NH)
        self.V_WDW = take(NCT * 31); self.V_BDW = take(NCT)
        self.V_LNG = take(NCT); self.V_LNB = take(NCT); self.V_HGN = take(NH); self.V_HM = take(1)
        self.NV = o


class Tracker:
    EPOCH = 60000

    def __init__(self):
        self.streams = {n: [] for n in ("pe", "act", "dve", "pool", "sp")}
        self.cnt = {n: 0 for n in self.streams}
        self.epoch = {n: 0 for n in self.streams}
        self.waited = {n: {} for n in self.streams}
        self.keys = {}
        self.dcnt = {}
        self.semids = set()
        self.last_ev = {}
        self.pending = {n: False for n in self.streams}

    def _need(self, eng, ev, needs):
        if ev is None:
            return
        sid, val = ev
        if self.waited[eng].get(sid, 0) >= val:
            return
        if needs.get(sid, 0) < val:
            needs[sid] = val

    dead = False

    def emit(self, eng, fn, reads=(), writes=(), signal=True, dsem=None):
        if self.dead:
            return None
        needs = {}
        if dsem is None and self.cnt[eng] >= 55000 and not self.pending[eng]:
            self.epoch[eng] += 1; self.cnt[eng] = 0
        my_sid = (eng, self.epoch[eng])
        excl = my_sid if eng == "pe" else None
        for k in reads:
            st = self.keys.get(k)
            if st is not None:
                self._need(eng, st[0], needs)
        for k in writes:
            st = self.keys.get(k)
            if st is not None:
                lw = st[0]
                if lw is not None and lw[0] != excl:
                    self._need(eng, lw, needs)
                for sid, val in st[1].items():
                    if sid != excl:
                        self._need(eng, (sid, val), needs)
        waits = []
        for sid, val in needs.items():
            if sid[0] == eng and sid[1] == self.epoch[eng]:
                assert val <= self.cnt[eng], ("wait on future own milestone", eng, val, self.cnt[eng])
            elif sid[0] in self.cnt and sid[1] == self.epoch[sid[0]]:
                assert val <= self.cnt[sid[0]], ("wait on future milestone", eng, sid, val)
            waits.append((sid, val))
            self.waited[eng][sid] = val
        if dsem is not None:
            ep, c = self.dcnt.get(dsem, (0, 0))
            if c + 16 > self.EPOCH:
                ep += 1; c = 0
            c += 16
            self.dcnt[dsem] = (ep, c)
            sid = ("dma", dsem, ep)
            ev = (sid, c)
            inc = (sid, 16)
        elif signal:
            assert self.cnt[eng] + 1 <= self.EPOCH
            self.pending[eng] = False
            self.cnt[eng] += 1
            ev = (my_sid, self.cnt[eng])
            inc = (my_sid, 1)
        else:
            ev = (my_sid, self.cnt[eng] + 1)
            self.pending[eng] = True
            assert self.cnt[eng] + 1 <= self.EPOCH
            inc = None
        self.semids.add(ev[0])
        self.last_ev[ev[0]] = max(self.last_ev.get(ev[0], 0), ev[1])
        for k in reads:
            if isinstance(k, tuple) and k[0] == "psbw":
                continue
            st = self.keys.setdefault(k, [None, {}])
            st[1][ev[0]] = max(st[1].get(ev[0], 0), ev[1])
        for k in writes:
            self.keys[k] = [ev, {}]
        self.streams[eng].append((waits, fn, inc))
        return ev

    def barrier(self):
        evs = dict(self.last_ev)
        for eng in self.streams:
            waits = []
            for sid, val in evs.items():
                if sid[0] == eng:
                    continue
                if self.waited[eng].get(sid, 0) < val:
                    waits.append((sid, val)); self.waited[eng][sid] = val
            if waits:
                self.streams[eng].append((waits, None, None))
        self.keys = {}

    def final_wait(self, eng):
        waits = []
        for sid, val in self.last_ev.items():
            if sid[0] == eng:
                continue
            if self.waited[eng].get(sid, 0) < val:
                waits.append((sid, val)); self.waited[eng][sid] = val
        self.streams[eng].append((waits, None, None))

    def replay(self, nc, es):
        sems = {}
        for i, sid in enumerate(sorted(self.semids, key=str)):
            sems[sid] = es.enter_context(nc.semaphore("s%d" % i))
        blk = es.enter_context(nc.Block())
        T = self

        def run(name, e):
            for waits, fn, inc in T.streams[name]:
                for sid, val in waits:
                    e.wait_ge(sems[sid], val)
                if fn is not None:
                    ins = fn(e)
                    if inc is not None:
                        ins.then_inc(sems[inc[0]], inc[1])

        @blk.sync
        def _(e): run("sp", e)

        @blk.gpsimd
        def _(e): run("pool", e)

        @blk.scalar
        def _(e): run("act", e)

        @blk.vector
        def _(e): run("dve", e)

        @blk.tensor
        def _(e): run("pe", e)


def build(cfg):
    c = cfg
    D, KD, NH, NCT, G, NT, NCH, PH = c.D, c.KD, c.NH, c.NCT, c.G, c.NT, c.NCH, c.PH
    nc = bass.Bass("TRN2", target_bir_lowering=False)
    xT_d = nc.dram_tensor("xT", [c.NGP, 128, KD, G], F32, kind="ExternalInput").ap()
    wall_d = nc.dram_tensor("wall", [c.NTILES, 128, D], F32, kind="ExternalInput").ap()
    wada_d = nc.dram_tensor("wada", [KD, 128, 8 * D], F32, kind="ExternalInput").ap()
    vec_d = nc.dram_tensor("vec", [128, c.NV], F32, kind="ExternalInput").ap()
    keys_d = nc.dram_tensor("keysT", [128, 2 * PH, 128], F32, kind="ExternalInput").ap()
    out_d = nc.dram_tensor("outT", [c.NGL, 128, KD, G], F32, kind="ExternalOutput").ap()
    segs = [(c.T_WIN, c.T_WOUT), (c.T_WOUT, c.T_UT), (c.T_UT, c.T_V), (c.T_V, c.NTILES)]
    wbs = [nc.dram_tensor("wb%d" % j, [b_ - a_, 128, D], BF16, kind="Internal").ap() for j, (a_, b_) in enumerate(segs)]

    def wbt(i):
        for j, (a_, b_) in enumerate(segs):
            if a_ <= i < b_:
                return wbs[j][i - a_]
        raise IndexError(i)

    T = Tracker()
    es = ExitStack()

    def sb(name, shape, dt):
        return es.enter_context(nc.sbuf_tensor(name, shape, dt))

    xT = sb("xT_sb", [128, KD, G], F32)
    hT = sb("hT_sb", [128, KD, G], BF16)
    ycat = sb("ycat", [128, KD, G], BF16)
    PT = sb("PT", [128, c.SBC, G], BF16)
    NB = 3
    wring = sb("wring", [128, NB, D], BF16)
    GH = sb("GH", [128, NT, PH, 4, 128], BF16)
    NTMP = 18
    tmp = sb("tmp", [128, NTMP, G], F32)
    tb = sb("tb16", [128, 12, G], BF16)
    s12 = sb("s12", [128, NT, PH, 2, 128], F32)
    S32 = sb("S32", [128, NH, 128], F32)
    Sbf = sb("Sbf", [128, NH, 128], BF16)
    ubuf = sb("ubuf", [128, 2, 30 + G], F32)
    tails = sb("tails", [128, NCT, 30], F32)
    vec = sb("vec_sb", [128, c.NV], F32)
    modT = sb("modT", [128, 8 * KD], F32)
    der = sb("der", [128, 8 * KD + 4 * NH], F32)
    keysb = sb("keysb", [128, 2 * PH, 128], BF16)
    ident = sb("ident", [128, 128], BF16)
    onesf = sb("onesf", [128, 128], F32)
    cmask = sb("cmask", [128, 128], F32)
    smask = sb("smask", [128, G], F32)
    zer = sb("zer", [128, 512], BF16)
    cact = sb("cact", [128, KD], F32)
    obuf = sb("obuf", [128, 2, 2, G], F32)
    tk = sb("tk", [128, 8, 16], F32)
    tkw = sb("tkw", [128, 2, 256], F32)
    tau = sb("tau", [128, NT, PH], F32)
    nbias = sb("nbias", [128, NT, PH], F32)
    dch = sb("dch", [128, 2, NCH], F32)
    psum = [es.enter_context(nc.psum_tensor("ps%d" % i, [128, 512], F32)) for i in range(8)]

    def PSH(b, h):
        return psum[b][:, h * 256:h * 256 + G], [("ps", b)]

    def PSB(b):
        return psum[b][:, :], [("ps", b)]

    DV = {}
    o = 0
    for nm, n in (("gs_m", KD), ("sh_m", KD), ("gt_m", KD), ("gs_f", KD), ("sh_f", KD), ("gt_f", KD),
                  ("gs_o", KD), ("sh_o", KD), ("lb", NH), ("oml", NH), ("noml", NH), ("sp", NH)):
        DV[nm] = o; o += n

    def dv(nm, i):
        return der[:, DV[nm] + i:DV[nm] + i + 1]

    def vc(off, i):
        return vec[:, off + i:off + i + 1]

    def act(out, in_, func, r, w, bias=None, scale=None, accum=None):
        kw = {}
        if bias is not None: kw["bias"] = bias
        if scale is not None: kw["scale"] = scale
        if accum is not None: kw["accum_out"] = accum
        return T.emit("act", lambda e: e.activation(out=out, in_=in_, func=func, **kw), r, w)

    def tt(out, in0, in1, op, r, w, eng="dve"):
        return T.emit(eng, lambda e: e.tensor_tensor(out=out, in0=in0, in1=in1, op=op), r, w)

    def ts(out, in0, s1, s2, op0, op1, r, w, eng="dve"):
        if s2 is None:
            return T.emit(eng, lambda e: e.tensor_scalar(out=out, in0=in0, scalar1=s1, scalar2=None, op0=op0), r, w)
        return T.emit(eng, lambda e: e.tensor_scalar(out=out, in0=in0, scalar1=s1, scalar2=s2, op0=op0, op1=op1), r, w)

    def stt(out, in0, s, in1, op0, op1, r, w):
        return T.emit("dve", lambda e: e.scalar_tensor_tensor(out=out, in0=in0, scalar=s, in1=in1, op0=op0, op1=op1), r, w)

    def cp(eng, out, in_, r, w):
        if eng == "act":
            return T.emit("act", lambda e: e.copy(out=out, in_=in_), r, w)
        return T.emit(eng, lambda e: e.tensor_copy(out=out, in_=in_), r, w)

    def mm(out, lhsT, rhs, start, stop, r, w, signal=True, sgc=False):
        return T.emit("pe", lambda e: e.matmul(out, lhsT=lhsT, rhs=rhs, start=start, stop=stop, skip_group_check=sgc), r, w, signal=signal)

    def dma(q, out, in_, r, w, dsem):
        return T.emit(q, lambda e: e.dma_start(out=out, in_=in_), r, w, dsem=dsem)

    def TM(i):
        return tmp[:, i, :], [("tmp", i)]

    def TB(i):
        return tb[:, i, :], [("tb", i)]

    import os
    KSTOP = float(os.environ.get("KSTOP", "99"))

    def chk(n):
        if KSTOP <= n:
            T.dead = True

    T.emit("pool", lambda e: e.memset(onesf[:], 1.0), [], ["onesf"])
    T.emit("pool", lambda e: e.affine_select(out=ident[:], in_=onesf[:], pattern=[[-1, 128]], compare_op=ALU.is_equal,
                                              fill=0.0, base=0, channel_multiplier=1), ["onesf"], ["ident"])
    T.emit("pool", lambda e: e.affine_select(out=cmask[:], in_=onesf[:], pattern=[[1, 128]], compare_op=ALU.is_ge,
                                              fill=0.0, base=0, channel_multiplier=-1), ["onesf"], ["cmask"])
    T.emit("pool", lambda e: e.memset(cmask[0:64, 64:128], 0.0), [], ["cmask"])
    T.emit("pool", lambda e: e.memset(smask[:], 1.0), [], ["smask"])
    for ch in range(NCH):
        T.emit("pool", lambda e, ch=ch: e.memset(smask[:, ch * 64:ch * 64 + 1], 0.0), [], ["smask"])
    T.emit("pool", lambda e: e.memset(zer[:], 0.0), [], ["zer"])
    T.emit("pool", lambda e: e.memset(S32[:], 0.0), [], ["S32"])
    T.emit("pool", lambda e: e.memset(Sbf[:], 0.0), [], ["Sbf"])
    T.emit("pool", lambda e: e.memset(tails[:], 0.0), [], ["tails"])
    dma("sp", vec[:], vec_d, [], ["vec"], "d_vec")
    act(cact[:], vec[:, c.V_C:c.V_C + KD], AF.Silu, ["vec"], ["cact"])

    chk(0)
    stg = tmp[:].rearrange("p a b -> p (a b)")[:, 0:D]
    stg_keys = [("tmp", i) for i in range(NTMP)]
    kst = tmp[:].rearrange("p a b -> p (a b)")[:, 0:2 * PH * 128]
    dma("sp", kst, keys_d.rearrange("p a n -> p (a n)"), [], stg_keys, "d_stg")
    cp("dve", keysb[:].rearrange("p a n -> p (a n)"), kst, stg_keys, ["keysb"])
    NCB = 8 * D // D
    mp, mpk = PSB(7)
    mm(mp, zer[:, 0:128], zer[:, 0:512], True, False, ["zer"], mpk, sgc=True)
    for k in range(KD):
        for cbk in range(NCB):
            dma("sp", stg, wada_d[k, :, cbk * D:(cbk + 1) * D], [], stg_keys, "d_stg")
            nj = D // 128
            for j in range(nj):
                col = cbk * nj + j
                mm(psum[7][:, col:col + 1], stg[:, j * 128:(j + 1) * 128], cact[:, k:k + 1], False, True,
                   stg_keys + ["cact"], mpk, signal=(j == nj - 1), sgc=True)
    tt(modT[:], psum[7][:, 0:8 * KD], vec[:, c.V_BADA:c.V_BADA + 8 * KD], ALU.add, mpk + ["vec"], ["modT"])
    def msl(s):
        return modT[:, s * KD:(s + 1) * KD]
    def dsl(nm, n=KD):
        return der[:, DV[nm]:DV[nm] + n]
    stt(dsl("gs_m"), msl(1), 1.0, vec[:, c.V_GMIX:c.V_GMIX + KD], ALU.add, ALU.mult, ["modT", "vec"], ["der"])
    stt(dsl("gs_f"), msl(4), 1.0, vec[:, c.V_GFFN:c.V_GFFN + KD], ALU.add, ALU.mult, ["modT", "vec"], ["der"])
    stt(dsl("gs_o"), msl(7), 1.0, vec[:, c.V_GOUT:c.V_GOUT + KD], ALU.add, ALU.mult, ["modT", "vec"], ["der"])
    for nm, s in (("sh_m", 0), ("gt_m", 2), ("sh_f", 3), ("gt_f", 5), ("sh_o", 6)):
        cp("dve", dsl(nm), msl(s), ["modT"], ["der"])
    tt(dsl("sp", NH), vec[:, c.V_LB0:c.V_LB0 + NH], vec[:, c.V_LB1:c.V_LB1 + NH], ALU.subtract, ["vec", "der"], ["der"])
    act(dsl("lb", NH), dsl("sp", NH), AF.Sigmoid, ["der"], ["der"])
    ts(dsl("oml", NH), dsl("lb", NH), -1.0, 1.0, ALU.mult, ALU.add, ["der"], ["der"])
    ts(dsl("noml", NH), dsl("oml", NH), -1.0, None, ALU.mult, None, ["der"], ["der"])

    chk(1)
    ghflat = GH[:].rearrange("p a b c d -> p (a b c d)")
    assert NT * PH * 4 * 128 >= 2 * D or True
    ncb = max(1, min(2, (NT * PH * 4 * 128) // D))
    for i in range(c.NTILES):
        dma("sp", stg, wall_d[i], [], stg_keys, "d_stg")
        s = i % ncb
        cb = ghflat[:, s * D:(s + 1) * D]
        eng = ("dve", "act", "pool")[i % 3]
        cp(eng, cb, stg, stg_keys, [("cb", s)])
        dma("pool", wbt(i), cb, [("cb", s)], [("wb", i)], "d_cbst%d" % s)
    chk(2)
    T.barrier()

    def group_order(halo=False):
        od = []
        for hh in range(NH):
            od += [("t", c.T_WIN + hh), ("t", c.T_WIN + NH + hh), ("t", c.T_WIN + 2 * NH + hh), ("t", c.T_WIN + 3 * NH + hh)]
        for ct in range(NCT):
            od += [("t", c.T_WIN + 4 * NH + ct), ("t", c.T_WIN + 4 * NH + NCT + ct)]
        if halo:
            return od
        od += [("t", c.T_WOUT + i) for i in range(KD)]
        od += [("t", c.T_WPQ + i) for i in range(c.NQT)]
        for sbi in range(c.NSB):
            od += [("t", c.T_UT + sbi * c.SBC + i) for i in range(c.SBC)]
            for dq in range(c.NDQ):
                od += [("v", sbi, dq, cg) for cg in range(c.SBC // c.VU)]
        return od

    order = (group_order(True) if c.HALO else []) + group_order() * c.NGL
    ws = {"issued": 0, "pos": 0}

    def ws_issue(j):
        it = order[j]
        slot = j % NB
        if it[0] == "t":
            dma("sp", wring[:, slot, :], wbt(it[1]), [], [("w", slot)], "d_w%d" % slot)
        else:
            _, sbi, dq, cg = it
            c0 = sbi * c.SBC + cg * c.VU
            W = c.DQW * 128
            src = wbs[3][c0:c0 + c.VU, :, dq * W:(dq + 1) * W].rearrange("c p w -> p c w")
            dst = wring[:, slot, 0:c.VU * W].rearrange("p (c w) -> p c w", c=c.VU)
            dma("sp", dst, src, [], [("w", slot)], "d_w%d" % slot)

    def ws_next(expect):
        j = ws["pos"]
        assert order[j] == expect, (order[j], expect)
        while ws["issued"] < min(len(order), j + NB):
            ws_issue(ws["issued"]); ws["issued"] += 1
        ws["pos"] += 1
        slot = j % NB
        return wring[:, slot, :], [("w", slot)]

    inv_sqrt_eps = None

    def rms_rstd(eps):
        pn, pnk = PSH(7, 0)
        for k in range(KD):
            sq, sqk = TM(k % 2)
            act(sq, xT[:, k, :], AF.Square, [("xT", k)], sqk)
            mm(pn, onesf[:], sq, k == 0, k == KD - 1, ["onesf"] + sqk, pnk, signal=(k % 2 == 1 or k == KD - 1))
        sd, sdk = TM(2)
        act(sd, pn, AF.Sqrt, pnk, sdk, bias=eps_ap, scale=1.0 / D)
        rs, rsk = TM(3)
        T.emit("dve", lambda e: e.reciprocal(out=rs, in_=sd), sdk, rsk)
        return rs, rsk

    def make_hT(gs, sh, rs, rsk):
        for k in range(KD):
            t, tkk = TM(4 + k % 2)
            tt(t, xT[:, k, :], rs, ALU.mult, [("xT", k)] + rsk, tkk)
            act(hT[:, k, :], t, AF.Identity, tkk + ["der"], [("hT", k)], bias=dv(sh, k), scale=dv(gs, k))

    def proj(wt, wtk, rhs_tile, rhs_key, out, outk):
        for k in range(KD):
            mm(out, wt[:, k * 128:(k + 1) * 128], rhs_tile[:, k, :], k == 0, k == KD - 1,
               wtk + [(rhs_key, k)], outk, signal=(k == KD - 1))

    eps_t = sb("eps_t", [128, 2], F32)
    T.emit("pool", lambda e: e.memset(eps_t[:, 0:1], 1e-6), [], ["eps"])
    T.emit("pool", lambda e: e.memset(eps_t[:, 1:2], 1e-5), [], ["eps"])
    eps_ap = eps_t[:, 0:1]
    lneps_ap = eps_t[:, 1:2]

    proj_slots = [(0, 0), (1, 0), (5, 0), (6, 0)]
    pslot = {"i": 0}

    def next_pslot():
        b, h = proj_slots[pslot["i"] % len(proj_slots)]
        pslot["i"] += 1
        return PSH(b, h)

    for g in range(c.NGP):
        halo = (c.HALO == 1 and g == 0)
        xk = [("xT", k) for k in range(KD)]
        dma("pool", xT[:], xT_d[g], [], xk, "d_x")
        rs, rsk = rms_rstd(1e-6)
        make_hT("gs_m", "sh_m", rs, rsk)
        chk(3)
        for hh in range(NH):
            pq, pqk = next_pslot(); pf, pfk = next_pslot(); pi, pik = next_pslot(); pg, pgk = next_pslot()
            for (po, pok, tix) in ((pq, pqk, hh), (pf, pfk, NH + hh), (pi, pik, 2 * NH + hh), (pg, pgk, 3 * NH + hh)):
                wt, wtk = ws_next(("t", c.T_WIN + tix))
                proj(wt, wtk, hT, "hT", po, pok)
            q, qk = TM(6); sg, sgk = TM(7); sgg, sggk = TM(8)
            act(q, pq, AF.Silu, pqk, qk)
            act(sgg, pg, AF.Silu, pgk, sggk)
            act(sg, pf, AF.Sigmoid, pfk, sgk)
            vT, vTk = TB(0)
            cp("act", vT, pi, pik, vTk)
            chk(3.1)
            f, fk = TM(9); kk, kkk = TM(10)
            ts(f, sg, dv("oml", hh), dv("lb", hh), ALU.mult, ALU.add, sgk + ["der"], fk)
            ts(kk, sg, dv("noml", hh), dv("oml", hh), ALU.mult, ALU.add, sgk + ["der"], kkk)
            lf, lfk = TM(11)
            act(lf, f, AF.Ln, fk, lfk)
            b, bk = TM(12)
            T.emit("dve", lambda e, b=b, lf=lf: e.tensor_tensor_scan(out=b, data0=smask[:], data1=lf, initial=0.0,
                                                                      op0=ALU.mult, op1=ALU.add), lfk + ["smask"], bk)
            chk(3.2)
            eb, ebk = TM(13); enb, enbk = TM(14)
            act(eb, b, AF.Exp, bk, ebk)
            act(enb, b, AF.Exp, bk, enbk, scale=-1.0)
            dc = dch[:, hh % 2, :]; dck = [("dch", hh % 2)]
            blast = b.rearrange("p (c s) -> p c s", s=64)[:, :, 63]
            act(dc, blast, AF.Exp, bk, dck)
            Qt, Qtk = TB(1); Ktb, Ktbk = TB(2); Kh, Khk = TB(3)
            tt(Qt, q, eb, ALU.mult, qk + ebk, Qtk)
            K32, K32k = TM(15)
            tt(K32, kk, enb, ALU.mult, kkk + enbk, K32k)
            cp("act", Ktb, K32, K32k, Ktbk)
            tt(Kh.rearrange("p (c s) -> p c s", s=64), K32.rearrange("p (c s) -> p c s", s=64),
               dc.unsqueeze(2).broadcast_to([128, NCH, 64]), ALU.mult, K32k + dck, Khk)
            chk(3.3)
            psc, psck = PSH(2, 0)
            for tb_ in range(NT):
                mm(psc[:, tb_ * 128:(tb_ + 1) * 128], Ktb[:, tb_ * 128:(tb_ + 1) * 128], Qt[:, tb_ * 128:(tb_ + 1) * 128],
                   True, True, Ktbk + Qtk, psck, signal=(tb_ == NT - 1))
            scm, scmk = TB(4)
            tt(scm.rearrange("p (a t) -> p a t", a=NT), psc.rearrange("p (a t) -> p a t", a=NT),
               cmask[:].unsqueeze(1).broadcast_to([128, NT, 128]), ALU.mult, psck + ["cmask"], scmk)
            chk(3.4)
            pv, pvk = PSH(3, 0); pk, pkk = PSH(4, 0)
            for tb_ in range(NT):
                mm(pv[:, tb_ * 128:(tb_ + 1) * 128], vT[:, tb_ * 128:(tb_ + 1) * 128], ident[:], True, True,
                   vTk + ["ident"], pvk, signal=(tb_ == NT - 1))
            chk(3.41)
            for tb_ in range(NT):
                mm(pk[:, tb_ * 128:(tb_ + 1) * 128], Kh[:, tb_ * 128:(tb_ + 1) * 128], ident[:], True, True,
                   Khk + ["ident"], pkk, signal=(tb_ == NT - 1))
            chk(3.42)
            Vt, Vtk = TB(5); Kt, Ktk = TB(6)
            cp("act", Vt, pv, pvk, Vtk)
            chk(3.43)
            ts(Kt, pk, 1.0, None, ALU.mult, None, pkk, Ktk)
            chk(3.5)
            po_, pok_ = PSH(7, 0)
            for tb_ in range(NT):
                mm(po_[:, tb_ * 128:(tb_ + 1) * 128], Vt[:, tb_ * 128:(tb_ + 1) * 128], scm[:, tb_ * 128:(tb_ + 1) * 128],
                   tb_ == 0, False, Vtk + scmk, pok_, signal=False, sgc=True)
            pss, pssk = PSH(2, 0)
            skey = [("S", hh)]
            for ch in range(NCH):
                tb_ = ch // 2; p0 = (ch % 2) * 64
                mm(po_[:, ch * 64:(ch + 1) * 64], Sbf[:, hh, :], Qt[:, ch * 64:(ch + 1) * 64], False, True,
                   [("Sb", hh)] + Qtk, pok_, signal=True, sgc=True)
                mm(pss[:, 0:128], Kt[p0:p0 + 64, tb_ * 128:(tb_ + 1) * 128], Vt[p0:p0 + 64, tb_ * 128:(tb_ + 1) * 128],
                   True, True, Ktk + Vtk, pssk)
                stt(S32[:, hh, :], S32[:, hh, :], dc[:, ch:ch + 1], pss[:, 0:128], ALU.mult, ALU.add,
                    skey + dck + pssk, skey)
                cp("act", Sbf[:, hh, :], S32[:, hh, :], skey, [("Sb", hh)])
            chk(3.6)
            osq, osqk = TM(16)
            act(osq, po_, AF.Square, pok_, osqk)
            pn, pnk = PSH(3, 0)
            mm(pn, onesf[:], osq, True, True, ["onesf"] + osqk, pnk)
            sd, sdk = TM(17)
            act(sd, pn, AF.Sqrt, pnk, sdk, bias=eps_ap, scale=1.0 / 128)
            ri, rik = TM(16)
            T.emit("dve", lambda e, ri=ri, sd=sd: e.reciprocal(out=ri, in_=sd), sdk, rik)
            t1, t1k = TM(17)
            tt(t1, po_, ri, ALU.mult, pok_ + rik, t1k)
            stt(ycat[:, hh, :], t1, vc(c.V_HGN, hh), sgg, ALU.mult, ALU.mult, t1k + sggk + ["vec"], [("yc", hh)])
        chk(4)
        pl1, pl1k = PSH(2, 0); pl2, pl2k = PSH(3, 0)
        for ct in range(NCT):
            pa, pak = next_pslot(); pb, pbk = next_pslot()
            wt, wtk = ws_next(("t", c.T_WIN + 4 * NH + ct)); proj(wt, wtk, hT, "hT", pa, pak)
            wt, wtk = ws_next(("t", c.T_WIN + 4 * NH + NCT + ct)); proj(wt, wtk, hT, "hT", pb, pbk)
            sgb, sgbk = TM(6 + ct % 2)
            act(sgb, pb, AF.Sigmoid, pbk + ["vec"], sgbk, bias=vc(c.V_BGB, ct))
            ub = ubuf[:, ct % 2, :]; ubk = [("ub", ct % 2)]
            cp("pool", ub[:, 0:30], tails[:, ct, :], [("tl", ct)], ubk)
            stt(ub[:, 30:30 + G], pa, vc(c.V_BGA, ct), sgb, ALU.add, ALU.mult, pak + sgbk + ["vec"], ubk)
            cp("pool", tails[:, ct, :], ub[:, G:G + 30], ubk, [("tl", ct)])
            acc, acck = TM(8 + ct % 2)
            ts(acc, ub[:, 0:G], vc(c.V_WDW, ct * 31), vc(c.V_BDW, ct), ALU.mult, ALU.add, ubk + ["vec"], acck)
            for j in range(1, 31):
                stt(acc, ub[:, j:j + G], vc(c.V_WDW, ct * 31 + j), acc, ALU.mult, ALU.add, ubk + acck + ["vec"], acck)
            sq, sqk = TM(10 + ct % 2)
            act(sq, acc, AF.Square, acck, sqk)
            mm(pl1, onesf[:], acc, ct == 0, ct == NCT - 1, ["onesf"] + acck, pl1k)
            mm(pl2, onesf[:], sq, ct == 0, ct == NCT - 1, ["onesf"] + sqk, pl2k)
            cp("act", ycat[:, NH + ct, :], acc, acck, [("yc", NH + ct)])
        if halo:
            hm = vec[:, c.V_HM:c.V_HM + 1]
            allS = [("S", h_) for h_ in range(NH)]; allSb = [("Sb", h_) for h_ in range(NH)]; allT = [("tl", t_) for t_ in range(NCT)]
            ts(S32[:].rearrange("p a b -> p (a b)"), S32[:].rearrange("p a b -> p (a b)"), hm, None, ALU.mult, None, allS + ["vec"], allS)
            ts(Sbf[:].rearrange("p a b -> p (a b)"), Sbf[:].rearrange("p a b -> p (a b)"), hm, None, ALU.mult, None, allSb + ["vec"], allSb)
            ts(tails[:].rearrange("p a b -> p (a b)"), tails[:].rearrange("p a b -> p (a b)"), hm, None, ALU.mult, None, allT + ["vec"], allT)
            continue
        mean, meank = TM(12); msq, msqk = TM(13); var, vark = TM(14); lsd, lsdk = TM(15); lrs, lrsk = TM(16)
        act(mean, pl1, AF.Identity, pl1k, meank, scale=1.0 / c.CW)
        tt(msq, mean, mean, ALU.mult, meank, msqk)
        stt(var, pl2, 1.0 / c.CW, msq, ALU.mult, ALU.subtract, pl2k + msqk, vark)
        act(lsd, var, AF.Sqrt, vark, lsdk, bias=lneps_ap, scale=1.0)
        T.emit("dve", lambda e, lrs=lrs, lsd=lsd: e.reciprocal(out=lrs, in_=lsd), lsdk, lrsk)
        for ct in range(NCT):
            t, tk_ = TM(6 + ct % 2); t2, t2k = TM(8 + ct % 2)
            tt(t, ycat[:, NH + ct, :], mean, ALU.subtract, [("yc", NH + ct)] + meank, tk_)
            tt(t2, t, lrs, ALU.mult, tk_ + lrsk, t2k)
            act(ycat[:, NH + ct, :], t2, AF.Silu, t2k + ["vec"], [("yc", NH + ct)], bias=vc(c.V_LNB, ct), scale=vc(c.V_LNG, ct))
        chk(5)
        for dt in range(KD):
            wt, wtk = ws_next(("t", c.T_WOUT + dt))
            po2, po2k = next_pslot()
            proj(wt, wtk, ycat, "yc", po2, po2k)
            stt(xT[:, dt, :], po2, dv("gt_m", dt), xT[:, dt, :], ALU.mult, ALU.add, po2k + ["der", ("xT", dt)], [("xT", dt)])
        chk(6)
        rs, rsk = rms_rstd(1e-6)
        make_hT("gs_f", "sh_f", rs, rsk)
        qTt = PT
        for jt in range(c.NQT):
            wt, wtk = ws_next(("t", c.T_WPQ + jt))
            pq2, pq2k = next_pslot()
            proj(wt, wtk, hT, "hT", pq2, pq2k)
            cp("act", qTt[:, jt, :], pq2, pq2k, [("PT", jt)])
        chk(7)
        for tt_ in range(NT):
            for h in range(PH):
                b_, hf = [(0, 0), (1, 0), (2, 0), (3, 0)][h % 4]
                pS, pSk = PSH(b_, hf)
                for half in range(2):
                    mm(pS[:, half * 128:(half + 1) * 128], qTt[:, 2 * h + half, tt_ * 128:(tt_ + 1) * 128], keysb[:, 2 * h + half, :],
                       True, True, [("PT", 2 * h + half), "keysb"], pSk, signal=(half == 1))
                skey2 = [("s12", tt_, h)]
                cp("act", s12[:, tt_, h, :, :].rearrange("p a n -> p (a n)"), pS, pSk, skey2)
                for half in range(2):
                    sv = s12[:, tt_, h, half, :]
                    v16 = tk[:, half, :]
                    T.emit("dve", lambda e, sv=sv, v16=v16: e.max(out=v16[:, 0:8], in_=sv), skey2, [("tk", half)])
                    wv = tkw[:, 0, 0:128]
                    T.emit("dve", lambda e, sv=sv, v16=v16, wv=wv: e.match_replace(out=wv, in_to_replace=v16[:, 0:8], in_values=sv, imm_value=NEG),
                           skey2 + [("tk", half)], [("tkw", 0)])
                    T.emit("dve", lambda e, v16=v16, wv=wv: e.max(out=v16[:, 8:16], in_=wv), [("tkw", 0)], [("tk", half)])
                cand = tkw[:, 1, :]
                tt(cand.rearrange("p (a b) -> p a b", a=16), tk[:, 0, :].unsqueeze(2).broadcast_to([128, 16, 16]),
                   tk[:, 1, :].unsqueeze(1).broadcast_to([128, 16, 16]), ALU.add, [("tk", 0), ("tk", 1)], [("tkw", 1)])
                tops = tk[:, 2, :]
                T.emit("dve", lambda e, tops=tops, cand=cand: e.max(out=tops[:, 0:8], in_=cand), [("tkw", 1)], [("tk", 2)])
                cw = tkw[:, 0, :]
                T.emit("dve", lambda e, tops=tops, cand=cand, cw=cw: e.match_replace(out=cw, in_to_replace=tops[:, 0:8], in_values=cand, imm_value=NEG),
                       [("tkw", 1), ("tk", 2)], [("tkw", 0)])
                T.emit("dve", lambda e, tops=tops, cw=cw: e.max(out=tops[:, 8:16], in_=cw), [("tkw", 0)], [("tk", 2)])
                cp("dve", tau[:, tt_, h:h + 1], tops[:, 15:16], [("tk", 2)], [("tau", tt_, h)])
                negm = tk[:, 3, 0:1]; Z = tk[:, 3, 1:2]; lnZ = tk[:, 3, 2:3]; exs = tk[:, 4, :]
                ts(negm, tops[:, 0:1], -1.0, None, ALU.mult, None, [("tk", 2)], [("tk", 3)])
                act(exs, tops, AF.Exp, [("tk", 2), ("tk", 3)], [("tk", 4), ("tk", 3)], bias=negm, accum=Z)
                act(lnZ, Z, AF.Ln, [("tk", 3)], [("tk", 3)])
                tt(nbias[:, tt_, h:h + 1], negm, lnZ, ALU.subtract, [("tk", 3)], [("nb", tt_, h)])
        chk(8)
        acc_slots = [(0, 0), (1, 0), (2, 0), (3, 0)]
        for sbi in range(c.NSB):
            for blk in range(c.SBC // 4):
                c0 = sbi * c.SBC + blk * 4
                for tt_ in range(NT):
                    for h in range(PH):
                        i_ = (tt_ * PH + h) % 2
                        val = tmp[:, 2 * i_:2 * i_ + 2, :].rearrange("p a b -> p (a b)")
                        valk = [("tmp", 2 * i_), ("tmp", 2 * i_ + 1)]
                        Wv = tmp[:, 4 + 2 * i_:6 + 2 * i_, :].rearrange("p a b -> p (a b)")
                        Wk = [("tmp", 4 + 2 * i_), ("tmp", 5 + 2 * i_)]
                        tt(val.rearrange("p (a n) -> p a n", a=4),
                           s12[:, tt_, h, 0, c0:c0 + 4].unsqueeze(2).broadcast_to([128, 4, 128]),
                           s12[:, tt_, h, 1, :].unsqueeze(1).broadcast_to([128, 4, 128]), ALU.add,
                           [("s12", tt_, h)], valk)
                        act(Wv, val, AF.Exp, valk + [("nb", tt_, h)], Wk, bias=nbias[:, tt_, h:h + 1])
                        stt(GH[:, tt_, h, :, :].rearrange("p a n -> p (a n)"), val, tau[:, tt_, h:h + 1], Wv, ALU.is_ge, ALU.mult,
                            valk + Wk + [("tau", tt_, h)], [("GH", tt_, h)])
                for ci in range(4):
                    ch_ = c0 + ci
                    cc = blk * 4 + ci
                    ut, utk = ws_next(("t", c.T_UT + ch_))
                    pA, pAk = PSH(4 + 2 * (cc % 2), 0)
                    proj(ut, utk, hT, "hT", pA, pAk)
                    pG, pGk = PSH(5 + 2 * (cc % 2), 0)
                    for tt_ in range(NT):
                        for h in range(PH):
                            mm(pG[:, tt_ * 128:(tt_ + 1) * 128], GH[:, tt_, h, ci, :], ident[:], h == 0, h == PH - 1,
                               [("GH", tt_, h), "ident"], pGk, signal=(h == PH - 1 and tt_ == NT - 1))
                    ge, gek = TM(8 + cc % 2)
                    act(ge, pA, AF.Gelu, pAk, gek)
                    tt(PT[:, cc, :], ge, pG, ALU.mult, gek + pGk, [("PT", cc)])
            for dq in range(c.NDQ):
                nb_ = c.DQW
                for bnk in range(nb_):
                    pz, pzk = PSB(bnk)
                    mm(pz, zer[:, 0:128], zer[:, 0:512], True, False, ["zer"], pzk, sgc=True)
                for cg in range(c.SBC // c.VU):
                    vb, vbk = ws_next(("v", sbi, dq, cg))
                    W = c.DQW * 128
                    for ci in range(c.VU):
                        cc = cg * c.VU + ci
                        for dtl in range(c.DQW):
                            pa2, pa2k = PSH(*acc_slots[dtl])
                            mm(pa2, vb[:, ci * W + dtl * 128:ci * W + (dtl + 1) * 128], PT[:, cc, :], False, True,
                               vbk + [("PT", cc)], pa2k, signal=(cc == c.SBC - 1 or (ci == c.VU - 1 and dtl == c.DQW - 1)), sgc=True)
                for dtl in range(c.DQW):
                    dt = dq * c.DQW + dtl
                    pa2, pa2k = PSH(*acc_slots[dtl])
                    stt(xT[:, dt, :], pa2, dv("gt_f", dt), xT[:, dt, :], ALU.mult, ALU.add,
                        pa2k + ["der", ("xT", dt)], [("xT", dt)])
        chk(9)
        rs, rsk = rms_rstd(1e-6)
        for k4 in range(0, KD, 2):
            ob = obuf[:, (k4 // 2) % 2, :, :]; obk = [("ob", (k4 // 2) % 2)]
            for k in range(k4, min(KD, k4 + 2)):
                t, tkk = TM(4 + k % 2)
                tt(t, xT[:, k, :], rs, ALU.mult, [("xT", k)] + rsk, tkk)
                act(ob[:, k - k4, :], t, AF.Identity, tkk + ["der"], obk, bias=dv("sh_o", k), scale=dv("gs_o", k))
            n = min(KD, k4 + 2) - k4
            dma("pool", out_d[g - c.HALO, :, k4:k4 + n, :], ob[:, 0:n, :], obk, [("out", g, k4)], "d_o%d" % ((k4 // 2) % 2))
    T.dead = False
    T.final_wait("sp")
    T.replay(nc, es)
    es.close()
    return nc


def host_prepare(cfg, x, c, w_ada, b_ada, w_ada_out, b_ada_out, g_mix, g_ffn, g_out, w_in, b_glu,
                 lb_logits, w_dw, b_dw, ln_g, ln_b, hg_norm_g, w_out, w_pq, sub_keys1, sub_keys2,
                 expert_u, expert_v):
    cf = cfg
    D, KD, G = cf.D, cf.KD, cf.G
    f = lambda a: np.asarray(a, dtype=np.float32)
    x = f(x)[0]
    xTall = x.reshape(cf.NG, G, KD, 128).transpose(0, 3, 2, 1)

    def coltiles(w):
        K, N = w.shape
        return w.reshape(K // 128, 128, N // 128, 128).transpose(2, 1, 0, 3).reshape(N // 128, 128, K)

    wall = np.empty((cf.NTILES, 128, D), np.float32)
    wall[cf.T_WIN:cf.T_WIN + cf.NCOL] = coltiles(f(w_in)[0])
    wall[cf.T_WOUT:cf.T_WOUT + KD] = coltiles(f(w_out)[0])
    wall[cf.T_WPQ:cf.T_WPQ + cf.NQT] = coltiles(f(w_pq)[0])
    u = f(expert_u)[0]
    wall[cf.T_UT:cf.T_UT + cf.NEC] = u.reshape(cf.NEC, 128, KD, 128).transpose(0, 3, 2, 1).reshape(cf.NEC, 128, D)
    wall[cf.T_V:cf.T_V + cf.NEC] = f(expert_v)[0].reshape(cf.NEC, 128, D)
    wcat = np.concatenate([f(w_ada)[0], f(w_ada_out)], axis=1)
    wada = np.ascontiguousarray(wcat.reshape(KD, 128, 8 * D))
    vec = np.zeros((128, cf.NV), np.float32)

    def put(off, v, n):
        vec[:, off:off + n] = v.reshape(n, 128).T

    put(cf.V_C, f(c)[0], KD)
    put(cf.V_BADA, np.concatenate([f(b_ada)[0], f(b_ada_out)]), 8 * KD)
    put(cf.V_GMIX, f(g_mix)[0], KD); put(cf.V_GFFN, f(g_ffn)[0], KD); put(cf.V_GOUT, f(g_out), KD)
    bg = f(b_glu)[0]
    put(cf.V_BGA, bg[:cf.CW], cf.NCT); put(cf.V_BGB, bg[cf.CW:], cf.NCT)
    lbl = f(lb_logits)
    put(cf.V_LB0, lbl[0], cf.NH); put(cf.V_LB1, lbl[1], cf.NH)
    wd = f(w_dw)[0]
    vec[:, cf.V_WDW:cf.V_WDW + cf.NCT * 31] = wd.reshape(31, cf.NCT, 128).transpose(2, 1, 0).reshape(128, cf.NCT * 31)
    put(cf.V_BDW, f(b_dw)[0], cf.NCT); put(cf.V_LNG, f(ln_g)[0], cf.NCT); put(cf.V_LNB, f(ln_b)[0], cf.NCT)
    put(cf.V_HGN, f(hg_norm_g)[0], cf.NH)
    k1 = f(sub_keys1)[0]; k2 = f(sub_keys2)[0]
    ks = np.stack([k1, k2], axis=1).reshape(2 * cf.PH, 128, 128)
    keysT = np.ascontiguousarray(ks.transpose(2, 0, 1))
    maps = []
    for r in range(cf.NCORES):
        g0 = r * cf.NGL
        xr = np.zeros((cf.NGP, 128, KD, G), np.float32)
        if cf.HALO and r > 0:
            xr[0] = xTall[g0 - 1]
        xr[cf.HALO:] = xTall[g0:g0 + cf.NGL]
        vr = vec.copy()
        vr[:, cf.V_HM] = 0.0 if r == 0 else 1.0
        maps.append({"xT": xr, "wall": wall, "wada": wada, "vec": vr, "keysT": keysT})
    return maps


def run(cfg, inputs):
    nc = build(cfg)
    maps = host_prepare(cfg, **inputs)
    res = run_bass_kernel_spmd(nc, maps, core_ids=list(range(cfg.NCORES)))
    oT = np.concatenate([res.results[r]["outT"] for r in range(cfg.NCORES)], axis=0)
    out = oT.transpose(0, 3, 2, 1).reshape(1, cfg.SEQ, cfg.D)
    return np.ascontiguousarray(out.astype(np.float32))


def kernel(**inputs):
    return run(Cfg(), inputs)
```

```python
import numpy as np
from contextlib import ExitStack
import concourse.bass as bass
import concourse.mybir as mybir
from concourse.bass_utils import run_bass_kernel_spmd

F32 = mybir.dt.float32
BF16 = mybir.dt.bfloat16
AF = mybir.ActivationFunctionType
ALU = mybir.AluOpType
NEG = -1.0e30


class Cfg:
    def __init__(self, D=4096, SEQ=16384, PH=8, G=256, NCORES=8):
        self.D = D; self.SEQ = SEQ; self.PH = PH; self.G = G; self.NCORES = NCORES
        self.KD = D // 128
        self.NH = (D // 2) // 128
        self.NCT = (D // 2) // 128
        self.CW = D // 2
        self.NCOL = 3 * D // 128
        self.NQT = 2 * PH
        self.NEC = 128
        self.SBC = 32
        self.NSB = self.NEC // self.SBC
        self.NT = G // 128
        self.NCH = G // 64
        self.NG = SEQ // G
        self.NGL = self.NG // NCORES
        self.HALO = 1 if NCORES > 1 else 0
        self.NGP = self.NGL + self.HALO
        self.DQW = min(4, self.KD)
        self.NDQ = self.KD // self.DQW
        self.VU = D // (self.DQW * 128)
        self.T_WIN = 0
        self.T_WOUT = self.T_WIN + self.NCOL
        self.T_WPQ = self.T_WOUT + self.KD
        self.T_UT = self.T_WPQ + self.NQT
        self.T_V = self.T_UT + self.NEC
        self.NTILES = self.T_V + self.NEC
        o = 0
        def take(n):
            nonlocal o
            r = o; o += n; return r
        KD, NH, NCT = self.KD, self.NH, self.NCT
        self.V_C = take(KD); self.V_BADA = take(8 * KD)
        self.V_GMIX = take(KD); self.V_GFFN = take(KD); self.V_GOUT = take(KD)
        self.V_BGA = take(NCT); self.V_BGB = take(NCT)
        self.V_LB0 = take(NH); self.V_LB1 = take(NH)
        self.V_WDW = take(NCT * 31); self.V_BDW = take(NCT)
        self.V_LNG = take(NCT); self.V_LNB = take(NCT); self.V_HGN = take(NH); self.V_HM = take(1)
        self.NV = o


class Tracker:
    EPOCH = 60000

    def __init__(self):
        self.streams = {n: [] for n in ("pe", "act", "dve", "pool", "sp")}
        self.cnt = {n: 0 for n in self.streams}
        self.epoch = {n: 0 for n in self.streams}
        self.waited = {n: {} for n in self.streams}
        self.keys = {}
        self.dcnt = {}
        self.semids = set()
        self.last_ev = {}
        self.pending = {n: False for n in self.streams}

    def _need(self, eng, ev, needs):
        if ev is None:
            return
        sid, val = ev
        if self.waited[eng].get(sid, 0) >= val:
            return
        if needs.get(sid, 0) < val:
            needs[sid] = val

    dead = False

    def emit(self, eng, fn, reads=(), writes=(), signal=True, dsem=None):
        if self.dead:
            return None
        needs = {}
        if dsem is None and self.cnt[eng] >= 55000 and not self.pending[eng]:
            self.epoch[eng] += 1; self.cnt[eng] = 0
        my_sid = (eng, self.epoch[eng])
        excl = my_sid if eng == "pe" else None
        for k in reads:
            st = self.keys.get(k)
            if st is not None:
                self._need(eng, st[0], needs)
        for k in writes:
            st = self.keys.get(k)
            if st is not None:
                lw = st[0]
                if lw is not None and lw[0] != excl:
                    self._need(eng, lw, needs)
                for sid, val in st[1].items():
                    if sid != excl:
                        self._need(eng, (sid, val), needs)
        waits = []
        for sid, val in needs.items():
            if sid[0] == eng and sid[1] == self.epoch[eng]:
                assert val <= self.cnt[eng], ("wait on future own milestone", eng, val, self.cnt[eng])
            elif sid[0] in self.cnt and sid[1] == self.epoch[sid[0]]:
                assert val <= self.cnt[sid[0]], ("wait on future milestone", eng, sid, val)
            waits.append((sid, val))
            self.waited[eng][sid] = val
        if dsem is not None:
            ep, c = self.dcnt.get(dsem, (0, 0))
            if c + 16 > self.EPOCH:
                ep += 1; c = 0
            c += 16
            self.dcnt[dsem] = (ep, c)
            sid = ("dma", dsem, ep)
            ev = (sid, c)
            inc = (sid, 16)
        elif signal:
            assert self.cnt[eng] + 1 <= self.EPOCH
            self.pending[eng] = False
            self.cnt[eng] += 1
            ev = (my_sid, self.cnt[eng])
            inc = (my_sid, 1)
        else:
            ev = (my_sid, self.cnt[eng] + 1)
            self.pending[eng] = True
            assert self.cnt[eng] + 1 <= self.EPOCH
            inc = None
        self.semids.add(ev[0])
        self.last_ev[ev[0]] = max(self.last_ev.get(ev[0], 0), ev[1])
        for k in reads:
            if isinstance(k, tuple) and k[0] == "psbw":
                continue
            st = self.keys.setdefault(k, [None, {}])
            st[1][ev[0]] = max(st[1].get(ev[0], 0), ev[1])
        for k in writes:
            self.keys[k] = [ev, {}]
        self.streams[eng].append((waits, fn, inc))
        return ev

    def barrier(self):
        evs = dict(self.last_ev)
        for eng in self.streams:
            waits = []
            for sid, val in evs.items():
                if sid[0] == eng:
                    continue
                if self.waited[eng].get(sid, 0) < val:
                    waits.append((sid, val)); self.waited[eng][sid] = val
            if waits:
                self.streams[eng].append((waits, None, None))
        self.keys = {}

    def final_wait(self, eng):
        waits = []
        for sid, val in self.last_ev.items():
            if sid[0] == eng:
                continue
            if self.waited[eng].get(sid, 0) < val:
                waits.append((sid, val)); self.waited[eng][sid] = val
        self.streams[eng].append((waits, None, None))

    def replay(self, nc, es):
        sems = {}
        for i, sid in enumerate(sorted(self.semids, key=str)):
            sems[sid] = es.enter_context(nc.semaphore("s%d" % i))
        blk = es.enter_context(nc.Block())
        T = self

        def run(name, e):
            for waits, fn, inc in T.streams[name]:
                for sid, val in waits:
                    e.wait_ge(sems[sid], val)
                if fn is not None:
                    ins = fn(e)
                    if inc is not None:
                        ins.then_inc(sems[inc[0]], inc[1])

        @blk.sync
        def _(e): run("sp", e)

        @blk.gpsimd
        def _(e): run("pool", e)

        @blk.scalar
        def _(e): run("act", e)

        @blk.vector
        def _(e): run("dve", e)

        @blk.tensor
        def _(e): run("pe", e)


def build(cfg):
    c = cfg
    D, KD, NH, NCT, G, NT, NCH, PH = c.D, c.KD, c.NH, c.NCT, c.G, c.NT, c.NCH, c.PH
    nc = bass.Bass("TRN2", target_bir_lowering=False)
    xT_d = nc.dram_tensor("xT", [c.NGP, 128, KD, G], F32, kind="ExternalInput").ap()
    wall_d = nc.dram_tensor("wall", [c.NTILES, 128, D], F32, kind="ExternalInput").ap()
    wada_d = nc.dram_tensor("wada", [KD, 128, 8 * D], F32, kind="ExternalInput").ap()
    vec_d = nc.dram_tensor("vec", [128, c.NV], F32, kind="ExternalInput").ap()
    keys_d = nc.dram_tensor("keysT", [128, 2 * PH, 128], F32, kind="ExternalInput").ap()
    out_d = nc.dram_tensor("outT", [c.NGL, 128, KD, G], F32, kind="ExternalOutput").ap()
    segs = [(c.T_WIN, c.T_WOUT), (c.T_WOUT, c.T_UT), (c.T_UT, c.T_V), (c.T_V, c.NTILES)]
    wbs = [nc.dram_tensor("wb%d" % j, [b_ - a_, 128, D], BF16, kind="Internal").ap() for j, (a_, b_) in enumerate(segs)]

    def wbt(i):
        for j, (a_, b_) in enumerate(segs):
            if a_ <= i < b_:
                return wbs[j][i - a_]
        raise IndexError(i)

    T = Tracker()
    es = ExitStack()

    def sb(name, shape, dt):
        return es.enter_context(nc.sbuf_tensor(name, shape, dt))

    xT = sb("xT_sb", [128, KD, G], F32)
    hT = sb("hT_sb", [128, KD, G], BF16)
    ycat = sb("ycat", [128, KD, G], BF16)
    PT = sb("PT", [128, c.SBC, G], BF16)
    NB = 3
    wring = sb("wring", [128, NB, D], BF16)
    GH = sb("GH", [128, 2, NT, PH, 2, 128], BF16)
    NTMP = 18
    tmp = sb("tmp", [128, NTMP, G], F32)
    tb = sb("tb16", [128, 12, G], BF16)
    s12 = sb("s12", [128, NT, PH, 2, 128], F32)
    S32 = sb("S32", [128, NH, 128], F32)
    Sbf = sb("Sbf", [128, NH, 128], BF16)
    ubuf = sb("ubuf", [128, 2, 30 + G], F32)
    tails = sb("tails", [128, NCT, 30], F32)
    vec = sb("vec_sb", [128, c.NV], F32)
    modT = sb("modT", [128, 8 * KD], F32)
    der = sb("der", [128, 8 * KD + 4 * NH], F32)
    keysb = sb("keysb", [128, 2 * PH, 128], BF16)
    ident = sb("ident", [128, 128], BF16)
    onesf = sb("onesf", [128, 128], F32)
    cmask = sb("cmask", [128, 128], F32)
    smask = sb("smask", [128, G], F32)
    zer = sb("zer", [128, 512], BF16)
    cact = sb("cact", [128, KD], F32)
    obuf = sb("obuf", [128, 2, 2, G], F32)
    tk = sb("tk", [128, 8, 16], F32)
    tkw = sb("tkw", [128, 2, 256], F32)
    tau = sb("tau", [128, NT, PH], F32)
    nbias = sb("nbias", [128, NT, PH], F32)
    dch = sb("dch", [128, 2, NCH], F32)
    psum = [es.enter_context(nc.psum_tensor("ps%d" % i, [128, 512], F32)) for i in range(8)]

    def PSH(b, h):
        return psum[b][:, h * 256:h * 256 + G], [("ps", b)]

    def PSB(b):
        return psum[b][:, :], [("ps", b)]

    DV = {}
    o = 0
    for nm, n in (("gs_m", KD), ("sh_m", KD), ("gt_m", KD), ("gs_f", KD), ("sh_f", KD), ("gt_f", KD),
                  ("gs_o", KD), ("sh_o", KD), ("lb", NH), ("oml", NH), ("noml", NH), ("sp", NH)):
        DV[nm] = o; o += n

    def dv(nm, i):
        return der[:, DV[nm] + i:DV[nm] + i + 1]

    def vc(off, i):
        return vec[:, off + i:off + i + 1]

    def act(out, in_, func, r, w, bias=None, scale=None, accum=None):
        kw = {}
        if bias is not None: kw["bias"] = bias
        if scale is not None: kw["scale"] = scale
        if accum is not None: kw["accum_out"] = accum
        return T.emit("act", lambda e: e.activation(out=out, in_=in_, func=func, **kw), r, w)

    def tt(out, in0, in1, op, r, w, eng="dve"):
        return T.emit(eng, lambda e: e.tensor_tensor(out=out, in0=in0, in1=in1, op=op), r, w)

    def ts(out, in0, s1, s2, op0, op1, r, w, eng="dve"):
        if s2 is None:
            return T.emit(eng, lambda e: e.tensor_scalar(out=out, in0=in0, scalar1=s1, scalar2=None, op0=op0), r, w)
        return T.emit(eng, lambda e: e.tensor_scalar(out=out, in0=in0, scalar1=s1, scalar2=s2, op0=op0, op1=op1), r, w)

    def stt(out, in0, s, in1, op0, op1, r, w):
        return T.emit("dve", lambda e: e.scalar_tensor_tensor(out=out, in0=in0, scalar=s, in1=in1, op0=op0, op1=op1), r, w)

    def cp(eng, out, in_, r, w):
        if eng == "act":
            return T.emit("act", lambda e: e.copy(out=out, in_=in_), r, w)
        return T.emit(eng, lambda e: e.tensor_copy(out=out, in_=in_), r, w)

    def mm(out, lhsT, rhs, start, stop, r, w, signal=True, sgc=False):
        return T.emit("pe", lambda e: e.matmul(out, lhsT=lhsT, rhs=rhs, start=start, stop=stop, skip_group_check=sgc), r, w, signal=signal)

    def dma(q, out, in_, r, w, dsem):
        return T.emit(q, lambda e: e.dma_start(out=out, in_=in_), r, w, dsem=dsem)

    def TM(i):
        return tmp[:, i, :], [("tmp", i)]

    def TB(i):
        return tb[:, i, :], [("tb", i)]

    import os
    KSTOP = float(os.environ.get("KSTOP", "99"))

    def chk(n):
        if KSTOP <= n:
            T.dead = True

    T.emit("pool", lambda e: e.memset(onesf[:], 1.0), [], ["onesf"])
    T.emit("pool", lambda e: e.affine_select(out=ident[:], in_=onesf[:], pattern=[[-1, 128]], compare_op=ALU.is_equal,
                                              fill=0.0, base=0, channel_multiplier=1), ["onesf"], ["ident"])
    T.emit("pool", lambda e: e.affine_select(out=cmask[:], in_=onesf[:], pattern=[[1, 128]], compare_op=ALU.is_ge,
                                              fill=0.0, base=0, channel_multiplier=-1), ["onesf"], ["cmask"])
    T.emit("pool", lambda e: e.memset(cmask[0:64, 64:128], 0.0), [], ["cmask"])
    T.emit("pool", lambda e: e.memset(smask[:], 1.0), [], ["smask"])
    for ch in range(NCH):
        T.emit("pool", lambda e, ch=ch: e.memset(smask[:, ch * 64:ch * 64 + 1], 0.0), [], ["smask"])
    T.emit("pool", lambda e: e.memset(zer[:], 0.0), [], ["zer"])
    T.emit("pool", lambda e: e.memset(S32[:], 0.0), [], ["S32"])
    T.emit("pool", lambda e: e.memset(Sbf[:], 0.0), [], ["Sbf"])
    T.emit("pool", lambda e: e.memset(tails[:], 0.0), [], ["tails"])
    dma("sp", vec[:], vec_d, [], ["vec"], "d_vec")
    act(cact[:], vec[:, c.V_C:c.V_C + KD], AF.Silu, ["vec"], ["cact"])

    chk(0)
    stg = tmp[:].rearrange("p a b -> p (a b)")[:, 0:D]
    stg_keys = [("tmp", i) for i in range(NTMP)]
    kst = tmp[:].rearrange("p a b -> p (a b)")[:, 0:2 * PH * 128]
    dma("sp", kst, keys_d.rearrange("p a n -> p (a n)"), [], stg_keys, "d_stg")
    cp("dve", keysb[:].rearrange("p a n -> p (a n)"), kst, stg_keys, ["keysb"])
    NCB = 8 * D // D
    mp, mpk = PSB(7)
    mm(mp, zer[:, 0:128], zer[:, 0:512], True, False, ["zer"], mpk, sgc=True)
    for k in range(KD):
        for cbk in range(NCB):
            dma("sp", stg, wada_d[k, :, cbk * D:(cbk + 1) * D], [], stg_keys, "d_stg")
            nj = D // 128
            for j in range(nj):
                col = cbk * nj + j
                mm(psum[7][:, col:col + 1], stg[:, j * 128:(j + 1) * 128], cact[:, k:k + 1], False, True,
                   stg_keys + ["cact"], mpk, signal=(j == nj - 1), sgc=True)
    tt(modT[:], psum[7][:, 0:8 * KD], vec[:, c.V_BADA:c.V_BADA + 8 * KD], ALU.add, mpk + ["vec"], ["modT"])
    def msl(s):
        return modT[:, s * KD:(s + 1) * KD]
    def dsl(nm, n=KD):
        return der[:, DV[nm]:DV[nm] + n]
    stt(dsl("gs_m"), msl(1), 1.0, vec[:, c.V_GMIX:c.V_GMIX + KD], ALU.add, ALU.mult, ["modT", "vec"], ["der"])
    stt(dsl("gs_f"), msl(4), 1.0, vec[:, c.V_GFFN:c.V_GFFN + KD], ALU.add, ALU.mult, ["modT", "vec"], ["der"])
    stt(dsl("gs_o"), msl(7), 1.0, vec[:, c.V_GOUT:c.V_GOUT + KD], ALU.add, ALU.mult, ["modT", "vec"], ["der"])
    for nm, s in (("sh_m", 0), ("gt_m", 2), ("sh_f", 3), ("gt_f", 5), ("sh_o", 6)):
        cp("dve", dsl(nm), msl(s), ["modT"], ["der"])
    tt(dsl("sp", NH), vec[:, c.V_LB0:c.V_LB0 + NH], vec[:, c.V_LB1:c.V_LB1 + NH], ALU.subtract, ["vec", "der"], ["der"])
    act(dsl("lb", NH), dsl("sp", NH), AF.Sigmoid, ["der"], ["der"])
    ts(dsl("oml", NH), dsl("lb", NH), -1.0, 1.0, ALU.mult, ALU.add, ["der"], ["der"])
    ts(dsl("noml", NH), dsl("oml", NH), -1.0, None, ALU.mult, None, ["der"], ["der"])

    chk(1)
    ghflat = GH[:].rearrange("p e a b c d -> p (e a b c d)")
    assert NT * PH * 4 * 128 >= 2 * D or True
    ncb = max(1, min(2, (NT * PH * 4 * 128) // D))
    for i in range(c.NTILES):
        dma("sp", stg, wall_d[i], [], stg_keys, "d_stg")
        s = i % ncb
        cb = ghflat[:, s * D:(s + 1) * D]
        eng = ("dve", "act", "pool")[i % 3]
        cp(eng, cb, stg, stg_keys, [("cb", s)])
        dma("pool", wbt(i), cb, [("cb", s)], [("wb", i)], "d_cbst%d" % s)
    chk(2)
    T.barrier()

    def group_order(halo=False):
        od = []
        for hh in range(NH):
            od += [("t", c.T_WIN + hh), ("t", c.T_WIN + NH + hh), ("t", c.T_WIN + 2 * NH + hh), ("t", c.T_WIN + 3 * NH + hh)]
        for ct in range(NCT):
            od += [("t", c.T_WIN + 4 * NH + ct), ("t", c.T_WIN + 4 * NH + NCT + ct)]
        if halo:
            return od
        od += [("t", c.T_WOUT + i) for i in range(KD)]
        od += [("t", c.T_WPQ + i) for i in range(c.NQT)]
        for sbi in range(c.NSB):
            od += [("t", c.T_UT + sbi * c.SBC + i) for i in range(c.SBC)]
            for dq in range(c.NDQ):
                od += [("v", sbi, dq, cg) for cg in range(c.SBC // c.VU)]
        return od

    order = (group_order(True) if c.HALO else []) + group_order() * c.NGL
    ws = {"issued": 0, "pos": 0}

    def ws_issue(j):
        it = order[j]
        slot = j % NB
        if it[0] == "t":
            dma("sp", wring[:, slot, :], wbt(it[1]), [], [("w", slot)], "d_w%d" % slot)
        else:
            _, sbi, dq, cg = it
            c0 = sbi * c.SBC + cg * c.VU
            W = c.DQW * 128
            src = wbs[3][c0:c0 + c.VU, :, dq * W:(dq + 1) * W].rearrange("c p w -> p c w")
            dst = wring[:, slot, 0:c.VU * W].rearrange("p (c w) -> p c w", c=c.VU)
            dma("sp", dst, src, [], [("w", slot)], "d_w%d" % slot)

    def ws_next(expect):
        j = ws["pos"]
        assert order[j] == expect, (order[j], expect)
        while ws["issued"] < min(len(order), j + NB):
            ws_issue(ws["issued"]); ws["issued"] += 1
        ws["pos"] += 1
        slot = j % NB
        return wring[:, slot, :], [("w", slot)]

    inv_sqrt_eps = None

    def rms_rstd(eps):
        pn, pnk = PSH(7, 0)
        for k in range(KD):
            sq, sqk = TM(k % 2)
            act(sq, xT[:, k, :], AF.Square, [("xT", k)], sqk)
            mm(pn, onesf[:], sq, k == 0, k == KD - 1, ["onesf"] + sqk, pnk, signal=(k % 2 == 1 or k == KD - 1))
        sd, sdk = TM(2)
        act(sd, pn, AF.Sqrt, pnk, sdk, bias=eps_ap, scale=1.0 / D)
        rs, rsk = TM(3)
        T.emit("dve", lambda e: e.reciprocal(out=rs, in_=sd), sdk, rsk)
        return rs, rsk

    def make_hT(gs, sh, rs, rsk):
        for k in range(KD):
            t, tkk = TM(4 + k % 2)
            tt(t, xT[:, k, :], rs, ALU.mult, [("xT", k)] + rsk, tkk)
            act(hT[:, k, :], t, AF.Identity, tkk + ["der"], [("hT", k)], bias=dv(sh, k), scale=dv(gs, k))

    def proj(wt, wtk, rhs_tile, rhs_key, out, outk):
        for k in range(KD):
            mm(out, wt[:, k * 128:(k + 1) * 128], rhs_tile[:, k, :], k == 0, k == KD - 1,
               wtk + [(rhs_key, k)], outk, signal=(k == KD - 1))

    eps_t = sb("eps_t", [128, 2], F32)
    T.emit("pool", lambda e: e.memset(eps_t[:, 0:1], 1e-6), [], ["eps"])
    T.emit("pool", lambda e: e.memset(eps_t[:, 1:2], 1e-5), [], ["eps"])
    eps_ap = eps_t[:, 0:1]
    lneps_ap = eps_t[:, 1:2]

    proj_slots = [(0, 0), (1, 0), (5, 0), (6, 0)]
    pslot = {"i": 0}

    def next_pslot():
        b, h = proj_slots[pslot["i"] % len(proj_slots)]
        pslot["i"] += 1
        return PSH(b, h)

    for g in range(c.NGP):
        halo = (c.HALO == 1 and g == 0)
        xk = [("xT", k) for k in range(KD)]
        dma("pool", xT[:], xT_d[g], [], xk, "d_x")
        rs, rsk = rms_rstd(1e-6)
        make_hT("gs_m", "sh_m", rs, rsk)
        chk(3)
        for hh in range(NH):
            pq, pqk = next_pslot(); pf, pfk = next_pslot(); pi, pik = next_pslot(); pg, pgk = next_pslot()
            for (po, pok, tix) in ((pq, pqk, hh), (pf, pfk, NH + hh), (pi, pik, 2 * NH + hh), (pg, pgk, 3 * NH + hh)):
                wt, wtk = ws_next(("t", c.T_WIN + tix))
                proj(wt, wtk, hT, "hT", po, pok)
            q, qk = TM(6); sg, sgk = TM(7); sgg, sggk = TM(8)
            act(q, pq, AF.Silu, pqk, qk)
            act(sgg, pg, AF.Silu, pgk, sggk)
            act(sg, pf, AF.Sigmoid, pfk, sgk)
            vT, vTk = TB(0)
            cp("act", vT, pi, pik, vTk)
            chk(3.1)
            f, fk = TM(9); kk, kkk = TM(10)
            ts(f, sg, dv("oml", hh), dv("lb", hh), ALU.mult, ALU.add, sgk + ["der"], fk)
            ts(kk, sg, dv("noml", hh), dv("oml", hh), ALU.mult, ALU.add, sgk + ["der"], kkk)
            lf, lfk = TM(11)
            act(lf, f, AF.Ln, fk, lfk)
            b, bk = TM(12)
            T.emit("dve", lambda e, b=b, lf=lf: e.tensor_tensor_scan(out=b, data0=smask[:], data1=lf, initial=0.0,
                                                                      op0=ALU.mult, op1=ALU.add), lfk + ["smask"], bk)
            chk(3.2)
            eb, ebk = TM(13); enb, enbk = TM(14)
            act(eb, b, AF.Exp, bk, ebk)
            act(enb, b, AF.Exp, bk, enbk, scale=-1.0)
            dc = dch[:, hh % 2, :]; dck = [("dch", hh % 2)]
            blast = b.rearrange("p (c s) -> p c s", s=64)[:, :, 63]
            act(dc, blast, AF.Exp, bk, dck)
            Qt, Qtk = TB(1); Ktb, Ktbk = TB(2); Kh, Khk = TB(3)
            tt(Qt, q, eb, ALU.mult, qk + ebk, Qtk)
            K32, K32k = TM(15)
            tt(K32, kk, enb, ALU.mult, kkk + enbk, K32k)
            cp("act", Ktb, K32, K32k, Ktbk)
            tt(Kh.rearrange("p (c s) -> p c s", s=64), K32.rearrange("p (c s) -> p c s", s=64),
               dc.unsqueeze(2).broadcast_to([128, NCH, 64]), ALU.mult, K32k + dck, Khk)
            chk(3.3)
            psc, psck = PSH(2, 0)
            for tb_ in range(NT):
                mm(psc[:, tb_ * 128:(tb_ + 1) * 128], Ktb[:, tb_ * 128:(tb_ + 1) * 128], Qt[:, tb_ * 128:(tb_ + 1) * 128],
                   True, True, Ktbk + Qtk, psck, signal=(tb_ == NT - 1))
            scm, scmk = TB(4)
            tt(scm.rearrange("p (a t) -> p a t", a=NT), psc.rearrange("p (a t) -> p a t", a=NT),
               cmask[:].unsqueeze(1).broadcast_to([128, NT, 128]), ALU.mult, psck + ["cmask"], scmk)
            chk(3.4)
            pv, pvk = PSH(3, 0); pk, pkk = PSH(4, 0)
            for tb_ in range(NT):
                mm(pv[:, tb_ * 128:(tb_ + 1) * 128], vT[:, tb_ * 128:(tb_ + 1) * 128], ident[:], True, True,
                   vTk + ["ident"], pvk, signal=(tb_ == NT - 1))
            chk(3.41)
            for tb_ in range(NT):
                mm(pk[:, tb_ * 128:(tb_ + 1) * 128], Kh[:, tb_ * 128:(tb_ + 1) * 128], ident[:], True, True,
                   Khk + ["ident"], pkk, signal=(tb_ == NT - 1))
            chk(3.42)
            Vt, Vtk = TB(5); Kt, Ktk = TB(6)
            cp("act", Vt, pv, pvk, Vtk)
            chk(3.43)
            ts(Kt, pk, 1.0, None, ALU.mult, None, pkk, Ktk)
            chk(3.5)
            po_, pok_ = PSH(7, 0)
            for tb_ in range(NT):
                mm(po_[:, tb_ * 128:(tb_ + 1) * 128], Vt[:, tb_ * 128:(tb_ + 1) * 128], scm[:, tb_ * 128:(tb_ + 1) * 128],
                   tb_ == 0, False, Vtk + scmk, pok_, signal=False, sgc=True)
            pss, pssk = PSH(2, 0)
            skey = [("S", hh)]
            for ch in range(NCH):
                tb_ = ch // 2; p0 = (ch % 2) * 64
                mm(po_[:, ch * 64:(ch + 1) * 64], Sbf[:, hh, :], Qt[:, ch * 64:(ch + 1) * 64], False, True,
                   [("Sb", hh)] + Qtk, pok_, signal=True, sgc=True)
                mm(pss[:, 0:128], Kt[p0:p0 + 64, tb_ * 128:(tb_ + 1) * 128], Vt[p0:p0 + 64, tb_ * 128:(tb_ + 1) * 128],
                   True, True, Ktk + Vtk, pssk)
                stt(S32[:, hh, :], S32[:, hh, :], dc[:, ch:ch + 1], pss[:, 0:128], ALU.mult, ALU.add,
                    skey + dck + pssk, skey)
                cp("act", Sbf[:, hh, :], S32[:, hh, :], skey, [("Sb", hh)])
            chk(3.6)
            osq, osqk = TM(16)
            act(osq, po_, AF.Square, pok_, osqk)
            pn, pnk = PSH(3, 0)
            mm(pn, onesf[:], osq, True, True, ["onesf"] + osqk, pnk)
            sd, sdk = TM(17)
            act(sd, pn, AF.Sqrt, pnk, sdk, bias=eps_ap, scale=1.0 / 128)
            ri, rik = TM(16)
            T.emit("dve", lambda e, ri=ri, sd=sd: e.reciprocal(out=ri, in_=sd), sdk, rik)
            t1, t1k = TM(17)
            tt(t1, po_, ri, ALU.mult, pok_ + rik, t1k)
            stt(ycat[:, hh, :], t1, vc(c.V_HGN, hh), sgg, ALU.mult, ALU.mult, t1k + sggk + ["vec"], [("yc", hh)])
        chk(4)
        pl1, pl1k = PSH(2, 0); pl2, pl2k = PSH(3, 0)
        for ct in range(NCT):
            pa, pak = next_pslot(); pb, pbk = next_pslot()
            wt, wtk = ws_next(("t", c.T_WIN + 4 * NH + ct)); proj(wt, wtk, hT, "hT", pa, pak)
            wt, wtk = ws_next(("t", c.T_WIN + 4 * NH + NCT + ct)); proj(wt, wtk, hT, "hT", pb, pbk)
            sgb, sgbk = TM(6 + ct % 2)
            act(sgb, pb, AF.Sigmoid, pbk + ["vec"], sgbk, bias=vc(c.V_BGB, ct))
            ub = ubuf[:, ct % 2, :]; ubk = [("ub", ct % 2)]
            cp("pool", ub[:, 0:30], tails[:, ct, :], [("tl", ct)], ubk)
            stt(ub[:, 30:30 + G], pa, vc(c.V_BGA, ct), sgb, ALU.add, ALU.mult, pak + sgbk + ["vec"], ubk)
            cp("pool", tails[:, ct, :], ub[:, G:G + 30], ubk, [("tl", ct)])
            acc, acck = TM(8 + ct % 2)
            ts(acc, ub[:, 0:G], vc(c.V_WDW, ct * 31), vc(c.V_BDW, ct), ALU.mult, ALU.add, ubk + ["vec"], acck)
            for j in range(1, 31):
                stt(acc, ub[:, j:j + G], vc(c.V_WDW, ct * 31 + j), acc, ALU.mult, ALU.add, ubk + acck + ["vec"], acck)
            sq, sqk = TM(10 + ct % 2)
            act(sq, acc, AF.Square, acck, sqk)
            mm(pl1, onesf[:], acc, ct == 0, ct == NCT - 1, ["onesf"] + acck, pl1k)
            mm(pl2, onesf[:], sq, ct == 0, ct == NCT - 1, ["onesf"] + sqk, pl2k)
            cp("act", ycat[:, NH + ct, :], acc, acck, [("yc", NH + ct)])
        if halo:
            hm = vec[:, c.V_HM:c.V_HM + 1]
            allS = [("S", h_) for h_ in range(NH)]; allSb = [("Sb", h_) for h_ in range(NH)]; allT = [("tl", t_) for t_ in range(NCT)]
            ts(S32[:].rearrange("p a b -> p (a b)"), S32[:].rearrange("p a b -> p (a b)"), hm, None, ALU.mult, None, allS + ["vec"], allS)
            ts(Sbf[:].rearrange("p a b -> p (a b)"), Sbf[:].rearrange("p a b -> p (a b)"), hm, None, ALU.mult, None, allSb + ["vec"], allSb)
            ts(tails[:].rearrange("p a b -> p (a b)"), tails[:].rearrange("p a b -> p (a b)"), hm, None, ALU.mult, None, allT + ["vec"], allT)
            continue
        mean, meank = TM(12); msq, msqk = TM(13); var, vark = TM(14); lsd, lsdk = TM(15); lrs, lrsk = TM(16)
        act(mean, pl1, AF.Identity, pl1k, meank, scale=1.0 / c.CW)
        tt(msq, mean, mean, ALU.mult, meank, msqk)
        stt(var, pl2, 1.0 / c.CW, msq, ALU.mult, ALU.subtract, pl2k + msqk, vark)
        act(lsd, var, AF.Sqrt, vark, lsdk, bias=lneps_ap, scale=1.0)
        T.emit("dve", lambda e, lrs=lrs, lsd=lsd: e.reciprocal(out=lrs, in_=lsd), lsdk, lrsk)
        for ct in range(NCT):
            t, tk_ = TM(6 + ct % 2); t2, t2k = TM(8 + ct % 2)
            tt(t, ycat[:, NH + ct, :], mean, ALU.subtract, [("yc", NH + ct)] + meank, tk_)
            tt(t2, t, lrs, ALU.mult, tk_ + lrsk, t2k)
            act(ycat[:, NH + ct, :], t2, AF.Silu, t2k + ["vec"], [("yc", NH + ct)], bias=vc(c.V_LNB, ct), scale=vc(c.V_LNG, ct))
        chk(5)
        for dt in range(KD):
            wt, wtk = ws_next(("t", c.T_WOUT + dt))
            po2, po2k = next_pslot()
            proj(wt, wtk, ycat, "yc", po2, po2k)
            stt(xT[:, dt, :], po2, dv("gt_m", dt), xT[:, dt, :], ALU.mult, ALU.add, po2k + ["der", ("xT", dt)], [("xT", dt)])
        chk(6)
        rs, rsk = rms_rstd(1e-6)
        make_hT("gs_f", "sh_f", rs, rsk)
        qTt = PT
        for jt in range(c.NQT):
            wt, wtk = ws_next(("t", c.T_WPQ + jt))
            pq2, pq2k = next_pslot()
            proj(wt, wtk, hT, "hT", pq2, pq2k)
            cp("act", qTt[:, jt, :], pq2, pq2k, [("PT", jt)])
        chk(7)
        for tt_ in range(NT):
            for h in range(PH):
                b_, hf = [(0, 0), (1, 0), (2, 0), (3, 0)][h % 4]
                pS, pSk = PSH(b_, hf)
                for half in range(2):
                    mm(pS[:, half * 128:(half + 1) * 128], qTt[:, 2 * h + half, tt_ * 128:(tt_ + 1) * 128], keysb[:, 2 * h + half, :],
                       True, True, [("PT", 2 * h + half), "keysb"], pSk, signal=(half == 1))
                skey2 = [("s12", tt_, h)]
                cp("act", s12[:, tt_, h, :, :].rearrange("p a n -> p (a n)"), pS, pSk, skey2)
                for half in range(2):
                    sv = s12[:, tt_, h, half, :]
                    v16 = tk[:, half, :]
                    T.emit("dve", lambda e, sv=sv, v16=v16: e.max(out=v16[:, 0:8], in_=sv), skey2, [("tk", half)])
                    wv = tkw[:, 0, 0:128]
                    T.emit("dve", lambda e, sv=sv, v16=v16, wv=wv: e.match_replace(out=wv, in_to_replace=v16[:, 0:8], in_values=sv, imm_value=NEG),
                           skey2 + [("tk", half)], [("tkw", 0)])
                    T.emit("dve", lambda e, v16=v16, wv=wv: e.max(out=v16[:, 8:16], in_=wv), [("tkw", 0)], [("tk", half)])
                cand = tkw[:, 1, :]
                tt(cand.rearrange("p (a b) -> p a b", a=16), tk[:, 0, :].unsqueeze(2).broadcast_to([128, 16, 16]),
                   tk[:, 1, :].unsqueeze(1).broadcast_to([128, 16, 16]), ALU.add, [("tk", 0), ("tk", 1)], [("tkw", 1)], eng="pool")
                tops = tk[:, 2, :]
                T.emit("dve", lambda e, tops=tops, cand=cand: e.max(out=tops[:, 0:8], in_=cand), [("tkw", 1)], [("tk", 2)])
                cw = tkw[:, 0, :]
                T.emit("dve", lambda e, tops=tops, cand=cand, cw=cw: e.match_replace(out=cw, in_to_replace=tops[:, 0:8], in_values=cand, imm_value=NEG),
                       [("tkw", 1), ("tk", 2)], [("tkw", 0)])
                T.emit("dve", lambda e, tops=tops, cw=cw: e.max(out=tops[:, 8:16], in_=cw), [("tkw", 0)], [("tk", 2)])
                cp("dve", tau[:, tt_, h:h + 1], tops[:, 15:16], [("tk", 2)], [("tau", tt_, h)])
                negm = tk[:, 3, 0:1]; Z = tk[:, 3, 1:2]; lnZ = tk[:, 3, 2:3]; exs = tk[:, 4, :]
                ts(negm, tops[:, 0:1], -1.0, None, ALU.mult, None, [("tk", 2)], [("tk", 3)])
                act(exs, tops, AF.Exp, [("tk", 2), ("tk", 3)], [("tk", 4), ("tk", 3)], bias=negm, accum=Z)
                act(lnZ, Z, AF.Ln, [("tk", 3)], [("tk", 3)])
                tt(nbias[:, tt_, h:h + 1], negm, lnZ, ALU.subtract, [("tk", 3)], [("nb", tt_, h)])
        chk(8)
        acc_slots = [(0, 0), (1, 0), (2, 0), (3, 0)]
        for sbi in range(c.NSB):
            for blk in range(c.SBC // 2):
                c0 = sbi * c.SBC + blk * 2
                gb = blk % 2
                for tt_ in range(NT):
                    for h in range(PH):
                        i_ = (tt_ * PH + h) % 4
                        val, valk = TM(i_)
                        Wv, Wk = TM(4 + i_)
                        tt(val.rearrange("p (a n) -> p a n", a=2),
                           s12[:, tt_, h, 0, c0:c0 + 2].unsqueeze(2).broadcast_to([128, 2, 128]),
                           s12[:, tt_, h, 1, :].unsqueeze(1).broadcast_to([128, 2, 128]), ALU.add,
                           [("s12", tt_, h)], valk, eng="pool")
                        act(Wv, val, AF.Exp, valk + [("nb", tt_, h)], Wk, bias=nbias[:, tt_, h:h + 1])
                        stt(GH[:, gb, tt_, h, :, :].rearrange("p a n -> p (a n)"), val, tau[:, tt_, h:h + 1], Wv, ALU.is_ge, ALU.mult,
                            valk + Wk + [("tau", tt_, h)], [("GH", gb, tt_, h)])
                for ci in range(2):
                    ch_ = c0 + ci
                    cc = blk * 2 + ci
                    ut, utk = ws_next(("t", c.T_UT + ch_))
                    pA, pAk = PSH(4 + 2 * (cc % 2), 0)
                    proj(ut, utk, hT, "hT", pA, pAk)
                    pG, pGk = PSH(5 + 2 * (cc % 2), 0)
                    for tt_ in range(NT):
                        for h in range(PH):
                            mm(pG[:, tt_ * 128:(tt_ + 1) * 128], GH[:, gb, tt_, h, ci, :], ident[:], h == 0, h == PH - 1,
                               [("GH", gb, tt_, h), "ident"], pGk, signal=(h == PH - 1 and tt_ == NT - 1))
                    ge, gek = TM(8 + cc % 2)
                    act(ge, pA, AF.Gelu, pAk, gek)
                    tt(PT[:, cc, :], ge, pG, ALU.mult, gek + pGk, [("PT", cc)])
            for dq in range(c.NDQ):
                nb_ = c.DQW
                for bnk in range(nb_):
                    pz, pzk = PSB(bnk)
                    mm(pz, zer[:, 0:128], zer[:, 0:512], True, False, ["zer"], pzk, sgc=True)
                for cg in range(c.SBC // c.VU):
                    vb, vbk = ws_next(("v", sbi, dq, cg))
                    W = c.DQW * 128
                    for ci in range(c.VU):
                        cc = cg * c.VU + ci
                        for dtl in range(c.DQW):
                            pa2, pa2k = PSH(*acc_slots[dtl])
                            mm(pa2, vb[:, ci * W + dtl * 128:ci * W + (dtl + 1) * 128], PT[:, cc, :], False, True,
                               vbk + [("PT", cc)], pa2k, signal=(cc == c.SBC - 1 or (ci == c.VU - 1 and dtl == c.DQW - 1)), sgc=True)
                for dtl in range(c.DQW):
                    dt = dq * c.DQW + dtl
                    pa2, pa2k = PSH(*acc_slots[dtl])
                    stt(xT[:, dt, :], pa2, dv("gt_f", dt), xT[:, dt, :], ALU.mult, ALU.add,
                        pa2k + ["der", ("xT", dt)], [("xT", dt)])
        chk(9)
        rs, rsk = rms_rstd(1e-6)
        for k4 in range(0, KD, 2):
            ob = obuf[:, (k4 // 2) % 2, :, :]; obk = [("ob", (k4 // 2) % 2)]
            for k in range(k4, min(KD, k4 + 2)):
                t, tkk = TM(4 + k % 2)
                tt(t, xT[:, k, :], rs, ALU.mult, [("xT", k)] + rsk, tkk)
                act(ob[:, k - k4, :], t, AF.Identity, tkk + ["der"], obk, bias=dv("sh_o", k), scale=dv("gs_o", k))
            n = min(KD, k4 + 2) - k4
            dma("pool", out_d[g - c.HALO, :, k4:k4 + n, :], ob[:, 0:n, :], obk, [("out", g, k4)], "d_o%d" % ((k4 // 2) % 2))
    T.dead = False
    T.final_wait("sp")
    T.replay(nc, es)
    es.close()
    return nc


def host_prepare(cfg, x, c, w_ada, b_ada, w_ada_out, b_ada_out, g_mix, g_ffn, g_out, w_in, b_glu,
                 lb_logits, w_dw, b_dw, ln_g, ln_b, hg_norm_g, w_out, w_pq, sub_keys1, sub_keys2,
                 expert_u, expert_v):
    cf = cfg
    D, KD, G = cf.D, cf.KD, cf.G
    f = lambda a: np.asarray(a, dtype=np.float32)
    x = f(x)[0]
    xTall = x.reshape(cf.NG, G, KD, 128).transpose(0, 3, 2, 1)

    def coltiles(w):
        K, N = w.shape
        return w.reshape(K // 128, 128, N // 128, 128).transpose(2, 1, 0, 3).reshape(N // 128, 128, K)

    wall = np.empty((cf.NTILES, 128, D), np.float32)
    wall[cf.T_WIN:cf.T_WIN + cf.NCOL] = coltiles(f(w_in)[0])
    wall[cf.T_WOUT:cf.T_WOUT + KD] = coltiles(f(w_out)[0])
    wall[cf.T_WPQ:cf.T_WPQ + cf.NQT] = coltiles(f(w_pq)[0])
    u = f(expert_u)[0]
    wall[cf.T_UT:cf.T_UT + cf.NEC] = u.reshape(cf.NEC, 128, KD, 128).transpose(0, 3, 2, 1).reshape(cf.NEC, 128, D)
    wall[cf.T_V:cf.T_V + cf.NEC] = f(expert_v)[0].reshape(cf.NEC, 128, D)
    wcat = np.concatenate([f(w_ada)[0], f(w_ada_out)], axis=1)
    wada = np.ascontiguousarray(wcat.reshape(KD, 128, 8 * D))
    vec = np.zeros((128, cf.NV), np.float32)

    def put(off, v, n):
        vec[:, off:off + n] = v.reshape(n, 128).T

    put(cf.V_C, f(c)[0], KD)
    put(cf.V_BADA, np.concatenate([f(b_ada)[0], f(b_ada_out)]), 8 * KD)
    put(cf.V_GMIX, f(g_mix)[0], KD); put(cf.V_GFFN, f(g_ffn)[0], KD); put(cf.V_GOUT, f(g_out), KD)
    bg = f(b_glu)[0]
    put(cf.V_BGA, bg[:cf.CW], cf.NCT); put(cf.V_BGB, bg[cf.CW:], cf.NCT)
    lbl = f(lb_logits)
    put(cf.V_LB0, lbl[0], cf.NH); put(cf.V_LB1, lbl[1], cf.NH)
    wd = f(w_dw)[0]
    vec[:, cf.V_WDW:cf.V_WDW + cf.NCT * 31] = wd.reshape(31, cf.NCT, 128).transpose(2, 1, 0).reshape(128, cf.NCT * 31)
    put(cf.V_BDW, f(b_dw)[0], cf.NCT); put(cf.V_LNG, f(ln_g)[0], cf.NCT); put(cf.V_LNB, f(ln_b)[0], cf.NCT)
    put(cf.V_HGN, f(hg_norm_g)[0], cf.NH)
    k1 = f(sub_keys1)[0]; k2 = f(sub_keys2)[0]
    ks = np.stack([k1, k2], axis=1).reshape(2 * cf.PH, 128, 128)
    keysT = np.ascontiguousarray(ks.transpose(2, 0, 1))
    maps = []
    for r in range(cf.NCORES):
        g0 = r * cf.NGL
        xr = np.zeros((cf.NGP, 128, KD, G), np.float32)
        if cf.HALO and r > 0:
            xr[0] = xTall[g0 - 1]
        xr[cf.HALO:] = xTall[g0:g0 + cf.NGL]
        vr = vec.copy()
        vr[:, cf.V_HM] = 0.0 if r == 0 else 1.0
        maps.append({"xT": xr, "wall": wall, "wada": wada, "vec": vr, "keysT": keysT})
    return maps


def run(cfg, inputs):
    nc = build(cfg)
    maps = host_prepare(cfg, **inputs)
    res = run_bass_kernel_spmd(nc, maps, core_ids=list(range(cfg.NCORES)))
    oT = np.concatenate([res.results[r]["outT"] for r in range(cfg.NCORES)], axis=0)
    out = oT.transpose(0, 3, 2, 1).reshape(1, cfg.SEQ, cfg.D)
    return np.ascontiguousarray(out.astype(np.float32))


def kernel(**inputs):
    return run(Cfg(), inputs)
```

```python
import numpy as np
from contextlib import ExitStack
import concourse.bass as bass
import concourse.mybir as mybir
from concourse.bass_utils import run_bass_kernel_spmd

F32 = mybir.dt.float32
BF16 = mybir.dt.bfloat16
AF = mybir.ActivationFunctionType
ALU = mybir.AluOpType
NEG = -1.0e30


class Cfg:
    def __init__(self, D=4096, SEQ=16384, PH=8, G=256, NCORES=8):
        self.D = D; self.SEQ = SEQ; self.PH = PH; self.G = G; self.NCORES = NCORES
        self.KD = D // 128
        self.NH = (D // 2) // 128
        self.NCT = (D // 2) // 128
        self.CW = D // 2
        self.NCOL = 3 * D // 128
        self.NQT = 2 * PH
        self.NEC = 128
        self.SBC = 32
        self.NSB = self.NEC // self.SBC
        self.NT = G // 128
        self.NCH = G // 64
        self.NG = SEQ // G
        self.NGL = self.NG // NCORES
        self.HALO = 1 if NCORES > 1 else 0
        self.NGP = self.NGL + self.HALO
        self.DQW = min(4, self.KD)
        self.NDQ = self.KD // self.DQW
        self.VU = D // (self.DQW * 128)
        self.T_WIN = 0
        self.T_WOUT = self.T_WIN + self.NCOL
        self.T_WPQ = self.T_WOUT + self.KD
        self.T_UT = self.T_WPQ + self.NQT
        self.T_V = self.T_UT + self.NEC
        self.NTILES = self.T_V + self.NEC
        o = 0
        def take(n):
            nonlocal o
            r = o; o += n; return r
        KD, NH, NCT = self.KD, self.NH, self.NCT
        self.V_C = take(KD); self.V_BADA = take(8 * KD)
        self.V_GMIX = take(KD); self.V_GFFN = take(KD); self.V_GOUT = take(KD)
        self.V_BGA = take(NCT); self.V_BGB = take(NCT)
        self.V_LB0 = take(NH); self.V_LB1 = take(NH)
        self.V_WDW = take(NCT * 31); self.V_BDW = take(NCT)
        self.V_LNG = take(NCT); self.V_LNB = take(NCT); self.V_HGN = take(NH); self.V_HM = take(1)
        self.NV = o


class Tracker:
    EPOCH = 60000

    def __init__(self):
        self.streams = {n: [] for n in ("pe", "act", "dve", "pool", "sp")}
        self.cnt = {n: 0 for n in self.streams}
        self.epoch = {n: 0 for n in self.streams}
        self.waited = {n: {} for n in self.streams}
        self.keys = {}
        self.dcnt = {}
        self.semids = set()
        self.last_ev = {}
        self.pending = {n: False for n in self.streams}

    def _need(self, eng, ev, needs):
        if ev is None:
            return
        sid, val = ev
        if self.waited[eng].get(sid, 0) >= val:
            return
        if needs.get(sid, 0) < val:
            needs[sid] = val

    dead = False

    def emit(self, eng, fn, reads=(), writes=(), signal=True, dsem=None):
        if self.dead:
            return None
        needs = {}
        if dsem is None and self.cnt[eng] >= 55000 and not self.pending[eng]:
            self.epoch[eng] += 1; self.cnt[eng] = 0
        my_sid = (eng, self.epoch[eng])
        excl = my_sid if eng == "pe" else None
        for k in reads:
            st = self.keys.get(k)
            if st is not None:
                self._need(eng, st[0], needs)
        for k in writes:
            st = self.keys.get(k)
            if st is not None:
                lw = st[0]
                if lw is not None and lw[0] != excl:
                    self._need(eng, lw, needs)
                for sid, val in st[1].items():
                    if sid != excl:
                        self._need(eng, (sid, val), needs)
        waits = []
        for sid, val in needs.items():
            if sid[0] == eng and sid[1] == self.epoch[eng]:
                assert val <= self.cnt[eng], ("wait on future own milestone", eng, val, self.cnt[eng])
            elif sid[0] in self.cnt and sid[1] == self.epoch[sid[0]]:
                assert val <= self.cnt[sid[0]], ("wait on future milestone", eng, sid, val)
            waits.append((sid, val))
            self.waited[eng][sid] = val
        if dsem is not None:
            ep, c = self.dcnt.get(dsem, (0, 0))
            if c + 16 > self.EPOCH:
                ep += 1; c = 0
            c += 16
            self.dcnt[dsem] = (ep, c)
            sid = ("dma", dsem, ep)
            ev = (sid, c)
            inc = (sid, 16)
        elif signal:
            assert self.cnt[eng] + 1 <= self.EPOCH
            self.pending[eng] = False
            self.cnt[eng] += 1
            ev = (my_sid, self.cnt[eng])
            inc = (my_sid, 1)
        else:
            ev = (my_sid, self.cnt[eng] + 1)
            self.pending[eng] = True
            assert self.cnt[eng] + 1 <= self.EPOCH
            inc = None
        self.semids.add(ev[0])
        self.last_ev[ev[0]] = max(self.last_ev.get(ev[0], 0), ev[1])
        for k in reads:
            if isinstance(k, tuple) and k[0] == "psbw":
                continue
            st = self.keys.setdefault(k, [None, {}])
            st[1][ev[0]] = max(st[1].get(ev[0], 0), ev[1])
        for k in writes:
            self.keys[k] = [ev, {}]
        self.streams[eng].append((waits, fn, inc))
        return ev

    def barrier(self):
        evs = dict(self.last_ev)
        for eng in self.streams:
            waits = []
            for sid, val in evs.items():
                if sid[0] == eng:
                    continue
                if self.waited[eng].get(sid, 0) < val:
                    waits.append((sid, val)); self.waited[eng][sid] = val
            if waits:
                self.streams[eng].append((waits, None, None))
        self.keys = {}

    def final_wait(self, eng):
        waits = []
        for sid, val in self.last_ev.items():
            if sid[0] == eng:
                continue
            if self.waited[eng].get(sid, 0) < val:
                waits.append((sid, val)); self.waited[eng][sid] = val
        self.streams[eng].append((waits, None, None))

    def replay(self, nc, es):
        sems = {}
        for i, sid in enumerate(sorted(self.semids, key=str)):
            sems[sid] = es.enter_context(nc.semaphore("s%d" % i))
        blk = es.enter_context(nc.Block())
        T = self

        def run(name, e):
            for waits, fn, inc in T.streams[name]:
                for sid, val in waits:
                    e.wait_ge(sems[sid], val)
                if fn is not None:
                    ins = fn(e)
                    if inc is not None:
                        ins.then_inc(sems[inc[0]], inc[1])

        @blk.sync
        def _(e): run("sp", e)

        @blk.gpsimd
        def _(e): run("pool", e)

        @blk.scalar
        def _(e): run("act", e)

        @blk.vector
        def _(e): run("dve", e)

        @blk.tensor
        def _(e): run("pe", e)


def build(cfg):
    c = cfg
    D, KD, NH, NCT, G, NT, NCH, PH = c.D, c.KD, c.NH, c.NCT, c.G, c.NT, c.NCH, c.PH
    nc = bass.Bass("TRN2", target_bir_lowering=False)
    xT_d = nc.dram_tensor("xT", [c.NGP, 128, KD, G], F32, kind="ExternalInput").ap()
    wall_d = nc.dram_tensor("wall", [c.NTILES, 128, D], F32, kind="ExternalInput").ap()
    wada_d = nc.dram_tensor("wada", [KD, 128, 8 * D], F32, kind="ExternalInput").ap()
    vec_d = nc.dram_tensor("vec", [128, c.NV], F32, kind="ExternalInput").ap()
    keys_d = nc.dram_tensor("keysT", [128, 2 * PH, 128], F32, kind="ExternalInput").ap()
    out_d = nc.dram_tensor("outT", [c.NGL, 128, KD, G], F32, kind="ExternalOutput").ap()
    segs = [(c.T_WIN, c.T_WOUT), (c.T_WOUT, c.T_UT), (c.T_UT, c.T_V), (c.T_V, c.NTILES)]
    wbs = [nc.dram_tensor("wb%d" % j, [b_ - a_, 128, D], BF16, kind="Internal").ap() for j, (a_, b_) in enumerate(segs)]

    def wbt(i):
        for j, (a_, b_) in enumerate(segs):
            if a_ <= i < b_:
                return wbs[j][i - a_]
        raise IndexError(i)

    T = Tracker()
    es = ExitStack()

    def sb(name, shape, dt):
        return es.enter_context(nc.sbuf_tensor(name, shape, dt))

    xT = sb("xT_sb", [128, KD, G], F32)
    hT = sb("hT_sb", [128, KD, G], BF16)
    ycat = sb("ycat", [128, KD, G], BF16)
    PT = sb("PT", [128, c.SBC, G], BF16)
    NB = 3
    wring = sb("wring", [128, NB, D], BF16)
    GH = sb("GH", [128, 2, NT, PH, 2, 128], BF16)
    NTMP = 18
    tmp = sb("tmp", [128, NTMP, G], F32)
    tb = sb("tb16", [128, 12, G], BF16)
    s12 = sb("s12", [128, NT, PH, 2, 128], F32)
    S32 = sb("S32", [128, NH, 128], F32)
    Sbf = sb("Sbf", [128, NH, 128], BF16)
    ubuf = sb("ubuf", [128, 2, 30 + G], F32)
    tails = sb("tails", [128, NCT, 30], F32)
    vec = sb("vec_sb", [128, c.NV], F32)
    modT = sb("modT", [128, 8 * KD], F32)
    der = sb("der", [128, 8 * KD + 4 * NH], F32)
    keysb = sb("keysb", [128, 2 * PH, 128], BF16)
    ident = sb("ident", [128, 128], BF16)
    onesf = sb("onesf", [128, 128], F32)
    cmask = sb("cmask", [128, 128], F32)
    smask = sb("smask", [128, G], F32)
    zer = sb("zer", [128, 512], BF16)
    cact = sb("cact", [128, KD], F32)
    obuf = sb("obuf", [128, 2, 2, G], F32)
    tk = sb("tk", [128, 8, 16], F32)
    tkw = sb("tkw", [128, 2, 256], F32)
    tau = sb("tau", [128, NT, PH], F32)
    nbias = sb("nbias", [128, NT, PH], F32)
    dch = sb("dch", [128, 2, NCH], F32)
    psum = [es.enter_context(nc.psum_tensor("ps%d" % i, [128, 512], F32)) for i in range(8)]

    def PSH(b, h):
        return psum[b][:, h * 256:h * 256 + G], [("ps", b)]

    def PSB(b):
        return psum[b][:, :], [("ps", b)]

    DV = {}
    o = 0
    for nm, n in (("gs_m", KD), ("sh_m", KD), ("gt_m", KD), ("gs_f", KD), ("sh_f", KD), ("gt_f", KD),
                  ("gs_o", KD), ("sh_o", KD), ("lb", NH), ("oml", NH), ("noml", NH), ("sp", NH)):
        DV[nm] = o; o += n

    def dv(nm, i):
        return der[:, DV[nm] + i:DV[nm] + i + 1]

    def vc(off, i):
        return vec[:, off + i:off + i + 1]

    def act(out, in_, func, r, w, bias=None, scale=None, accum=None):
        kw = {}
        if bias is not None: kw["bias"] = bias
        if scale is not None: kw["scale"] = scale
        if accum is not None: kw["accum_out"] = accum
        return T.emit("act", lambda e: e.activation(out=out, in_=in_, func=func, **kw), r, w)

    def tt(out, in0, in1, op, r, w, eng="dve"):
        return T.emit(eng, lambda e: e.tensor_tensor(out=out, in0=in0, in1=in1, op=op), r, w)

    def ts(out, in0, s1, s2, op0, op1, r, w, eng="dve"):
        if s2 is None:
            return T.emit(eng, lambda e: e.tensor_scalar(out=out, in0=in0, scalar1=s1, scalar2=None, op0=op0), r, w)
        return T.emit(eng, lambda e: e.tensor_scalar(out=out, in0=in0, scalar1=s1, scalar2=s2, op0=op0, op1=op1), r, w)

    def stt(out, in0, s, in1, op0, op1, r, w):
        return T.emit("dve", lambda e: e.scalar_tensor_tensor(out=out, in0=in0, scalar=s, in1=in1, op0=op0, op1=op1), r, w)

    def cp(eng, out, in_, r, w):
        if eng == "act":
            return T.emit("act", lambda e: e.copy(out=out, in_=in_), r, w)
        return T.emit(eng, lambda e: e.tensor_copy(out=out, in_=in_), r, w)

    def mm(out, lhsT, rhs, start, stop, r, w, signal=True, sgc=False):
        return T.emit("pe", lambda e: e.matmul(out, lhsT=lhsT, rhs=rhs, start=start, stop=stop, skip_group_check=sgc), r, w, signal=signal)

    def dma(q, out, in_, r, w, dsem):
        return T.emit(q, lambda e: e.dma_start(out=out, in_=in_), r, w, dsem=dsem)

    def TM(i):
        return tmp[:, i, :], [("tmp", i)]

    def TB(i):
        return tb[:, i, :], [("tb", i)]

    import os
    KSTOP = float(os.environ.get("KSTOP", "99"))

    def chk(n):
        if KSTOP <= n:
            T.dead = True

    T.emit("pool", lambda e: e.memset(onesf[:], 1.0), [], ["onesf"])
    T.emit("pool", lambda e: e.affine_select(out=ident[:], in_=onesf[:], pattern=[[-1, 128]], compare_op=ALU.is_equal,
                                              fill=0.0, base=0, channel_multiplier=1), ["onesf"], ["ident"])
    T.emit("pool", lambda e: e.affine_select(out=cmask[:], in_=onesf[:], pattern=[[1, 128]], compare_op=ALU.is_ge,
                                              fill=0.0, base=0, channel_multiplier=-1), ["onesf"], ["cmask"])
    T.emit("pool", lambda e: e.memset(cmask[0:64, 64:128], 0.0), [], ["cmask"])
    T.emit("pool", lambda e: e.memset(smask[:], 1.0), [], ["smask"])
    for ch in range(NCH):
        T.emit("pool", lambda e, ch=ch: e.memset(smask[:, ch * 64:ch * 64 + 1], 0.0), [], ["smask"])
    T.emit("pool", lambda e: e.memset(zer[:], 0.0), [], ["zer"])
    T.emit("pool", lambda e: e.memset(S32[:], 0.0), [], ["S32"])
    T.emit("pool", lambda e: e.memset(Sbf[:], 0.0), [], ["Sbf"])
    T.emit("pool", lambda e: e.memset(tails[:], 0.0), [], ["tails"])
    dma("sp", vec[:], vec_d, [], ["vec"], "d_vec")
    act(cact[:], vec[:, c.V_C:c.V_C + KD], AF.Silu, ["vec"], ["cact"])

    chk(0)
    stg = tmp[:].rearrange("p a b -> p (a b)")[:, 0:D]
    stg_keys = [("tmp", i) for i in range(NTMP)]
    kst = tmp[:].rearrange("p a b -> p (a b)")[:, 0:2 * PH * 128]
    dma("sp", kst, keys_d.rearrange("p a n -> p (a n)"), [], stg_keys, "d_stg")
    cp("dve", keysb[:].rearrange("p a n -> p (a n)"), kst, stg_keys, ["keysb"])
    NCB = 8 * D // D
    xflat = xT[:].rearrange("p a b -> p (a b)")
    assert 2 * D <= KD * G
    stgs = [(xflat[:, 0:D], [("stg", 0)]), (xflat[:, D:2 * D], [("stg", 1)])]
    sidx = {"i": 0}

    def next_stg():
        i = sidx["i"] % 2
        sidx["i"] += 1
        return stgs[i][0], stgs[i][1], "d_stg%d" % i
    mp, mpk = PSB(7)
    mm(mp, zer[:, 0:128], zer[:, 0:512], True, False, ["zer"], mpk, sgc=True)
    for k in range(KD):
        for cbk in range(NCB):
            sg_, sgk_, sgd_ = next_stg()
            dma("sp", sg_, wada_d[k, :, cbk * D:(cbk + 1) * D], [], sgk_, sgd_)
            nj = D // 128
            for j in range(nj):
                col = cbk * nj + j
                mm(psum[7][:, col:col + 1], sg_[:, j * 128:(j + 1) * 128], cact[:, k:k + 1], False, True,
                   sgk_ + ["cact"], mpk, signal=(j == nj - 1), sgc=True)
    tt(modT[:], psum[7][:, 0:8 * KD], vec[:, c.V_BADA:c.V_BADA + 8 * KD], ALU.add, mpk + ["vec"], ["modT"])
    def msl(s):
        return modT[:, s * KD:(s + 1) * KD]
    def dsl(nm, n=KD):
        return der[:, DV[nm]:DV[nm] + n]
    stt(dsl("gs_m"), msl(1), 1.0, vec[:, c.V_GMIX:c.V_GMIX + KD], ALU.add, ALU.mult, ["modT", "vec"], ["der"])
    stt(dsl("gs_f"), msl(4), 1.0, vec[:, c.V_GFFN:c.V_GFFN + KD], ALU.add, ALU.mult, ["modT", "vec"], ["der"])
    stt(dsl("gs_o"), msl(7), 1.0, vec[:, c.V_GOUT:c.V_GOUT + KD], ALU.add, ALU.mult, ["modT", "vec"], ["der"])
    for nm, s in (("sh_m", 0), ("gt_m", 2), ("sh_f", 3), ("gt_f", 5), ("sh_o", 6)):
        cp("dve", dsl(nm), msl(s), ["modT"], ["der"])
    tt(dsl("sp", NH), vec[:, c.V_LB0:c.V_LB0 + NH], vec[:, c.V_LB1:c.V_LB1 + NH], ALU.subtract, ["vec", "der"], ["der"])
    act(dsl("lb", NH), dsl("sp", NH), AF.Sigmoid, ["der"], ["der"])
    ts(dsl("oml", NH), dsl("lb", NH), -1.0, 1.0, ALU.mult, ALU.add, ["der"], ["der"])
    ts(dsl("noml", NH), dsl("oml", NH), -1.0, None, ALU.mult, None, ["der"], ["der"])

    chk(1)
    ghflat = GH[:].rearrange("p e a b c d -> p (e a b c d)")
    assert NT * PH * 4 * 128 >= 2 * D or True
    ncb = max(1, min(2, (NT * PH * 4 * 128) // D))
    for i in range(c.NTILES):
        sg_, sgk_, sgd_ = next_stg()
        dma("sp", sg_, wall_d[i], [], sgk_, sgd_)
        s = i % ncb
        cb = ghflat[:, s * D:(s + 1) * D]
        eng = ("dve", "act", "pool")[i % 3]
        cp(eng, cb, sg_, sgk_, [("cb", s)])
        dma("pool", wbt(i), cb, [("cb", s)], [("wb", i)], "d_cbst%d" % s)
    chk(2)
    T.barrier()

    def group_order(halo=False):
        od = []
        for hh in range(NH):
            od += [("t", c.T_WIN + hh), ("t", c.T_WIN + NH + hh), ("t", c.T_WIN + 2 * NH + hh), ("t", c.T_WIN + 3 * NH + hh)]
        for ct in range(NCT):
            od += [("t", c.T_WIN + 4 * NH + ct), ("t", c.T_WIN + 4 * NH + NCT + ct)]
        if halo:
            return od
        od += [("t", c.T_WOUT + i) for i in range(KD)]
        od += [("t", c.T_WPQ + i) for i in range(c.NQT)]
        for sbi in range(c.NSB):
            od += [("t", c.T_UT + sbi * c.SBC + i) for i in range(c.SBC)]
            for dq in range(c.NDQ):
                od += [("v", sbi, dq, cg) for cg in range(c.SBC // c.VU)]
        return od

    order = (group_order(True) if c.HALO else []) + group_order() * c.NGL
    ws = {"issued": 0, "pos": 0}

    def ws_issue(j):
        it = order[j]
        slot = j % NB
        if it[0] == "t":
            dma("sp", wring[:, slot, :], wbt(it[1]), [], [("w", slot)], "d_w%d" % slot)
        else:
            _, sbi, dq, cg = it
            c0 = sbi * c.SBC + cg * c.VU
            W = c.DQW * 128
            src = wbs[3][c0:c0 + c.VU, :, dq * W:(dq + 1) * W].rearrange("c p w -> p c w")
            dst = wring[:, slot, 0:c.VU * W].rearrange("p (c w) -> p c w", c=c.VU)
            dma("sp", dst, src, [], [("w", slot)], "d_w%d" % slot)

    def ws_next(expect):
        j = ws["pos"]
        assert order[j] == expect, (order[j], expect)
        while ws["issued"] < min(len(order), j + NB):
            ws_issue(ws["issued"]); ws["issued"] += 1
        ws["pos"] += 1
        slot = j % NB
        return wring[:, slot, :], [("w", slot)]

    inv_sqrt_eps = None

    def rms_rstd(eps):
        pn, pnk = PSH(7, 0)
        for k in range(KD):
            sq, sqk = TM(k % 2)
            act(sq, xT[:, k, :], AF.Square, [("xT", k)], sqk)
            mm(pn, onesf[:], sq, k == 0, k == KD - 1, ["onesf"] + sqk, pnk, signal=(k % 2 == 1 or k == KD - 1))
        sd, sdk = TM(2)
        act(sd, pn, AF.Sqrt, pnk, sdk, bias=eps_ap, scale=1.0 / D)
        rs, rsk = TM(3)
        T.emit("dve", lambda e: e.reciprocal(out=rs, in_=sd), sdk, rsk)
        return rs, rsk

    def make_hT(gs, sh, rs, rsk):
        for k in range(KD):
            t, tkk = TM(4 + k % 2)
            tt(t, xT[:, k, :], rs, ALU.mult, [("xT", k)] + rsk, tkk)
            act(hT[:, k, :], t, AF.Identity, tkk + ["der"], [("hT", k)], bias=dv(sh, k), scale=dv(gs, k))

    def proj(wt, wtk, rhs_tile, rhs_key, out, outk):
        for k in range(KD):
            mm(out, wt[:, k * 128:(k + 1) * 128], rhs_tile[:, k, :], k == 0, k == KD - 1,
               wtk + [(rhs_key, k)], outk, signal=(k == KD - 1))

    eps_t = sb("eps_t", [128, 2], F32)
    T.emit("pool", lambda e: e.memset(eps_t[:, 0:1], 1e-6), [], ["eps"])
    T.emit("pool", lambda e: e.memset(eps_t[:, 1:2], 1e-5), [], ["eps"])
    eps_ap = eps_t[:, 0:1]
    lneps_ap = eps_t[:, 1:2]

    proj_slots = [(0, 0), (1, 0), (5, 0), (6, 0)]
    pslot = {"i": 0}

    def next_pslot():
        b, h = proj_slots[pslot["i"] % len(proj_slots)]
        pslot["i"] += 1
        return PSH(b, h)

    for g in range(c.NGP):
        halo = (c.HALO == 1 and g == 0)
        xk = [("xT", k) for k in range(KD)]
        dma("pool", xT[:], xT_d[g], [], xk, "d_x")
        rs, rsk = rms_rstd(1e-6)
        make_hT("gs_m", "sh_m", rs, rsk)
        chk(3)
        for hh in range(NH):
            pq, pqk = next_pslot(); pf, pfk = next_pslot(); pi, pik = next_pslot(); pg, pgk = next_pslot()
            for (po, pok, tix) in ((pq, pqk, hh), (pf, pfk, NH + hh), (pi, pik, 2 * NH + hh), (pg, pgk, 3 * NH + hh)):
                wt, wtk = ws_next(("t", c.T_WIN + tix))
                proj(wt, wtk, hT, "hT", po, pok)
            q, qk = TM(6); sg, sgk = TM(7); sgg, sggk = TM(8)
            act(q, pq, AF.Silu, pqk, qk)
            act(sgg, pg, AF.Silu, pgk, sggk)
            act(sg, pf, AF.Sigmoid, pfk, sgk)
            vT, vTk = TB(0)
            cp("act", vT, pi, pik, vTk)
            chk(3.1)
            f, fk = TM(9); kk, kkk = TM(10)
            ts(f, sg, dv("oml", hh), dv("lb", hh), ALU.mult, ALU.add, sgk + ["der"], fk)
            ts(kk, sg, dv("noml", hh), dv("oml", hh), ALU.mult, ALU.add, sgk + ["der"], kkk)
            lf, lfk = TM(11)
            act(lf, f, AF.Ln, fk, lfk)
            b, bk = TM(12)
            T.emit("dve", lambda e, b=b, lf=lf: e.tensor_tensor_scan(out=b, data0=smask[:], data1=lf, initial=0.0,
                                                                      op0=ALU.mult, op1=ALU.add), lfk + ["smask"], bk)
            chk(3.2)
            eb, ebk = TM(13); enb, enbk = TM(14)
            act(eb, b, AF.Exp, bk, ebk)
            act(enb, b, AF.Exp, bk, enbk, scale=-1.0)
            dc = dch[:, hh % 2, :]; dck = [("dch", hh % 2)]
            blast = b.rearrange("p (c s) -> p c s", s=64)[:, :, 63]
            act(dc, blast, AF.Exp, bk, dck)
            Qt, Qtk = TB(1); Ktb, Ktbk = TB(2); Kh, Khk = TB(3)
            tt(Qt, q, eb, ALU.mult, qk + ebk, Qtk)
            K32, K32k = TM(15)
            tt(K32, kk, enb, ALU.mult, kkk + enbk, K32k)
            cp("act", Ktb, K32, K32k, Ktbk)
            tt(Kh.rearrange("p (c s) -> p c s", s=64), K32.rearrange("p (c s) -> p c s", s=64),
               dc.unsqueeze(2).broadcast_to([128, NCH, 64]), ALU.mult, K32k + dck, Khk)
            chk(3.3)
            psc, psck = PSH(2, 0)
            for tb_ in range(NT):
                mm(psc[:, tb_ * 128:(tb_ + 1) * 128], Ktb[:, tb_ * 128:(tb_ + 1) * 128], Qt[:, tb_ * 128:(tb_ + 1) * 128],
                   True, True, Ktbk + Qtk, psck, signal=(tb_ == NT - 1))
            scm, scmk = TB(4)
            tt(scm.rearrange("p (a t) -> p a t", a=NT), psc.rearrange("p (a t) -> p a t", a=NT),
               cmask[:].unsqueeze(1).broadcast_to([128, NT, 128]), ALU.mult, psck + ["cmask"], scmk)
            chk(3.4)
            pv, pvk = PSH(3, 0); pk, pkk = PSH(4, 0)
            for tb_ in range(NT):
                mm(pv[:, tb_ * 128:(tb_ + 1) * 128], vT[:, tb_ * 128:(tb_ + 1) * 128], ident[:], True, True,
                   vTk + ["ident"], pvk, signal=(tb_ == NT - 1))
            chk(3.41)
            for tb_ in range(NT):
                mm(pk[:, tb_ * 128:(tb_ + 1) * 128], Kh[:, tb_ * 128:(tb_ + 1) * 128], ident[:], True, True,
                   Khk + ["ident"], pkk, signal=(tb_ == NT - 1))
            chk(3.42)
            Vt, Vtk = TB(5); Kt, Ktk = TB(6)
            cp("act", Vt, pv, pvk, Vtk)
            chk(3.43)
            ts(Kt, pk, 1.0, None, ALU.mult, None, pkk, Ktk)
            chk(3.5)
            po_, pok_ = PSH(7, 0)
            for tb_ in range(NT):
                mm(po_[:, tb_ * 128:(tb_ + 1) * 128], Vt[:, tb_ * 128:(tb_ + 1) * 128], scm[:, tb_ * 128:(tb_ + 1) * 128],
                   tb_ == 0, False, Vtk + scmk, pok_, signal=False, sgc=True)
            pss, pssk = PSH(2, 0)
            skey = [("S", hh)]
            for ch in range(NCH):
                tb_ = ch // 2; p0 = (ch % 2) * 64
                mm(po_[:, ch * 64:(ch + 1) * 64], Sbf[:, hh, :], Qt[:, ch * 64:(ch + 1) * 64], False, True,
                   [("Sb", hh)] + Qtk, pok_, signal=True, sgc=True)
                mm(pss[:, 0:128], Kt[p0:p0 + 64, tb_ * 128:(tb_ + 1) * 128], Vt[p0:p0 + 64, tb_ * 128:(tb_ + 1) * 128],
                   True, True, Ktk + Vtk, pssk)
                stt(S32[:, hh, :], S32[:, hh, :], dc[:, ch:ch + 1], pss[:, 0:128], ALU.mult, ALU.add,
                    skey + dck + pssk, skey)
                cp("act", Sbf[:, hh, :], S32[:, hh, :], skey, [("Sb", hh)])
            chk(3.6)
            osq, osqk = TM(16)
            act(osq, po_, AF.Square, pok_, osqk)
            pn, pnk = PSH(3, 0)
            mm(pn, onesf[:], osq, True, True, ["onesf"] + osqk, pnk)
            sd, sdk = TM(17)
            act(sd, pn, AF.Sqrt, pnk, sdk, bias=eps_ap, scale=1.0 / 128)
            ri, rik = TM(16)
            T.emit("dve", lambda e, ri=ri, sd=sd: e.reciprocal(out=ri, in_=sd), sdk, rik)
            t1, t1k = TM(17)
            tt(t1, po_, ri, ALU.mult, pok_ + rik, t1k)
            stt(ycat[:, hh, :], t1, vc(c.V_HGN, hh), sgg, ALU.mult, ALU.mult, t1k + sggk + ["vec"], [("yc", hh)])
        chk(4)
        pl1, pl1k = PSH(2, 0); pl2, pl2k = PSH(3, 0)
        for ct in range(NCT):
            pa, pak = next_pslot(); pb, pbk = next_pslot()
            wt, wtk = ws_next(("t", c.T_WIN + 4 * NH + ct)); proj(wt, wtk, hT, "hT", pa, pak)
            wt, wtk = ws_next(("t", c.T_WIN + 4 * NH + NCT + ct)); proj(wt, wtk, hT, "hT", pb, pbk)
            sgb, sgbk = TM(6 + ct % 2)
            act(sgb, pb, AF.Sigmoid, pbk + ["vec"], sgbk, bias=vc(c.V_BGB, ct))
            ub = ubuf[:, ct % 2, :]; ubk = [("ub", ct % 2)]
            cp("pool", ub[:, 0:30], tails[:, ct, :], [("tl", ct)], ubk)
            stt(ub[:, 30:30 + G], pa, vc(c.V_BGA, ct), sgb, ALU.add, ALU.mult, pak + sgbk + ["vec"], ubk)
            cp("pool", tails[:, ct, :], ub[:, G:G + 30], ubk, [("tl", ct)])
            acc, acck = TM(8 + ct % 2)
            ts(acc, ub[:, 0:G], vc(c.V_WDW, ct * 31), vc(c.V_BDW, ct), ALU.mult, ALU.add, ubk + ["vec"], acck)
            for j in range(1, 31):
                stt(acc, ub[:, j:j + G], vc(c.V_WDW, ct * 31 + j), acc, ALU.mult, ALU.add, ubk + acck + ["vec"], acck)
            sq, sqk = TM(10 + ct % 2)
            act(sq, acc, AF.Square, acck, sqk)
            mm(pl1, onesf[:], acc, ct == 0, ct == NCT - 1, ["onesf"] + acck, pl1k)
            mm(pl2, onesf[:], sq, ct == 0, ct == NCT - 1, ["onesf"] + sqk, pl2k)
            cp("act", ycat[:, NH + ct, :], acc, acck, [("yc", NH + ct)])
        if halo:
            hm = vec[:, c.V_HM:c.V_HM + 1]
            allS = [("S", h_) for h_ in range(NH)]; allSb = [("Sb", h_) for h_ in range(NH)]; allT = [("tl", t_) for t_ in range(NCT)]
            ts(S32[:].rearrange("p a b -> p (a b)"), S32[:].rearrange("p a b -> p (a b)"), hm, None, ALU.mult, None, allS + ["vec"], allS)
            ts(Sbf[:].rearrange("p a b -> p (a b)"), Sbf[:].rearrange("p a b -> p (a b)"), hm, None, ALU.mult, None, allSb + ["vec"], allSb)
            ts(tails[:].rearrange("p a b -> p (a b)"), tails[:].rearrange("p a b -> p (a b)"), hm, None, ALU.mult, None, allT + ["vec"], allT)
            continue
        mean, meank = TM(12); msq, msqk = TM(13); var, vark = TM(14); lsd, lsdk = TM(15); lrs, lrsk = TM(16)
        act(mean, pl1, AF.Identity, pl1k, meank, scale=1.0 / c.CW)
        tt(msq, mean, mean, ALU.mult, meank, msqk)
        stt(var, pl2, 1.0 / c.CW, msq, ALU.mult, ALU.subtract, pl2k + msqk, vark)
        act(lsd, var, AF.Sqrt, vark, lsdk, bias=lneps_ap, scale=1.0)
        T.emit("dve", lambda e, lrs=lrs, lsd=lsd: e.reciprocal(out=lrs, in_=lsd), lsdk, lrsk)
        for ct in range(NCT):
            t, tk_ = TM(6 + ct % 2); t2, t2k = TM(8 + ct % 2)
            tt(t, ycat[:, NH + ct, :], mean, ALU.subtract, [("yc", NH + ct)] + meank, tk_)
            tt(t2, t, lrs, ALU.mult, tk_ + lrsk, t2k)
            act(ycat[:, NH + ct, :], t2, AF.Silu, t2k + ["vec"], [("yc", NH + ct)], bias=vc(c.V_LNB, ct), scale=vc(c.V_LNG, ct))
        chk(5)
        for dt in range(KD):
            wt, wtk = ws_next(("t", c.T_WOUT + dt))
            po2, po2k = next_pslot()
            proj(wt, wtk, ycat, "yc", po2, po2k)
            stt(xT[:, dt, :], po2, dv("gt_m", dt), xT[:, dt, :], ALU.mult, ALU.add, po2k + ["der", ("xT", dt)], [("xT", dt)])
        chk(6)
        rs, rsk = rms_rstd(1e-6)
        make_hT("gs_f", "sh_f", rs, rsk)
        qTt = PT
        for jt in range(c.NQT):
            wt, wtk = ws_next(("t", c.T_WPQ + jt))
            pq2, pq2k = next_pslot()
            proj(wt, wtk, hT, "hT", pq2, pq2k)
            cp("act", qTt[:, jt, :], pq2, pq2k, [("PT", jt)])
        chk(7)
        for tt_ in range(NT):
            for h in range(PH):
                b_, hf = [(0, 0), (1, 0), (2, 0), (3, 0)][h % 4]
                pS, pSk = PSH(b_, hf)
                for half in range(2):
                    mm(pS[:, half * 128:(half + 1) * 128], qTt[:, 2 * h + half, tt_ * 128:(tt_ + 1) * 128], keysb[:, 2 * h + half, :],
                       True, True, [("PT", 2 * h + half), "keysb"], pSk, signal=(half == 1))
                skey2 = [("s12", tt_, h)]
                cp("act", s12[:, tt_, h, :, :].rearrange("p a n -> p (a n)"), pS, pSk, skey2)
                for half in range(2):
                    sv = s12[:, tt_, h, half, :]
                    v16 = tk[:, half, :]
                    T.emit("dve", lambda e, sv=sv, v16=v16: e.max(out=v16[:, 0:8], in_=sv), skey2, [("tk", half)])
                    wv = tkw[:, 0, 0:128]
                    T.emit("dve", lambda e, sv=sv, v16=v16, wv=wv: e.match_replace(out=wv, in_to_replace=v16[:, 0:8], in_values=sv, imm_value=NEG),
                           skey2 + [("tk", half)], [("tkw", 0)])
                    T.emit("dve", lambda e, v16=v16, wv=wv: e.max(out=v16[:, 8:16], in_=wv), [("tkw", 0)], [("tk", half)])
                cand = tkw[:, 1, :]
                tt(cand.rearrange("p (a b) -> p a b", a=16), tk[:, 0, :].unsqueeze(2).broadcast_to([128, 16, 16]),
                   tk[:, 1, :].unsqueeze(1).broadcast_to([128, 16, 16]), ALU.add, [("tk", 0), ("tk", 1)], [("tkw", 1)], eng="pool")
                tops = tk[:, 2, :]
                T.emit("dve", lambda e, tops=tops, cand=cand: e.max(out=tops[:, 0:8], in_=cand), [("tkw", 1)], [("tk", 2)])
                cw = tkw[:, 0, :]
                T.emit("dve", lambda e, tops=tops, cand=cand, cw=cw: e.match_replace(out=cw, in_to_replace=tops[:, 0:8], in_values=cand, imm_value=NEG),
                       [("tkw", 1), ("tk", 2)], [("tkw", 0)])
                T.emit("dve", lambda e, tops=tops, cw=cw: e.max(out=tops[:, 8:16], in_=cw), [("tkw", 0)], [("tk", 2)])
                cp("dve", tau[:, tt_, h:h + 1], tops[:, 15:16], [("tk", 2)], [("tau", tt_, h)])
                negm = tk[:, 3, 0:1]; Z = tk[:, 3, 1:2]; lnZ = tk[:, 3, 2:3]; exs = tk[:, 4, :]
                ts(negm, tops[:, 0:1], -1.0, None, ALU.mult, None, [("tk", 2)], [("tk", 3)])
                act(exs, tops, AF.Exp, [("tk", 2), ("tk", 3)], [("tk", 4), ("tk", 3)], bias=negm, accum=Z)
                act(lnZ, Z, AF.Ln, [("tk", 3)], [("tk", 3)])
                tt(nbias[:, tt_, h:h + 1], negm, lnZ, ALU.subtract, [("tk", 3)], [("nb", tt_, h)])
        chk(8)
        acc_slots = [(0, 0), (1, 0), (2, 0), (3, 0)]
        for sbi in range(c.NSB):
            for blk in range(c.SBC // 2):
                c0 = sbi * c.SBC + blk * 2
                gb = blk % 2
                for tt_ in range(NT):
                    for h in range(PH):
                        i_ = (tt_ * PH + h) % 4
                        val, valk = TM(i_)
                        Wv, Wk = TM(4 + i_)
                        tt(val.rearrange("p (a n) -> p a n", a=2),
                           s12[:, tt_, h, 0, c0:c0 + 2].unsqueeze(2).broadcast_to([128, 2, 128]),
                           s12[:, tt_, h, 1, :].unsqueeze(1).broadcast_to([128, 2, 128]), ALU.add,
                           [("s12", tt_, h)], valk, eng="pool")
                        act(Wv, val, AF.Exp, valk + [("nb", tt_, h)], Wk, bias=nbias[:, tt_, h:h + 1])
                        stt(GH[:, gb, tt_, h, :, :].rearrange("p a n -> p (a n)"), val, tau[:, tt_, h:h + 1], Wv, ALU.is_ge, ALU.mult,
                            valk + Wk + [("tau", tt_, h)], [("GH", gb, tt_, h)])
                for ci in range(2):
                    ch_ = c0 + ci
                    cc = blk * 2 + ci
                    ut, utk = ws_next(("t", c.T_UT + ch_))
                    pA, pAk = PSH(4 + 2 * (cc % 2), 0)
                    proj(ut, utk, hT, "hT", pA, pAk)
                    pG, pGk = PSH(5 + 2 * (cc % 2), 0)
                    for tt_ in range(NT):
                        for h in range(PH):
                            mm(pG[:, tt_ * 128:(tt_ + 1) * 128], GH[:, gb, tt_, h, ci, :], ident[:], h == 0, h == PH - 1,
                               [("GH", gb, tt_, h), "ident"], pGk, signal=(h == PH - 1 and tt_ == NT - 1))
                    ge, gek = TM(8 + cc % 2)
                    act(ge, pA, AF.Gelu, pAk, gek)
                    tt(PT[:, cc, :], ge, pG, ALU.mult, gek + pGk, [("PT", cc)])
            for dq in range(c.NDQ):
                nb_ = c.DQW
                for bnk in range(nb_):
                    pz, pzk = PSB(bnk)
                    mm(pz, zer[:, 0:128], zer[:, 0:512], True, False, ["zer"], pzk, sgc=True)
                for cg in range(c.SBC // c.VU):
                    vb, vbk = ws_next(("v", sbi, dq, cg))
                    W = c.DQW * 128
                    for ci in range(c.VU):
                        cc = cg * c.VU + ci
                        for dtl in range(c.DQW):
                            pa2, pa2k = PSH(*acc_slots[dtl])
                            mm(pa2, vb[:, ci * W + dtl * 128:ci * W + (dtl + 1) * 128], PT[:, cc, :], False, True,
                               vbk + [("PT", cc)], pa2k, signal=(cc == c.SBC - 1 or (ci == c.VU - 1 and dtl == c.DQW - 1)), sgc=True)
                for dtl in range(c.DQW):
                    dt = dq * c.DQW + dtl
                    pa2, pa2k = PSH(*acc_slots[dtl])
                    stt(xT[:, dt, :], pa2, dv("gt_f", dt), xT[:, dt, :], ALU.mult, ALU.add,
                        pa2k + ["der", ("xT", dt)], [("xT", dt)])
        chk(9)
        rs, rsk = rms_rstd(1e-6)
        for k4 in range(0, KD, 2):
            ob = obuf[:, (k4 // 2) % 2, :, :]; obk = [("ob", (k4 // 2) % 2)]
            for k in range(k4, min(KD, k4 + 2)):
                t, tkk = TM(4 + k % 2)
                tt(t, xT[:, k, :], rs, ALU.mult, [("xT", k)] + rsk, tkk)
                act(ob[:, k - k4, :], t, AF.Identity, tkk + ["der"], obk, bias=dv("sh_o", k), scale=dv("gs_o", k))
            n = min(KD, k4 + 2) - k4
            dma("pool", out_d[g - c.HALO, :, k4:k4 + n, :], ob[:, 0:n, :], obk, [("out", g, k4)], "d_o%d" % ((k4 // 2) % 2))
    T.dead = False
    T.final_wait("sp")
    T.replay(nc, es)
    es.close()
    return nc


def host_prepare(cfg, x, c, w_ada, b_ada, w_ada_out, b_ada_out, g_mix, g_ffn, g_out, w_in, b_glu,
                 lb_logits, w_dw, b_dw, ln_g, ln_b, hg_norm_g, w_out, w_pq, sub_keys1, sub_keys2,
                 expert_u, expert_v):
    cf = cfg
    D, KD, G = cf.D, cf.KD, cf.G
    f = lambda a: np.asarray(a, dtype=np.float32)
    x = f(x)[0]
    xTall = x.reshape(cf.NG, G, KD, 128).transpose(0, 3, 2, 1)

    def coltiles(w):
        K, N = w.shape
        return w.reshape(K // 128, 128, N // 128, 128).transpose(2, 1, 0, 3).reshape(N // 128, 128, K)

    wall = np.empty((cf.NTILES, 128, D), np.float32)
    wall[cf.T_WIN:cf.T_WIN + cf.NCOL] = coltiles(f(w_in)[0])
    wall[cf.T_WOUT:cf.T_WOUT + KD] = coltiles(f(w_out)[0])
    wall[cf.T_WPQ:cf.T_WPQ + cf.NQT] = coltiles(f(w_pq)[0])
    u = f(expert_u)[0]
    wall[cf.T_UT:cf.T_UT + cf.NEC] = u.reshape(cf.NEC, 128, KD, 128).transpose(0, 3, 2, 1).reshape(cf.NEC, 128, D)
    wall[cf.T_V:cf.T_V + cf.NEC] = f(expert_v)[0].reshape(cf.NEC, 128, D)
    wcat = np.concatenate([f(w_ada)[0], f(w_ada_out)], axis=1)
    wada = np.ascontiguousarray(wcat.reshape(KD, 128, 8 * D))
    vec = np.zeros((128, cf.NV), np.float32)

    def put(off, v, n):
        vec[:, off:off + n] = v.reshape(n, 128).T

    put(cf.V_C, f(c)[0], KD)
    put(cf.V_BADA, np.concatenate([f(b_ada)[0], f(b_ada_out)]), 8 * KD)
    put(cf.V_GMIX, f(g_mix)[0], KD); put(cf.V_GFFN, f(g_ffn)[0], KD); put(cf.V_GOUT, f(g_out), KD)
    bg = f(b_glu)[0]
    put(cf.V_BGA, bg[:cf.CW], cf.NCT); put(cf.V_BGB, bg[cf.CW:], cf.NCT)
    lbl = f(lb_logits)
    put(cf.V_LB0, lbl[0], cf.NH); put(cf.V_LB1, lbl[1], cf.NH)
    wd = f(w_dw)[0]
    vec[:, cf.V_WDW:cf.V_WDW + cf.NCT * 31] = wd.reshape(31, cf.NCT, 128).transpose(2, 1, 0).reshape(128, cf.NCT * 31)
    put(cf.V_BDW, f(b_dw)[0], cf.NCT); put(cf.V_LNG, f(ln_g)[0], cf.NCT); put(cf.V_LNB, f(ln_b)[0], cf.NCT)
    put(cf.V_HGN, f(hg_norm_g)[0], cf.NH)
    k1 = f(sub_keys1)[0]; k2 = f(sub_keys2)[0]
    ks = np.stack([k1, k2], axis=1).reshape(2 * cf.PH, 128, 128)
    keysT = np.ascontiguousarray(ks.transpose(2, 0, 1))
    maps = []
    for r in range(cf.NCORES):
        g0 = r * cf.NGL
        xr = np.zeros((cf.NGP, 128, KD, G), np.float32)
        if cf.HALO and r > 0:
            xr[0] = xTall[g0 - 1]
        xr[cf.HALO:] = xTall[g0:g0 + cf.NGL]
        vr = vec.copy()
        vr[:, cf.V_HM] = 0.0 if r == 0 else 1.0
        maps.append({"xT": xr, "wall": wall, "wada": wada, "vec": vr, "keysT": keysT})
    return maps


def run(cfg, inputs):
    nc = build(cfg)
    maps = host_prepare(cfg, **inputs)
    res = run_bass_kernel_spmd(nc, maps, core_ids=list(range(cfg.NCORES)))
    oT = np.concatenate([res.results[r]["outT"] for r in range(cfg.NCORES)], axis=0)
    out = oT.transpose(0, 3, 2, 1).reshape(1, cfg.SEQ, cfg.D)
    return np.ascontiguousarray(out.astype(np.float32))


def kernel(**inputs):
    return run(Cfg(), inputs)
```
